# Optimizing a Trainium2 kernel written in Bass

```python
import jax
import jax.numpy as jnp
from jax import lax
import numpy as np

D_MODEL = 1024
BATCH = 32
SEQ = 2048
DEPTH = 4

D_MIX = 1024
M_HEADS = 4
M_HEAD_DIM = 96
M_WIDTH = M_HEADS * M_HEAD_DIM
M_CHUNK = 128
CONV_WIDTH = 4
A_HEADS = 6
A_NOPE = 64
A_ROPE = 32
A_QK_DIM = A_NOPE + A_ROPE
A_V_DIM = 64
A_WIDTH = A_HEADS * A_V_DIM
A_Q_RANK = 256
A_KV_RANK = 128
ROPE_THETA = 10000.0
Q_BLOCK = 128
C_GROUPS = 4
C_GROUP_DIM = 64
C_WIDTH = C_GROUPS * C_GROUP_DIM
C_CHUNK = 128
IN_SIZES = (M_WIDTH, M_WIDTH, M_WIDTH, M_WIDTH, M_HEADS, M_HEADS,
            A_Q_RANK, A_KV_RANK, A_ROPE, C_WIDTH, C_WIDTH)
D_IN = 4 * M_WIDTH + 2 * M_HEADS + A_Q_RANK + A_KV_RANK + A_ROPE + 2 * C_WIDTH
N_GROUPS = 4
EXPERTS_PER_GROUP = 8
N_EXPERTS = N_GROUPS * EXPERTS_PER_GROUP
TOP_K = 2
D_EXPERT = 256
MOE_BLOCK = 256
EPS = 1e-6

kernel_name = 'hybrid_mlstm_mla_gmlp_hmoe'


def rms_norm(x, gain):
    xf = x.astype(jnp.float32)
    y = xf * lax.rsqrt(jnp.mean(xf * xf, axis=-1, keepdims=True) + EPS)
    return (y * gain.astype(jnp.float32)).astype(x.dtype)


def head_rms_norm(x, gain, n_heads):
    b, s, w = x.shape
    y = rms_norm(x.reshape(b, s, n_heads, w // n_heads), gain.reshape(n_heads, w // n_heads))
    return y.reshape(b, s, w)


def split_cols(t, sizes):
    out, off = [], 0
    for size in sizes:
        out.append(t[..., off:off + size])
        off += size
    return out


def causal_depthwise_conv(x, w, b):
    width, seq = w.shape[0], x.shape[1]
    xp = jnp.pad(x, ((0, 0), (width - 1, 0), (0, 0)))
    y = b
    for j in range(width):
        y = y + xp[:, j:j + seq] * w[j]
    return y


def mlstm_chunkwise(q, k, v, i_pre, f_pre):
    B, S, H, Dh = q.shape
    L = M_CHUNK
    N = S // L
    f32 = jnp.float32
    def to_chunks(t):
        return t.astype(f32).reshape(B, N, L, H, -1).transpose(1, 0, 3, 2, 4)
    qc = to_chunks(q)
    kc = to_chunks(k) * (Dh ** -0.5)
    vc = to_chunks(v)
    log_f = jax.nn.log_sigmoid(f_pre.astype(f32)).reshape(B, N, L, H).transpose(1, 0, 3, 2)
    log_i = i_pre.astype(f32).reshape(B, N, L, H).transpose(1, 0, 3, 2)
    bcum = jnp.cumsum(log_f, axis=-1)
    causal = jnp.tril(jnp.ones((L, L), dtype=bool))

    def step(carry, inp):
        C, n, m = carry
        qt, kt, vt, bt, it = inp
        d = bt[..., :, None] - bt[..., None, :] + it[..., None, :]
        d = jnp.where(causal, d, -jnp.inf)
        inter = bt + m[..., None]
        m_row = jnp.maximum(inter, jnp.max(d, axis=-1))
        w_intra = jnp.exp(d - m_row[..., None])
        w_inter = jnp.exp(inter - m_row)
        s = jnp.einsum('bhld,bhsd->bhls', qt, kt) * w_intra
        num = (jnp.einsum('bhls,bhsd->bhld', s, vt)
               + w_inter[..., None] * jnp.einsum('bhvk,bhlk->bhlv', C, qt))
        den = jnp.sum(s, axis=-1) + w_inter * jnp.einsum('bhk,bhlk->bhl', n, qt)
        h = num / jnp.maximum(jnp.abs(den), jnp.exp(-m_row))[..., None]
        b_last = bt[..., -1]
        g = b_last[..., None] - bt + it
        m_new = jnp.maximum(b_last + m, jnp.max(g, axis=-1))
        a = jnp.exp(b_last + m - m_new)
        wg = jnp.exp(g - m_new[..., None])
        C_new = a[..., None, None] * C + jnp.einsum('bhs,bhsv,bhsk->bhvk', wg, vt, kt)
        n_new = a[..., None] * n + jnp.einsum('bhs,bhsk->bhk', wg, kt)
        return (C_new, n_new, m_new), h

    init = (jnp.zeros((B, H, Dh, Dh), f32), jnp.zeros((B, H, Dh), f32), jnp.zeros((B, H), f32))
    _, h = lax.scan(step, init, (qc, kc, vc, bcum, log_i))
    return h.transpose(1, 0, 3, 2, 4).reshape(B, S, H, Dh).astype(v.dtype)


def rope_tables(positions):
    inv_freq = 1.0 / (ROPE_THETA ** (jnp.arange(0, A_ROPE, 2, dtype=jnp.float32) / A_ROPE))
    ang = positions.astype(jnp.float32)[..., None] * inv_freq
    return jnp.cos(ang), jnp.sin(ang)


def apply_rope(x, cos, sin):
    half = x.shape[-1] // 2
    xf = x.astype(jnp.float32)
    c, s = cos[:, :, None, :], sin[:, :, None, :]
    x1, x2 = xf[..., :half], xf[..., half:]
    return jnp.concatenate([x1 * c - x2 * s, x1 * s + x2 * c], axis=-1).astype(x.dtype)


def causal_block_attention(q, k, v):
    B, S, H, Dq = q.shape
    nb = S // Q_BLOCK
    scale = Dq ** -0.5
    qb = q.reshape(B, nb, Q_BLOCK, H, Dq).transpose(1, 0, 2, 3, 4)
    kpos = jnp.arange(S)

    def one_block(args):
        qi, idx = args
        s = jnp.einsum('blhd,bshd->bhls', qi, k, preferred_element_type=jnp.float32) * scale
        qpos = idx * Q_BLOCK + jnp.arange(Q_BLOCK)
        s = jnp.where(kpos[None, :] <= qpos[:, None], s, -jnp.inf)
        p = jax.nn.softmax(s, axis=-1).astype(v.dtype)
        return jnp.einsum('bhls,bshd->blhd', p, v)

    o = lax.map(one_block, (qb, jnp.arange(nb)))
    return o.transpose(1, 0, 2, 3, 4).reshape(B, S, H, v.shape[-1])


def mla_mixer(c_q, c_kv, k_rope, cos, sin, q_gain, kv_gain, w_uq, w_ukv, qh_gain, kh_gain):
    B, S, _ = c_q.shape
    q = (rms_norm(c_q, q_gain) @ w_uq).reshape(B, S, A_HEADS, A_QK_DIM)
    kv = (rms_norm(c_kv, kv_gain) @ w_ukv).reshape(B, S, A_HEADS, A_NOPE + A_V_DIM)
    k_nope, v = kv[..., :A_NOPE], kv[..., A_NOPE:]
    k = jnp.concatenate([k_nope, jnp.broadcast_to(k_rope[:, :, None, :], (B, S, A_HEADS, A_ROPE))], axis=-1)
    q = rms_norm(q, qh_gain)
    k = rms_norm(k, kh_gain)
    q = jnp.concatenate([q[..., :A_NOPE], apply_rope(q[..., A_NOPE:], cos, sin)], axis=-1)
    k = jnp.concatenate([k[..., :A_NOPE], apply_rope(k[..., A_NOPE:], cos, sin)], axis=-1)
    o = causal_block_attention(q, k, v)
    return o.reshape(B, S, A_WIDTH)


def chunk_spatial_gating(u, v, v_gain, w_s, b_s):
    B, S, _ = u.shape
    n = S // C_CHUNK
    v = rms_norm(v.reshape(B, S, C_GROUPS, C_GROUP_DIM), v_gain.reshape(C_GROUPS, C_GROUP_DIM))
    vc = v.reshape(B, n, C_CHUNK, C_GROUPS, C_GROUP_DIM)
    w = w_s * jnp.tril(jnp.ones((C_CHUNK, C_CHUNK), dtype=w_s.dtype))
    mixed = jnp.einsum('gts,bnsgd->bntgd', w, vc) + b_s.T[None, None, :, :, None]
    return u * mixed.reshape(B, S, C_WIDTH)


def hier_moe(x, w_group, b_group, w_expert, b_expert, w1, w3, w2):
    B, S, D = x.shape
    T = B * S
    xt = x.reshape(T, D)
    f32 = jnp.float32
    g_logits = (xt @ w_group).astype(f32)
    g_prob = jax.nn.softmax(g_logits, axis=-1)
    _, g_sel = lax.top_k(g_logits + b_group.astype(f32), 1)
    g_sel = g_sel[:, 0]
    tok = jnp.arange(T)
    g_gate = g_prob[tok, g_sel]
    e_logits = (xt @ w_expert).astype(f32).reshape(T, N_GROUPS, EXPERTS_PER_GROUP)
    e_logits = e_logits[tok, g_sel]
    e_bias = b_expert.astype(f32).reshape(N_GROUPS, EXPERTS_PER_GROUP)[g_sel]
    e_prob = jax.nn.softmax(e_logits, axis=-1)
    _, e_local = lax.top_k(e_logits + e_bias, TOP_K)
    e_w = jnp.take_along_axis(e_prob, e_local, axis=1)
    e_w = e_w / jnp.sum(e_w, axis=-1, keepdims=True)
    weights = g_gate[:, None] * e_w
    expert_id = g_sel[:, None] * EXPERTS_PER_GROUP + e_local

    A = T * TOP_K
    flat_e = expert_id.reshape(A)
    flat_tok = jnp.arange(A, dtype=jnp.int32) // TOP_K
    flat_w = weights.reshape(A)
    order = jnp.argsort(flat_e)
    se, stok, sw = flat_e[order], flat_tok[order], flat_w[order]
    counts = jnp.bincount(flat_e, length=N_EXPERTS)
    starts = jnp.cumsum(counts) - counts
    padded = ((counts + MOE_BLOCK - 1) // MOE_BLOCK) * MOE_BLOCK
    pad_ends = jnp.cumsum(padded)
    pad_starts = pad_ends - padded
    dest = pad_starts[se] + jnp.arange(A) - starts[se]
    n_blocks = -(-A // MOE_BLOCK) + N_EXPERTS
    P = n_blocks * MOE_BLOCK
    row_tok = jnp.full((P,), T, dtype=jnp.int32).at[dest].set(stok)
    row_w = jnp.zeros((P,), dtype=x.dtype).at[dest].set(sw.astype(x.dtype))
    block_start = jnp.arange(n_blocks) * MOE_BLOCK
    block_e = jnp.minimum(jnp.sum(pad_ends[None, :] <= block_start[:, None], axis=1), N_EXPERTS - 1)
    x_pad = jnp.concatenate([xt, jnp.zeros((1, D), dtype=x.dtype)], axis=0)

    def expert_block(args):
        rows, rw, e = args
        xi = x_pad[rows]
        h = jax.nn.silu(xi @ w1[e]) * (xi @ w3[e])
        return (h @ w2[e]) * rw[:, None]

    yb = lax.map(expert_block, (row_tok.reshape(n_blocks, MOE_BLOCK), row_w.reshape(n_blocks, MOE_BLOCK), block_e))
    out = jnp.zeros((T + 1, D), dtype=x.dtype).at[row_tok].add(yb.reshape(P, D))[:T]
    return out.reshape(B, S, D)


def setup_inputs(seed: int = 0) -> dict:
    key = jax.random.key(seed)
    ks = jax.random.split(key, 32)
    nrm = jax.random.normal
    def gain(k, n):
        return 1.0 + 0.02 * nrm(k, (DEPTH, n), jnp.float32)
    x = nrm(ks[0], (BATCH, SEQ, D_MODEL), jnp.float32)
    offsets = jax.random.randint(ks[1], (BATCH, 1), 0, 4096, dtype=jnp.int32)
    positions = offsets + jnp.arange(SEQ, dtype=jnp.int32)[None, :]
    f_bias = jnp.linspace(3.0, 6.0, M_HEADS, dtype=jnp.float32)[None, :]
    m_gate_bias = jnp.concatenate([0.1 * nrm(ks[6], (DEPTH, M_HEADS), jnp.float32),
                                   f_bias + 0.1 * nrm(ks[7], (DEPTH, M_HEADS), jnp.float32)], axis=-1)
    return {
        'x': x,
        'positions': positions,
        'attn_norm': gain(ks[2], D_MODEL),
        'w_in': nrm(ks[3], (DEPTH, D_MODEL, D_IN), jnp.float32) * D_MODEL ** -0.5,
        'm_conv_w': nrm(ks[4], (DEPTH, CONV_WIDTH, 2 * M_WIDTH), jnp.float32) * CONV_WIDTH ** -0.5,
        'm_conv_b': 0.02 * nrm(ks[5], (DEPTH, 2 * M_WIDTH), jnp.float32),
        'm_gate_bias': m_gate_bias,
        'a_q_norm': gain(ks[8], A_Q_RANK),
        'a_kv_norm': gain(ks[9], A_KV_RANK),
        'a_w_uq': nrm(ks[10], (DEPTH, A_Q_RANK, A_HEADS * A_QK_DIM), jnp.float32) * A_Q_RANK ** -0.5,
        'a_w_ukv': nrm(ks[11], (DEPTH, A_KV_RANK, A_HEADS * (A_NOPE + A_V_DIM)), jnp.float32) * A_KV_RANK ** -0.5,
        'a_q_head_norm': gain(ks[12], A_QK_DIM),
        'a_k_head_norm': gain(ks[13], A_QK_DIM),
        'c_v_norm': gain(ks[14], C_WIDTH),
        'c_w_s': nrm(ks[15], (DEPTH, C_GROUPS, C_CHUNK, C_CHUNK), jnp.float32) * C_CHUNK ** -0.5,
        'c_b_s': 1.0 + 0.1 * nrm(ks[16], (DEPTH, C_GROUPS, C_CHUNK), jnp.float32),
        'mix_out_norm': gain(ks[17], D_MIX),
        'w_out': nrm(ks[18], (DEPTH, D_MIX, D_MODEL), jnp.float32) * (D_MIX ** -0.5) * ((2 * DEPTH) ** -0.5),
        'ffn_norm': gain(ks[19], D_MODEL),
        'w_group': nrm(ks[20], (DEPTH, D_MODEL, N_GROUPS), jnp.float32) * D_MODEL ** -0.5,
        'b_group': 0.01 * nrm(ks[21], (DEPTH, N_GROUPS), jnp.float32),
        'w_expert': nrm(ks[22], (DEPTH, D_MODEL, N_EXPERTS), jnp.float32) * D_MODEL ** -0.5,
        'b_expert': 0.01 * nrm(ks[23], (DEPTH, N_EXPERTS), jnp.float32),
        'w1': nrm(ks[24], (DEPTH, N_EXPERTS, D_MODEL, D_EXPERT), jnp.float32) * D_MODEL ** -0.5,
        'w3': nrm(ks[25], (DEPTH, N_EXPERTS, D_MODEL, D_EXPERT), jnp.float32) * D_MODEL ** -0.5,
        'w2': nrm(ks[26], (DEPTH, N_EXPERTS, D_EXPERT, D_MODEL), jnp.float32) * D_EXPERT ** -0.5 * ((2 * DEPTH) ** -0.5),
    }


def reference(x, positions, attn_norm, w_in, m_conv_w, m_conv_b, m_gate_bias, a_q_norm, a_kv_norm,
              a_w_uq, a_w_ukv, a_q_head_norm, a_k_head_norm, c_v_norm, c_w_s, c_b_s, mix_out_norm,
              w_out, ffn_norm, w_group, b_group, w_expert, b_expert, w1, w3, w2):
    B, S, _ = x.shape
    cos, sin = rope_tables(positions)
    for l in range(DEPTH):
        h = rms_norm(x, attn_norm[l])
        proj = h @ w_in[l]
        mq, mk, mv, mo, mi, mf, cq, ckv, krope, cu, cv = split_cols(proj, IN_SIZES)
        qk = jax.nn.silu(causal_depthwise_conv(jnp.concatenate([mq, mk], axis=-1), m_conv_w[l], m_conv_b[l]))
        mq, mk = qk[..., :M_WIDTH], qk[..., M_WIDTH:]
        gb = m_gate_bias[l]
        hm = mlstm_chunkwise(mq.reshape(B, S, M_HEADS, M_HEAD_DIM),
                             mk.reshape(B, S, M_HEADS, M_HEAD_DIM),
                             mv.reshape(B, S, M_HEADS, M_HEAD_DIM),
                             mi + gb[:M_HEADS], mf + gb[M_HEADS:])
        hm = jax.nn.sigmoid(mo) * hm.reshape(B, S, M_WIDTH)
        ha = mla_mixer(cq, ckv, krope, cos, sin, a_q_norm[l], a_kv_norm[l], a_w_uq[l], a_w_ukv[l],
                       a_q_head_norm[l], a_k_head_norm[l])
        hc = chunk_spatial_gating(jax.nn.gelu(cu), jax.nn.gelu(cv), c_v_norm[l], c_w_s[l], c_b_s[l])
        g = mix_out_norm[l]
        y = jnp.concatenate([head_rms_norm(hm, g[:M_WIDTH], M_HEADS),
                             head_rms_norm(ha, g[M_WIDTH:M_WIDTH + A_WIDTH], A_HEADS),
                             head_rms_norm(hc, g[M_WIDTH + A_WIDTH:], C_GROUPS)], axis=-1)
        x = x + y @ w_out[l]
        x = x + hier_moe(rms_norm(x, ffn_norm[l]), w_group[l], b_group[l], w_expert[l], b_expert[l],
                         w1[l], w3[l], w2[l])
    return x
```

```python
import math
from contextlib import ExitStack
import numpy as np
import concourse.bass as bass
import concourse.mybir as mybir
from concourse.bass_utils import run_bass_kernel_spmd

F32 = mybir.dt.float32
BF16 = mybir.dt.bfloat16
I32 = mybir.dt.int32
AF = mybir.ActivationFunctionType
ALU = mybir.AluOpType
AX = mybir.AxisListType

NCORES = 8
S = 2048
D = 1024
NT = 16
L = 4
DIN = 2472
O_MQ, O_MK, O_MV, O_MO, O_MI, O_MF, O_CQ, O_CKV, O_KR, O_CU, O_CV = (
    0, 384, 768, 1152, 1536, 1540, 1544, 1800, 1928, 1960, 2216)
EPS = 1e-6


class Buf:
    __slots__ = ("name", "w", "r")

    def __init__(self, name):
        self.name = name
        self.w = None
        self.r = []


class Tl:
    def __init__(self, t, name):
        self.t = t
        self.b = Buf(name)

    def __getitem__(self, k):
        return self.t[k]


class MK:
    ENGS = ("pe", "dve", "act", "pool", "sp")

    def __init__(self, nc, stack):
        self.nc = nc
        self.stack = stack
        self.ops = {e: [] for e in self.ENGS}
        self.sems = {}
        self.count = {}
        self.seen = {e: {} for e in self.ENGS}
        for e in self.ENGS:
            self._sem("E_" + e)
        self.nops = 0
        self.block = stack.enter_context(nc.Block())
        self.bfn = {"pe": self.block.tensor, "dve": self.block.vector, "act": self.block.scalar,
                    "pool": self.block.gpsimd, "sp": self.block.sync}

    def _emit(self, e, waits, fn, sem, inc):
        def body(eng):
            for s_, v in waits:
                eng.wait_ge(s_, v)
            if fn is not None:
                fn(eng).then_inc(sem, inc)
        self.bfn[e](body)

    def _sem(self, key):
        if key not in self.sems:
            self.sems[key] = self.stack.enter_context(self.nc.semaphore(key))
            self.count[key] = 0
        return self.sems[key]

    def sb(self, name, shape, dt):
        return Tl(self.stack.enter_context(self.nc.sbuf_tensor("sb_" + name, list(shape), dt)), name)

    def ps(self, name, shape, dt):
        return Tl(self.stack.enter_context(self.nc.psum_tensor(name, list(shape), dt)), name)

    def _waits(self, e, reads, writes):
        need = {}

        def add(ev):
            if ev is None:
                return
            k, v, src = ev
            if src == e and e == "pe":
                return
            if need.get(k, 0) < v:
                need[k] = v
        for b in reads:
            add(b.w)
        for b in writes:
            add(b.w)
            for ev in b.r:
                add(ev)
        out = []
        seen = self.seen[e]
        for k, v in need.items():
            if seen.get(k, 0) < v:
                seen[k] = v
                out.append((self.sems[k], v))
        return out

    def _reg(self, ev, reads, writes):
        for b in reads:
            b.r.append(ev)
        for b in writes:
            b.w = ev
            b.r = []

    def op(self, e, fn, reads=(), writes=()):
        reads = [getattr(x, "b", x) for x in reads]
        writes = [getattr(x, "b", x) for x in writes]
        waits = self._waits(e, reads, writes)
        key = "E_" + e
        self.count[key] += 1
        ev = (key, self.count[key], e)
        self._reg(ev, reads, writes)
        self._emit(e, waits, fn, self.sems[key], 1)
        self.nops += 1

    def dma(self, q, out, in_, reads=(), writes=(), stream=None):
        reads = [getattr(x, "b", x) for x in reads]
        writes = [getattr(x, "b", x) for x in writes]
        waits = self._waits(q, reads, writes)
        key = "D_" + (stream or (writes[0].name if writes else reads[0].name))
        self._sem(key)
        self.count[key] += 16
        ev = (key, self.count[key], "dma")
        self._reg(ev, reads, writes)
        self._emit(q, waits, lambda eng: eng.dma_start(out=out, in_=in_), self.sems[key], 16)
        self.nops += 1

    def barrier(self):
        for e in self.ENGS:
            waits = []
            for k, v in self.count.items():
                if v > 0 and self.seen[e].get(k, 0) < v and k != "E_" + e:
                    self.seen[e][k] = v
                    waits.append((self.sems[k], v))
            self._emit(e, waits, None, None, 0)


def build(n_seq=4, n_layers=4, dbg=False, do_moe=True):
    nc = bass.Bass("TRN2", target_bir_lowering=False)

    def din(name, shape, dt=F32):
        return nc.dram_tensor(name, list(shape), dt, kind="ExternalInput").ap()

    x_d = din("x", [n_seq, S, D])
    pos_d = din("posT", [n_seq, 128, NT], I32)
    w_in_d = din("w_in", [L, D, DIN])
    w_out_d = din("w_out", [L, D, D])
    w_uq_d = din("a_w_uq", [L, 256, 576])
    w_ukv_d = din("a_w_ukv", [L, 128, 768])
    c_ws_d = din("c_w_s", [L, 4, 128, 128])
    w_g_d = din("w_group", [L, D, 4])
    w_e_d = din("w_expert", [L, D, 32])
    w1_d = din("w1", [L, 32, D, 256])
    w3_d = din("w3", [L, 32, D, 256])
    w2_d = din("w2", [L, 32, 256, D])
    pv_d = din("pvec", [L, 128, 64])
    cw_d = din("convw", [L, 96, 40])
    rv_d = din("rvec", [L, 128, 496])
    c_d = din("consts", [128, 528])
    out_d = nc.dram_tensor("out", [n_seq, S, D], F32, kind="ExternalOutput").ap()
    dbg_d = {}
    if dbg:
        dbg_d["y"] = nc.dram_tensor("dbg_y", [S, D], F32, kind="ExternalOutput").ap()
        dbg_d["x1"] = nc.dram_tensor("dbg_x1", [S, D], F32, kind="ExternalOutput").ap()

    st = ExitStack()
    with st:
        mk = MK(nc, st)
        dve = lambda fn, r, w: mk.op("dve", fn, r, w)
        act = lambda fn, r, w: mk.op("act", fn, r, w)
        pe = lambda fn, r, w: mk.op("pe", fn, r, w)
        pool = lambda fn, r, w: mk.op("pool", fn, r, w)

        SZ = {F32: 4, BF16: 2, I32: 4}
        ARENA_F = 47104
        arena_t = st.enter_context(nc.sbuf_tensor("arena", [128, ARENA_F], F32))

        class Arena:
            def __init__(self):
                self.off = 0

            def __call__(self, name, shape, dt):
                free = 1
                for d_ in shape[1:]:
                    free *= d_
                n4 = (free * SZ[dt] + 3) // 4
                assert self.off + n4 <= ARENA_F, (name, self.off, n4)
                v = arena_t[0:shape[0], self.off:self.off + n4]
                self.off += n4
                if dt != F32:
                    v = v.bitcast(dt)
                v = v[:, 0:free]
                if len(shape) > 2:
                    names = "abcd"[:len(shape) - 1]
                    kw = {names[i]: shape[1 + i] for i in range(len(shape) - 1)}
                    v = v.rearrange("p (" + " ".join(names) + ") -> p " + " ".join(names), **kw)
                return Tl(v, name)

        cst = mk.sb("cst", [128, 528], F32)
        identb = mk.sb("identb", [128, 128], BF16)
        mask4b = mk.sb("mask4b", [128, 4, 128], F32)
        sc = mk.sb("sc", [128, 8], F32)
        cossin = mk.sb("cossin", [128, 2, NT, 16], F32)
        pvec = mk.sb("pvec", [128, 64], F32)
        rvec = mk.sb("rvec", [128, 496], F32)
        convw = mk.sb("convw", [96, 40], F32)
        posi = mk.sb("posi", [128, NT], I32)
        posf = mk.sb("posf", [128, NT], F32)
        stt = mk.sb("stt", [128, 8], F32)
        hst = mk.sb("hst", [128, 3, 14], F32)
        g_t = mk.sb("g_t", [128, 8, 4], F32)
        A = Arena()
        xtile = [A(f"xtile{i}", [128, D], F32) for i in range(2)]
        convd = A("convd", [96, 8, 4, 96], BF16)
        win_b = A("win_b", [128, 8, DIN], BF16)
        wout_b = A("wout_b", [128, 8, D], BF16)
        wuq_b = A("wuq_b", [128, 2, 576], BF16)
        wukv_b = A("wukv_b", [128, 768], BF16)
        wsT_b = A("wsT_b", [128, 4, 128], BF16)
        stage = A("stage", [128, DIN], F32)
        stage2 = A("stage2", [128, 1152], F32)
        KT = A("KT", [96, 6, S], BF16)
        Vaug = A("Vaug", [128, NT, 6, 65], BF16)
        junk = A("junk", [128, D], F32)
        hb = A("hb", [128, D], BF16)
        hTt = A("hTt", [128, 8, 128], BF16)
        xqk = A("xqk", [96, 8, 131], BF16)
        esil = A("esil", [96, 8, 128], F32)
        qkT = A("qkT", [96, 8, 128], BF16)
        Sm = A("Sm", [128, 4, 128], BF16)
        ktok = A("ktok", [128, 4, 96], BF16)
        Vt = A("Vt", [128, 4, 97], BF16)
        Cf = A("Cf", [96, 4, 97], F32)
        Cb = A("Cb", [96, 4, 97], BF16)
        og = A("og", [128, 384], F32)
        hm = A("hm", [128, 4, 96], F32)
        cT = A("cT", [128, 3, 128], BF16)
        qs = A("qs", [128, 6, 96], F32)
        ksv = A("ksv", [128, 6, 128], F32)
        kr = A("kr", [128, 6, 32], F32)
        krope = A("krope", [128, 32], F32)
        rt = A("rt", [128, 4, 6, 16], F32)
        qb = A("qb", [128, 6, 96], BF16)
        kb = A("kb", [128, 6, 96], BF16)
        QT = A("QT", [96, 6, 128], BF16)
        PT = [A(f"PT{i}", [128, 4, 128], BF16) for i in range(2)]
        ha = A("ha", [128, 6, 64], F32)
        ga = A("ga", [128, 512], F32)
        gb_ = A("gb_", [128, 512], F32)
        guv = A("guv", [128, 512], F32)
        vn = A("vn", [128, 256], BF16)
        hc = A("hc", [128, 256], F32)
        ycat = A("ycat", [128, D], BF16)
        yT = A("yT", [128, 8, 128], BF16)
        mixer_bytes = A.off * 4
        junk_mix = junk
        A = Arena()
        xres = [A(f"xres{n}", [128, D], F32) for n in range(NT)]
        xnT = A("xnT", [128, 8, S], BF16)
        xnf = A("xnf", [128, D], F32)
        xnTf = A("xnTf", [128, 8, 128], F32)
        wr = A("wr", [128, 8, 36], F32)
        rl = A("rl", [128, NT, 36], F32)
        rtmp = A("rtmp", [128, 12, NT, 8], F32)
        t32t = A("t32t", [128, NT, 32], F32)
        ropet = A("ropet", [128, 7, NT, 16], F32)
        gate = A("gate", [128, NT, 32], F32)
        wst = [A(f"wst{j}", [128, 2048], F32) for j in range(3)]
        web = [[A(f"web{i}_{j}", [128, 2048], BF16) for j in range(3)] for i in range(2)]
        s1 = [A(f"s1_{i}", [128, 512], BF16) for i in range(2)]
        hh = [A(f"hh{i}", [128, 2, 512], BF16) for i in range(2)]
        mjunk = A("mjunk", [128, D], F32)
        moe_bytes = A.off * 4
        xs_d = nc.dram_tensor("xs_scratch", [S, D], F32, kind="Internal").ap()
        xsb = [Buf(f"xs{n}") for n in range(NT)]

        PS = [mk.ps(f"ps{i}", [128, 512], F32) for i in range(4)]
        PP = [mk.ps(f"pp{i}", [128, 1024], F32) for i in range(2)]
        ctr = {"s": 0, "p": 0}

        def ps1(exclude=None):
            ctr["s"] += 1
            if PS[ctr["s"] % 4] is exclude:
                ctr["s"] += 1
            return PS[ctr["s"] % 4]

        def ps2():
            ctr["p"] += 1
            return PP[ctr["p"] % 2]

        def v3(ap, a):
            return ap.rearrange("p (a b) -> p a b", a=a)

        mk.dma("sp", cst[:], c_d, writes=[cst])
        ident = cst[:, 0:128]
        maskle = cst[:, 128:256]
        maskge = cst[:, 256:384]
        ones = cst[:, 384:512]
        invf = cst[:, 512:528]
        dve(lambda e: e.tensor_copy(out=identb[:], in_=ident), [cst], [identb])
        for h in range(4):
            dve(lambda e, h=h: e.tensor_copy(out=mask4b[:, h, :], in_=maskle), [cst], [mask4b])
        pool(lambda e: e.memset(sc[:, 0:1], EPS), [], [sc])
        pool(lambda e: e.memset(sc[:, 1:2], 1.0), [], [sc])
        pool(lambda e: e.memset(sc[:, 2:3], EPS / 4), [], [sc])
        pool(lambda e: e.memset(sc[:, 3:4], 0.0), [], [sc])
        c_eps, c_one, c_eps4 = sc[:, 0:1], sc[:, 1:2], sc[:, 2:3]

        def rsqrt(out, in_, scale, bias, r, w):
            act(lambda e: e.activation(out=out, in_=in_, func=AF.Ln, scale=scale, bias=bias), r + [sc], w)
            act(lambda e: e.activation(out=out, in_=out, func=AF.Exp, scale=-0.5), w, w)

        for sq in range(n_seq):
            mk.barrier()
            mk.dma("sp", posi[:], pos_d[sq], writes=[posi])
            dve(lambda e: e.tensor_copy(out=posf[:], in_=posi[:]), [posi], [posf])
            ang = ropet[:, 0, :, :]
            dve(lambda e: e.tensor_tensor(out=ang, in0=posf[:, :, None].broadcast_to([128, NT, 16]),
                                          in1=invf[:, None, :].broadcast_to([128, NT, 16]), op=ALU.mult),
                [posf, cst], [ropet])
            for ci, shift in ((0, math.pi / 2), (1, 0.0)):
                a2 = ropet[:, 1, :, :]
                ki = ropet[:, 2, :, :]
                kf = ropet[:, 3, :, :]
                m1 = ropet[:, 4, :, :]
                RP = [ropet]
                dve(lambda e: e.tensor_scalar(out=a2, in0=ang, scalar1=shift, scalar2=None, op0=ALU.add), RP, RP)
                dve(lambda e: e.tensor_scalar(out=ki.bitcast(I32), in0=a2, scalar1=1.0 / (2 * math.pi), scalar2=None,
                                              op0=ALU.mult), RP, RP)
                dve(lambda e: e.tensor_copy(out=kf, in_=ki.bitcast(I32)), RP, RP)
                dve(lambda e: e.scalar_tensor_tensor(out=a2, in0=kf, scalar=-2 * math.pi, in1=a2, op0=ALU.mult,
                                                     op1=ALU.add), RP, RP)
                dve(lambda e: e.tensor_scalar(out=m1, in0=a2, scalar1=math.pi, scalar2=-2 * math.pi, op0=ALU.is_gt,
                                              op1=ALU.mult), RP, RP)
                dve(lambda e: e.tensor_tensor(out=a2, in0=a2, in1=m1, op=ALU.add), RP, RP)
                dve(lambda e: e.tensor_scalar(out=m1, in0=a2, scalar1=-math.pi, scalar2=2 * math.pi, op0=ALU.is_lt,
                                              op1=ALU.mult), RP, RP)
                dve(lambda e: e.tensor_tensor(out=a2, in0=a2, in1=m1, op=ALU.add), RP, RP)
                act(lambda e: e.activation(out=cossin[:, ci, :, :], in_=a2, func=AF.Sin), RP, [cossin])

            for l in range(n_layers):
                mk.barrier()
                junk = junk_mix
                pool(lambda e: e.memset(Vaug[:], 1.0), [], [Vaug])
                mk.dma("sp", pvec[:], pv_d[l], writes=[pvec])
                mk.dma("sp", rvec[:], rv_d[l], writes=[rvec])
                mk.dma("sp", convw[:], cw_d[l], writes=[convw])
                for kc in range(8):
                    mk.dma("sp", stage[:], w_in_d[l, kc * 128:(kc + 1) * 128, :], writes=[stage])
                    pool(lambda e, kc=kc: e.tensor_scalar(out=win_b[:, kc, :], in0=stage[:], scalar1=pvec[:, kc:kc + 1],
                                                          scalar2=None, op0=ALU.mult), [stage, pvec], [win_b])
                for kc in range(8):
                    mk.dma("sp", stage2[:, 0:D], w_out_d[l, kc * 128:(kc + 1) * 128, :], writes=[stage2])
                    pool(lambda e, kc=kc: e.tensor_scalar(out=wout_b[:, kc, :], in0=stage2[:, 0:D],
                                                          scalar1=pvec[:, 8 + kc:9 + kc], scalar2=None, op0=ALU.mult),
                         [stage2, pvec], [wout_b])
                for c in range(2):
                    mk.dma("sp", stage2[:, 0:576], w_uq_d[l, c * 128:(c + 1) * 128, :], writes=[stage2])
                    pool(lambda e, c=c: e.tensor_scalar(out=wuq_b[:, c, :], in0=stage2[:, 0:576],
                                                        scalar1=pvec[:, 24 + c:25 + c], scalar2=None, op0=ALU.mult),
                         [stage2, pvec], [wuq_b])
                mk.dma("sp", stage2[:, 0:768], w_ukv_d[l], writes=[stage2])
                pool(lambda e: e.tensor_scalar(out=wukv_b[:], in0=stage2[:, 0:768], scalar1=pvec[:, 26:27],
                                               scalar2=None, op0=ALU.mult), [stage2, pvec], [wukv_b])
                mk.dma("sp", stage2[:, 0:512].rearrange("p (g s) -> p g s", g=4),
                       c_ws_d[l].rearrange("g t s -> t g s"), writes=[stage2])
                pool(lambda e: e.tensor_tensor(out=hb[:, 0:512].rearrange("p (g s) -> p g s", g=4),
                                               in0=stage2[:, 0:512].rearrange("p (g s) -> p g s", g=4),
                                               in1=maskge[:, None, :].broadcast_to([128, 4, 128]), op=ALU.mult),
                     [stage2, cst], [hb])
                pw = ps1()
                pwb = pw[:].bitcast(BF16)
                for g in range(4):
                    pe(lambda e, g=g: e.transpose(out=pwb[:, g * 128:(g + 1) * 128], in_=hb[:, g * 128:(g + 1) * 128],
                                                  identity=identb[:]), [hb, identb], [pw])
                dve(lambda e: e.tensor_copy(out=wsT_b[:].rearrange("p g t -> p (g t)"), in_=pwb[:, 0:512]), [pw], [wsT_b])
                for ch in range(8):
                    for j in range(4):
                        pool(lambda e, ch=ch, j=j: e.tensor_scalar(out=convd[:, ch, j, :], in0=cst[0:96, 0:96],
                                                                  scalar1=convw[:, ch * 4 + j:ch * 4 + j + 1],
                                                                  scalar2=None, op0=ALU.mult), [cst, convw], [convd])
                pool(lambda e: e.tensor_scalar(out=rvec[:, 0:4], in0=rvec[:, 0:4], scalar1=-0.5 * math.log(96.0),
                                               scalar2=None, op0=ALU.add), [rvec], [rvec])
                pool(lambda e: e.memset(Cf[:], 0.0), [], [Cf])
                pool(lambda e: e.memset(Cb[:], 0.0), [], [Cb])
                pool(lambda e: e.memset(xqk[:], 0.0), [], [xqk])
                gbias = rvec[:, 0:8]
                qhg = rvec[:, 8:104]
                khg = rvec[:, 104:200]
                cvg = rvec[:, 200:456]
                bgb = rvec[:, 456:460]
                beb = rvec[:, 460:492]
                bsb = pvec[:, 27:31]
                convb = convw[:, 32:40]

                for n in range(NT):
                    xt = xtile[n % 2]
                    tsl = slice(n * 128, (n + 1) * 128)
                    if l == 0:
                        mk.dma("sp", xt[:], x_d[sq, tsl, :], writes=[xt])
                    else:
                        mk.dma("sp", xt[:], xs_d[tsl, :], reads=[xsb[n]], writes=[xt])
                    act(lambda e: e.activation(out=junk[:], in_=xt[:], func=AF.Square, accum_out=stt[:, 0:1]),
                        [xt], [junk, stt])
                    rsqrt(stt[:, 1:2], stt[:, 0:1], 1.0 / D, c_eps, [stt], [stt])
                    dve(lambda e: e.tensor_scalar(out=hb[:], in0=xt[:], scalar1=stt[:, 1:2], scalar2=None, op0=ALU.mult),
                        [xt, stt], [hb])
                    p0 = ps1()
                    p0b = p0[:].bitcast(BF16)
                    for k in range(8):
                        pe(lambda e, k=k: e.transpose(out=p0b[:, k * 128:(k + 1) * 128], in_=hb[:, k * 128:(k + 1) * 128],
                                                      identity=identb[:]), [hb, identb], [p0])
                    act(lambda e: e.copy(out=hTt[:].rearrange("p k t -> p (k t)"), in_=p0b[:, 0:1024]), [p0], [hTt])

                    def proj_tok(c0, c1):
                        p = ps1()
                        for k in range(8):
                            pe(lambda e, k=k: e.matmul(p[:, 0:c1 - c0], lhsT=hTt[:, k, :], rhs=win_b[:, k, c0:c1],
                                                       start=(k == 0), stop=(k == 7)), [hTt, win_b], [p])
                        return p
                    pT2 = proj_tok(O_MO, O_MO + 392)
                    ipre, lf, einvb, ks, a_t, rec = (g_t[:, i, :] for i in range(6))
                    dve(lambda e: e.tensor_tensor(out=ipre, in0=pT2[:, 384:388], in1=gbias[:, 0:4], op=ALU.add),
                        [pT2, rvec], [g_t])
                    dve(lambda e: e.tensor_tensor(out=lf, in0=pT2[:, 388:392], in1=gbias[:, 4:8], op=ALU.add),
                        [pT2, rvec], [g_t])
                    act(lambda e: e.activation(out=lf, in_=lf, func=AF.Exp, scale=-1.0), [g_t], [g_t])
                    act(lambda e: e.activation(out=lf, in_=lf, func=AF.Ln, bias=c_one), [g_t, sc], [g_t])
                    pg = ps1()
                    pe(lambda e: e.matmul(pg[:, 0:4], lhsT=maskle, rhs=lf, start=True, stop=True), [cst, g_t], [pg])
                    pe(lambda e: e.matmul(pg[:, 4:8], lhsT=ones, rhs=lf, start=True, stop=True), [cst, g_t], [pg])
                    act(lambda e: e.activation(out=einvb, in_=pg[:, 0:4], func=AF.Exp), [pg], [g_t])
                    dve(lambda e: e.tensor_tensor(out=ks, in0=ipre, in1=pg[:, 0:4], op=ALU.add), [pg, g_t], [g_t])
                    act(lambda e: e.activation(out=ks, in_=ks, func=AF.Exp), [g_t], [g_t])
                    act(lambda e: e.activation(out=a_t, in_=pg[:, 4:8], func=AF.Exp, scale=-1.0), [pg], [g_t])
                    act(lambda e: e.activation(out=og[:], in_=pT2[:, 0:384], func=AF.Exp, scale=-1.0), [pT2], [og])
                    dve(lambda e: e.tensor_scalar(out=og[:], in0=og[:], scalar1=1.0, scalar2=None, op0=ALU.add), [og], [og])
                    dve(lambda e: e.reciprocal(out=og[:], in_=og[:]), [og], [og])
                    pT1 = proj_tok(O_MV, O_MV + 384)
                    for h in range(4):
                        dve(lambda e, h=h: e.tensor_scalar(out=Vt[:, h, 0:96], in0=pT1[:, h * 96:(h + 1) * 96],
                                                           scalar1=ks[:, h:h + 1], scalar2=None, op0=ALU.mult),
                            [pT1, g_t], [Vt])
                    dve(lambda e: e.tensor_copy(out=Vt[:, :, 96], in_=ks), [g_t], [Vt])
                    pq = ps2()
                    for ch in range(8):
                        for k in range(8):
                            pe(lambda e, ch=ch, k=k: e.matmul(pq[0:96, ch * 128:(ch + 1) * 128],
                                                              lhsT=win_b[:, k, ch * 96:(ch + 1) * 96], rhs=hTt[:, k, :],
                                                              start=(k == 0), stop=(k == 7)), [hTt, win_b], [pq])
                    act(lambda e: e.copy(out=xqk[:, :, 3:131], in_=v3(pq[0:96, :], 8)), [pq], [xqk])
                    pc = ps2()
                    for ch in range(8):
                        for j in range(4):
                            pe(lambda e, ch=ch, j=j: e.matmul(pc[0:96, ch * 128:(ch + 1) * 128], lhsT=convd[:, ch, j, :],
                                                              rhs=xqk[:, ch, j:j + 128], start=(j == 0), stop=(j == 3)),
                               [convd, xqk], [pc])
                    for ch in range(8):
                        act(lambda e, ch=ch: e.activation(out=esil[:, ch, :], in_=pc[0:96, ch * 128:(ch + 1) * 128],
                                                          func=AF.Identity, bias=convb[:, ch:ch + 1]),
                            [pc, convw], [esil])
                    act(lambda e: e.activation(out=junk[0:96, :], in_=esil[:].rearrange("p a b -> p (a b)"),
                                               func=AF.Exp, scale=-1.0), [esil], [junk])
                    dve(lambda e: e.tensor_scalar(out=junk[0:96, :], in0=junk[0:96, :], scalar1=1.0, scalar2=None,
                                                  op0=ALU.add), [junk], [junk])
                    dve(lambda e: e.reciprocal(out=junk[0:96, :], in_=junk[0:96, :]), [junk], [junk])
                    dve(lambda e: e.tensor_tensor(out=qkT[:].rearrange("p a b -> p (a b)"),
                                                  in0=esil[:].rearrange("p a b -> p (a b)"), in1=junk[0:96, :],
                                                  op=ALU.mult), [esil, junk], [qkT])
                    pool(lambda e: e.tensor_copy(out=xqk[:, :, 0:3], in_=xqk[:, :, 128:131]), [xqk], [xqk])

                    pS = ps1()
                    for h in range(4):
                        pe(lambda e, h=h: e.matmul(pS[:, h * 128:(h + 1) * 128], lhsT=qkT[:, 4 + h, :], rhs=qkT[:, h, :],
                                                   start=True, stop=True), [qkT], [pS])
                    dve(lambda e: e.tensor_tensor(out=Sm[:], in0=v3(pS[:], 4), in1=mask4b[:], op=ALU.mult),
                        [pS, mask4b], [Sm])
                    pk = ps1()
                    pkb = pk[:].bitcast(BF16)
                    for h in range(4):
                        pe(lambda e, h=h: e.transpose(out=pkb[:, h * 96:(h + 1) * 96], in_=qkT[:, 4 + h, :],
                                                      identity=identb[0:96, 0:96]), [qkT, identb], [pk])
                    act(lambda e: e.copy(out=ktok[:].rearrange("p a b -> p (a b)"), in_=pkb[:, 0:384]), [pk], [ktok])
                    pN = ps1()
                    for h in range(4):
                        pe(lambda e, h=h: e.matmul(pN[:, h * 97:(h + 1) * 97], lhsT=Sm[:, h, :], rhs=Vt[:, h, :],
                                                   start=True, stop=False), [Sm, Vt], [pN])
                        pe(lambda e, h=h: e.matmul(pN[:, h * 97:(h + 1) * 97], lhsT=qkT[:, h, :], rhs=Cb[:, h, :],
                                                   start=False, stop=True), [qkT, Cb], [pN])
                    pD = ps1()
                    for h in range(4):
                        pe(lambda e, h=h: e.matmul(pD[0:96, h * 97:(h + 1) * 97], lhsT=ktok[:, h, :], rhs=Vt[:, h, :],
                                                   start=True, stop=True), [ktok, Vt], [pD])
                    dve(lambda e: e.tensor_tensor(out=Cf[:].rearrange("p a b -> p (a b)"),
                                                  in0=Cf[:].rearrange("p a b -> p (a b)"), in1=pD[0:96, 0:388],
                                                  op=ALU.add), [Cf, pD], [Cf])
                    for h in range(4):
                        dve(lambda e, h=h: e.tensor_scalar(out=Cf[:, h, :], in0=Cf[:, h, :], scalar1=a_t[0:96, h:h + 1],
                                                           scalar2=None, op0=ALU.mult), [Cf, g_t], [Cf])
                    act(lambda e: e.copy(out=Cb[:], in_=Cf[:]), [Cf], [Cb])
                    pNv = pN[:, 0:388].rearrange("p (a b) -> p a b", a=4)
                    act(lambda e: e.activation(out=rec, in_=pNv[:, :, 96], func=AF.Abs), [pN], [g_t])
                    dve(lambda e: e.tensor_tensor(out=rec, in0=rec, in1=einvb, op=ALU.max), [g_t], [g_t])
                    dve(lambda e: e.reciprocal(out=rec, in_=rec), [g_t], [g_t])
                    dve(lambda e: e.tensor_tensor(out=hm[:], in0=pNv[:, :, 0:96],
                                                  in1=rec[:, :, None].broadcast_to([128, 4, 96]), op=ALU.mult),
                        [pN, g_t], [hm])
                    dve(lambda e: e.tensor_tensor(out=hm[:].rearrange("p a b -> p (a b)"),
                                                  in0=hm[:].rearrange("p a b -> p (a b)"), in1=og[:], op=ALU.mult),
                        [hm, og], [hm])

                    pT3 = proj_tok(O_CQ, O_CQ + 416)
                    act(lambda e: e.activation(out=junk[:, 0:256], in_=pT3[:, 0:256], func=AF.Square,
                                               accum_out=stt[:, 2:3]), [pT3], [junk, stt])
                    act(lambda e: e.activation(out=junk[:, 256:384], in_=pT3[:, 256:384], func=AF.Square,
                                               accum_out=stt[:, 3:4]), [pT3], [junk, stt])
                    act(lambda e: e.copy(out=krope[:], in_=pT3[:, 384:416]), [pT3], [krope])
                    act(lambda e: e.activation(out=junk[:, 384:416], in_=pT3[:, 384:416], func=AF.Square,
                                               accum_out=stt[:, 6:7]), [pT3], [junk, stt])
                    rsqrt(stt[:, 4:5], stt[:, 2:3], 1.0 / 256, c_eps, [stt], [stt])
                    rsqrt(stt[:, 5:6], stt[:, 3:4], 1.0 / 128, c_eps, [stt], [stt])
                    pC = ps1()
                    for c in range(3):
                        for k in range(8):
                            pe(lambda e, c=c, k=k: e.matmul(pC[:, c * 128:(c + 1) * 128],
                                                            lhsT=win_b[:, k, O_CQ + c * 128:O_CQ + (c + 1) * 128],
                                                            rhs=hTt[:, k, :], start=(k == 0), stop=(k == 7)),
                               [hTt, win_b], [pC])
                    act(lambda e: e.copy(out=cT[:].rearrange("p a b -> p (a b)"), in_=pC[:, 0:384]), [pC], [cT])
                    pQ = ps2()
                    for j in range(2):
                        for c in range(2):
                            pe(lambda e, j=j, c=c: e.matmul(pQ[:, j * 512:j * 512 + 288], lhsT=cT[:, c, :],
                                                            rhs=wuq_b[:, c, j * 288:(j + 1) * 288],
                                                            start=(c == 0), stop=(c == 1)), [cT, wuq_b], [pQ])
                    pK = ps2()
                    for j in range(2):
                        pe(lambda e, j=j: e.matmul(pK[:, j * 512:j * 512 + 384], lhsT=cT[:, 2, :],
                                                   rhs=wukv_b[:, j * 384:(j + 1) * 384], start=True, stop=True),
                           [cT, wukv_b], [pK])
                    for j in range(2):
                        dve(lambda e, j=j: e.tensor_scalar(out=qs[:, 3 * j:3 * j + 3, :].rearrange("p a b -> p (a b)"),
                                                           in0=pQ[:, j * 512:j * 512 + 288], scalar1=stt[:, 4:5],
                                                           scalar2=None, op0=ALU.mult), [pQ, stt], [qs])
                        dve(lambda e, j=j: e.tensor_scalar(out=ksv[:, 3 * j:3 * j + 3, :].rearrange("p a b -> p (a b)"),
                                                           in0=pK[:, j * 512:j * 512 + 384], scalar1=stt[:, 5:6],
                                                           scalar2=None, op0=ALU.mult), [pK, stt], [ksv])
                    jq = junk[:, 0:576].rearrange("p (a b) -> p a b", a=6)
                    act(lambda e: e.activation(out=jq, in_=qs[:], func=AF.Square), [qs], [junk])
                    dve(lambda e: e.reduce_sum(out=hst[:, 0, 0:6], in_=jq, axis=AX.X), [junk], [hst])
                    rsqrt(hst[:, 1, 0:6], hst[:, 0, 0:6], 1.0 / 96, c_eps, [hst], [hst])
                    dve(lambda e: e.tensor_tensor(out=qs[:], in0=qs[:], in1=hst[:, 1, 0:6, None].broadcast_to([128, 6, 96]),
                                                  op=ALU.mult), [qs, hst], [qs])
                    dve(lambda e: e.tensor_tensor(out=qs[:], in0=qs[:], in1=qhg[:, None, :].broadcast_to([128, 6, 96]),
                                                  op=ALU.mult), [qs, rvec], [qs])
                    jk = junk[:, 0:384].rearrange("p (a b) -> p a b", a=6)
                    act(lambda e: e.activation(out=jk, in_=ksv[:, :, 0:64], func=AF.Square), [ksv], [junk])
                    dve(lambda e: e.reduce_sum(out=hst[:, 0, 6:12], in_=jk, axis=AX.X), [junk], [hst])
                    dve(lambda e: e.tensor_scalar(out=hst[:, 0, 6:12], in0=hst[:, 0, 6:12], scalar1=stt[:, 6:7],
                                                  scalar2=None, op0=ALU.add), [hst, stt], [hst])
                    rsqrt(hst[:, 1, 6:12], hst[:, 0, 6:12], 1.0 / 96, c_eps, [hst], [hst])
                    rk = hst[:, 1, 6:12]
                    dve(lambda e: e.tensor_tensor(out=jk, in0=ksv[:, :, 0:64], in1=rk[:, :, None].broadcast_to([128, 6, 64]),
                                                  op=ALU.mult), [ksv, hst], [junk])
                    dve(lambda e: e.tensor_tensor(out=kb[:, :, 0:64], in0=jk,
                                                  in1=khg[:, None, 0:64].broadcast_to([128, 6, 64]), op=ALU.mult),
                        [junk, rvec], [kb])
                    dve(lambda e: e.tensor_tensor(out=kr[:], in0=krope[:, None, :].broadcast_to([128, 6, 32]),
                                                  in1=rk[:, :, None].broadcast_to([128, 6, 32]), op=ALU.mult),
                        [krope, hst], [kr])
                    dve(lambda e: e.tensor_tensor(out=kr[:], in0=kr[:], in1=khg[:, None, 64:96].broadcast_to([128, 6, 32]),
                                                  op=ALU.mult), [kr, rvec], [kr])
                    cosb = cossin[:, 0, n, None, :].broadcast_to([128, 6, 16])
                    sinb = cossin[:, 1, n, None, :].broadcast_to([128, 6, 16])

                    def rope(src1, src2, dst1, dst2, rb, wb):
                        t = [rt[:, i, :, :] for i in range(4)]
                        dve(lambda e: e.tensor_tensor(out=t[0], in0=src1, in1=cosb, op=ALU.mult), rb + [cossin], [rt])
                        dve(lambda e: e.tensor_tensor(out=t[1], in0=src2, in1=sinb, op=ALU.mult), rb + [cossin], [rt])
                        dve(lambda e: e.tensor_tensor(out=t[2], in0=src1, in1=sinb, op=ALU.mult), rb + [cossin], [rt])
                        dve(lambda e: e.tensor_tensor(out=t[3], in0=src2, in1=cosb, op=ALU.mult), rb + [cossin], [rt])
                        dve(lambda e: e.tensor_tensor(out=dst1, in0=t[0], in1=t[1], op=ALU.subtract), [rt], wb)
                        dve(lambda e: e.tensor_tensor(out=dst2, in0=t[2], in1=t[3], op=ALU.add), [rt], wb)
                    rope(qs[:, :, 64:80], qs[:, :, 80:96], qb[:, :, 64:80], qb[:, :, 80:96], [qs], [qb])
                    dve(lambda e: e.tensor_copy(out=qb[:, :, 0:64], in_=qs[:, :, 0:64]), [qs], [qb])
                    rope(kr[:, :, 0:16], kr[:, :, 16:32], kb[:, :, 64:80], kb[:, :, 80:96], [kr], [kb])
                    act(lambda e: e.copy(out=Vaug[:, n, :, 0:64], in_=ksv[:, :, 64:128]), [ksv], [Vaug])
                    pqt = ps1()
                    pqtb = pqt[:].bitcast(BF16)
                    for h in range(6):
                        pe(lambda e, h=h: e.transpose(out=pqtb[0:96, h * 128:(h + 1) * 128], in_=qb[:, h, :],
                                                      identity=identb[:]), [qb, identb], [pqt])
                    act(lambda e: e.copy(out=QT[:].rearrange("p a b -> p (a b)"), in_=pqtb[0:96, 0:768]), [pqt], [QT])
                    pkt = ps1()
                    pktb = pkt[:].bitcast(BF16)
                    for h in range(6):
                        pe(lambda e, h=h: e.transpose(out=pktb[0:96, h * 128:(h + 1) * 128], in_=kb[:, h, :],
                                                      identity=identb[:]), [kb, identb], [pkt])
                    act(lambda e: e.copy(out=KT[:, :, tsl], in_=pktb[0:96, 0:768].rearrange("p (a b) -> p a b", a=6)),
                        [pkt], [KT])
                    pO = ps1()
                    cnt = 0
                    for h in range(6):
                        for j0 in range(0, n + 1, 4):
                            nb = min(4, n + 1 - j0)
                            pa = ps1(exclude=pO)
                            for jj in range(nb):
                                j = j0 + jj
                                pe(lambda e, h=h, j=j, jj=jj: e.matmul(pa[:, jj * 128:(jj + 1) * 128],
                                                                       lhsT=KT[:, h, j * 128:(j + 1) * 128], rhs=QT[:, h, :],
                                                                       start=True, stop=True), [KT, QT], [pa])
                            pt = PT[cnt % 2]
                            cnt += 1
                            act(lambda e, nb=nb, pt=pt, pa=pa: e.activation(
                                out=pt[:, 0:nb, :].rearrange("p a b -> p (a b)"), in_=pa[:, 0:nb * 128], func=AF.Exp,
                                scale=96.0 ** -0.5), [pa], [pt])
                            if j0 + nb == n + 1:
                                pool(lambda e, nb=nb, pt=pt: e.tensor_tensor(out=pt[:, nb - 1, :], in0=pt[:, nb - 1, :],
                                                                            in1=maskle, op=ALU.mult), [pt, cst], [pt])
                            for jj in range(nb):
                                j = j0 + jj
                                pe(lambda e, h=h, j=j, jj=jj, pt=pt: e.matmul(pO[:, h * 65:(h + 1) * 65], lhsT=pt[:, jj, :],
                                                                              rhs=Vaug[:, j, h, :], start=(j == 0),
                                                                              stop=(j == n)), [pt, Vaug], [pO])
                    pOv = pO[:, 0:390].rearrange("p (a b) -> p a b", a=6)
                    dve(lambda e: e.reciprocal(out=hst[:, 2, 0:6], in_=pOv[:, :, 64]), [pO], [hst])
                    dve(lambda e: e.tensor_tensor(out=ha[:], in0=pOv[:, :, 0:64],
                                                  in1=hst[:, 2, 0:6, None].broadcast_to([128, 6, 64]), op=ALU.mult),
                        [pO, hst], [ha])

                    pT4 = proj_tok(O_CU, O_CU + 512)
                    act(lambda e: e.activation(out=ga[:], in_=pT4[:], func=AF.Square), [pT4], [ga])
                    dve(lambda e: e.tensor_scalar(out=ga[:], in0=ga[:], scalar1=0.044715, scalar2=1.0, op0=ALU.mult,
                                                  op1=ALU.add), [ga], [ga])
                    dve(lambda e: e.tensor_tensor(out=ga[:], in0=ga[:], in1=pT4[:], op=ALU.mult), [ga, pT4], [ga])
                    act(lambda e: e.activation(out=gb_[:], in_=ga[:], func=AF.Exp, scale=-2.0 * math.sqrt(2.0 / math.pi)),
                        [ga], [gb_])
                    dve(lambda e: e.tensor_scalar(out=gb_[:], in0=gb_[:], scalar1=1.0, scalar2=None, op0=ALU.add),
                        [gb_], [gb_])
                    dve(lambda e: e.reciprocal(out=gb_[:], in_=gb_[:]), [gb_], [gb_])
                    dve(lambda e: e.tensor_tensor(out=guv[:], in0=gb_[:], in1=pT4[:], op=ALU.mult), [gb_, pT4], [guv])
                    gv = guv[:, 256:512].rearrange("p (a b) -> p a b", a=4)
                    jv = junk[:, 0:256].rearrange("p (a b) -> p a b", a=4)
                    act(lambda e: e.activation(out=jv, in_=gv, func=AF.Square), [guv], [junk])
                    dve(lambda e: e.reduce_sum(out=g_t[:, 6, :], in_=jv, axis=AX.X), [junk], [g_t])
                    rsqrt(g_t[:, 7, :], g_t[:, 6, :], 1.0 / 64, c_eps, [g_t], [g_t])
                    dve(lambda e: e.tensor_tensor(out=vn[:].rearrange("p (a b) -> p a b", a=4), in0=gv,
                                                  in1=g_t[:, 7, :, None].broadcast_to([128, 4, 64]), op=ALU.mult),
                        [guv, g_t], [vn])
                    pM = ps1()
                    for g in range(4):
                        pe(lambda e, g=g: e.matmul(pM[:, g * 64:(g + 1) * 64], lhsT=wsT_b[:, g, :],
                                                   rhs=vn[:, g * 64:(g + 1) * 64], start=True, stop=True), [wsT_b, vn], [pM])
                    dve(lambda e: e.tensor_tensor(out=hc[:], in0=pM[:, 0:256], in1=cvg, op=ALU.mult), [pM, rvec], [hc])
                    hcv = hc[:].rearrange("p (a b) -> p a b", a=4)
                    dve(lambda e: e.tensor_tensor(out=hcv, in0=hcv, in1=bsb[:, :, None].broadcast_to([128, 4, 64]),
                                                  op=ALU.add), [hc, pvec], [hc])
                    dve(lambda e: e.tensor_tensor(out=hc[:], in0=hc[:], in1=guv[:, 0:256], op=ALU.mult), [hc, guv], [hc])

                    jm = junk[:, 0:384].rearrange("p (a b) -> p a b", a=4)
                    act(lambda e: e.activation(out=jm, in_=hm[:], func=AF.Square), [hm], [junk])
                    dve(lambda e: e.reduce_sum(out=hst[:, 0, 0:4], in_=jm, axis=AX.X), [junk], [hst])
                    ja = junk[:, 384:768].rearrange("p (a b) -> p a b", a=6)
                    act(lambda e: e.activation(out=ja, in_=ha[:], func=AF.Square), [ha], [junk])
                    dve(lambda e: e.reduce_sum(out=hst[:, 0, 4:10], in_=ja, axis=AX.X), [junk], [hst])
                    jc = junk[:, 768:1024].rearrange("p (a b) -> p a b", a=4)
                    act(lambda e: e.activation(out=jc, in_=hcv, func=AF.Square), [hc], [junk])
                    dve(lambda e: e.reduce_sum(out=hst[:, 0, 10:14], in_=jc, axis=AX.X), [junk], [hst])
                    rsqrt(hst[:, 1, 0:4], hst[:, 0, 0:4], 1.0 / 96, c_eps, [hst], [hst])
                    rsqrt(hst[:, 1, 4:14], hst[:, 0, 4:14], 1.0 / 64, c_eps, [hst], [hst])
                    dve(lambda e: e.tensor_tensor(out=ycat[:, 0:384].rearrange("p (a b) -> p a b", a=4), in0=hm[:],
                                                  in1=hst[:, 1, 0:4, None].broadcast_to([128, 4, 96]), op=ALU.mult),
                        [hm, hst], [ycat])
                    dve(lambda e: e.tensor_tensor(out=ycat[:, 384:768].rearrange("p (a b) -> p a b", a=6), in0=ha[:],
                                                  in1=hst[:, 1, 4:10, None].broadcast_to([128, 6, 64]), op=ALU.mult),
                        [ha, hst], [ycat])
                    dve(lambda e: e.tensor_tensor(out=ycat[:, 768:1024].rearrange("p (a b) -> p a b", a=4), in0=hcv,
                                                  in1=hst[:, 1, 10:14, None].broadcast_to([128, 4, 64]), op=ALU.mult),
                        [hc, hst], [ycat])
                    if dbg and sq == 0 and l == 0:
                        dve(lambda e: e.tensor_copy(out=junk[:], in_=ycat[:]), [ycat], [junk])
                        mk.dma("sp", dbg_d["y"][tsl, :], junk[:], reads=[junk], writes=[Buf("dbgy")], stream="dbg")
                    py = ps1()
                    pyb = py[:].bitcast(BF16)
                    for k in range(8):
                        pe(lambda e, k=k: e.transpose(out=pyb[:, k * 128:(k + 1) * 128], in_=ycat[:, k * 128:(k + 1) * 128],
                                                      identity=identb[:]), [ycat, identb], [py])
                    act(lambda e: e.copy(out=yT[:].rearrange("p a b -> p (a b)"), in_=pyb[:, 0:1024]), [py], [yT])
                    po = ps2()
                    for hf in range(2):
                        for k in range(8):
                            pe(lambda e, hf=hf, k=k: e.matmul(po[:, hf * 512:(hf + 1) * 512], lhsT=yT[:, k, :],
                                                              rhs=wout_b[:, k, hf * 512:(hf + 1) * 512],
                                                              start=(k == 0), stop=(k == 7)), [yT, wout_b], [po])
                    dve(lambda e: e.tensor_tensor(out=xt[:], in0=xt[:], in1=po[:], op=ALU.add), [xt, po], [xt])
                    if dbg and sq == 0 and l == 0:
                        mk.dma("sp", dbg_d["x1"][tsl, :], xt[:], reads=[xt], writes=[Buf("dbgx1")], stream="dbg")
                    if do_moe or l < n_layers - 1:
                        mk.dma("sp", xs_d[tsl, :], xt[:], reads=[xt], writes=[xsb[n]], stream="xs_st")
                    else:
                        mk.dma("sp", out_d[sq, tsl, :], xt[:], reads=[xt], writes=[Buf("outd")], stream="out")

                if not do_moe:
                    continue
                mk.barrier()
                junk = mjunk
                for n in range(NT):
                    mk.dma("sp", xres[n][:], xs_d[n * 128:(n + 1) * 128, :], reads=[xsb[n]], writes=[xres[n]])
                mk.dma("sp", wr[:, :, 0:4], w_g_d[l].rearrange("(k p) n -> p k n", p=128), writes=[wr])
                mk.dma("sp", wr[:, :, 4:36], w_e_d[l].rearrange("(k p) n -> p k n", p=128), writes=[wr])
                for k in range(8):
                    pool(lambda e, k=k: e.tensor_scalar(out=wr[:, k, :], in0=wr[:, k, :], scalar1=pvec[:, 16 + k:17 + k],
                                                        scalar2=None, op0=ALU.mult), [wr, pvec], [wr])
                for n in range(NT):
                    xt = xres[n]
                    act(lambda e: e.activation(out=junk[:], in_=xt[:], func=AF.Square, accum_out=stt[:, 0:1]),
                        [xt], [junk, stt])
                    rsqrt(stt[:, 1:2], stt[:, 0:1], 1.0 / D, c_eps, [stt], [stt])
                    dve(lambda e: e.tensor_scalar(out=xnf[:], in0=xt[:], scalar1=stt[:, 1:2], scalar2=None, op0=ALU.mult),
                        [xt, stt], [xnf])
                    for hf in range(2):
                        pt_ = ps1()
                        for k in range(4):
                            kk = hf * 4 + k
                            pe(lambda e, k=k, kk=kk: e.transpose(out=pt_[:, k * 128:(k + 1) * 128],
                                                                 in_=xnf[:, kk * 128:(kk + 1) * 128], identity=ident),
                               [xnf, cst], [pt_])
                        act(lambda e, hf=hf: e.copy(out=xnTf[:, hf * 4:hf * 4 + 4, :].rearrange("p a b -> p (a b)"),
                                                    in_=pt_[:]), [pt_], [xnTf])
                        dve(lambda e, hf=hf: e.tensor_copy(out=xnT[:, hf * 4:hf * 4 + 4, n * 128:(n + 1) * 128],
                                                           in_=v3(pt_[:], 4)), [pt_], [xnT])
                    pr = ps1()
                    for k in range(8):
                        pe(lambda e, k=k: e.matmul(pr[:, 0:36], lhsT=xnTf[:, k, :], rhs=wr[:, k, :], start=(k == 0),
                                                   stop=(k == 7)), [xnTf, wr], [pr])
                    act(lambda e, n=n: e.copy(out=rl[:, n, :], in_=pr[:, 0:36]), [pr], [rl])
                R = lambda i, w: rtmp[:, i, :, 0:w]
                gl = rl[:, :, 0:4]
                el = rl[:, :, 4:36]
                glb, gmx, goh, gex, gsm, ggt = R(0, 4), R(1, 1), R(2, 4), R(3, 4), R(4, 1), R(5, 1)
                RB = [rtmp, t32t]
                dve(lambda e: e.tensor_tensor(out=glb, in0=gl, in1=bgb[:, None, :].broadcast_to([128, NT, 4]), op=ALU.add),
                    [rl, rvec], RB)
                dve(lambda e: e.tensor_reduce(out=gmx, in_=glb, axis=AX.X, op=ALU.max), RB, RB)
                dve(lambda e: e.tensor_tensor(out=goh, in0=glb, in1=gmx.broadcast_to([128, NT, 4]), op=ALU.is_ge), RB, RB)
                dve(lambda e: e.tensor_reduce(out=gmx, in_=gl, axis=AX.X, op=ALU.max), [rl], RB)
                dve(lambda e: e.tensor_tensor(out=gex, in0=gl, in1=gmx.broadcast_to([128, NT, 4]), op=ALU.subtract),
                    [rl, rtmp], RB)
                act(lambda e: e.activation(out=gex, in_=gex, func=AF.Exp), RB, RB)
                dve(lambda e: e.tensor_reduce(out=gsm, in_=gex, axis=AX.X, op=ALU.add), RB, RB)
                dve(lambda e: e.tensor_tensor(out=gex, in0=gex, in1=goh, op=ALU.mult), RB, RB)
                dve(lambda e: e.tensor_reduce(out=ggt, in_=gex, axis=AX.X, op=ALU.add), RB, RB)
                dve(lambda e: e.reciprocal(out=gsm, in_=gsm), RB, RB)
                dve(lambda e: e.tensor_tensor(out=ggt, in0=ggt, in1=gsm, op=ALU.mult), RB, RB)
                t32, elb8, el8 = t32t[:], R(7, 8), R(8, 8)
                t32v = t32.rearrange("p n (g e) -> p n g e", g=4)
                gohb = goh[:, :, :, None].broadcast_to([128, NT, 4, 8])
                dve(lambda e: e.tensor_tensor(out=t32v, in0=el.rearrange("p n (g e) -> p n g e", g=4), in1=gohb,
                                              op=ALU.mult), [rl, rtmp], RB)
                dve(lambda e: e.tensor_reduce(out=el8, in_=t32.rearrange("p n (g e) -> p n e g", g=4), axis=AX.X,
                                              op=ALU.add), RB, RB)
                dve(lambda e: e.tensor_tensor(out=t32v, in0=beb[:, None, :].broadcast_to([128, NT, 32]).rearrange(
                    "p n (g e) -> p n g e", g=4), in1=gohb, op=ALU.mult), [rvec, rtmp], RB)
                dve(lambda e: e.tensor_reduce(out=elb8, in_=t32.rearrange("p n (g e) -> p n e g", g=4), axis=AX.X,
                                              op=ALU.add), RB, RB)
                dve(lambda e: e.tensor_tensor(out=elb8, in0=elb8, in1=el8, op=ALU.add), RB, RB)
                emx, eex, oh1, oh2, p1, p2 = R(9, 1), R(10, 8), R(11, 8), R(6, 8), R(1, 1), R(4, 1)
                dve(lambda e: e.tensor_reduce(out=emx, in_=el8, axis=AX.X, op=ALU.max), RB, RB)
                dve(lambda e: e.tensor_tensor(out=eex, in0=el8, in1=emx.broadcast_to([128, NT, 8]), op=ALU.subtract), RB, RB)
                act(lambda e: e.activation(out=eex, in_=eex, func=AF.Exp), RB, RB)
                dve(lambda e: e.tensor_reduce(out=emx, in_=elb8, axis=AX.X, op=ALU.max), RB, RB)
                dve(lambda e: e.tensor_tensor(out=oh1, in0=elb8, in1=emx.broadcast_to([128, NT, 8]), op=ALU.is_ge), RB, RB)
                dve(lambda e: e.scalar_tensor_tensor(out=elb8, in0=oh1, scalar=-1e30, in1=elb8, op0=ALU.mult, op1=ALU.add),
                    RB, RB)
                dve(lambda e: e.tensor_reduce(out=emx, in_=elb8, axis=AX.X, op=ALU.max), RB, RB)
                dve(lambda e: e.tensor_tensor(out=oh2, in0=elb8, in1=emx.broadcast_to([128, NT, 8]), op=ALU.is_ge), RB, RB)
                dve(lambda e: e.tensor_tensor(out=oh1, in0=oh1, in1=eex, op=ALU.mult), RB, RB)
                dve(lambda e: e.tensor_tensor(out=oh2, in0=oh2, in1=eex, op=ALU.mult), RB, RB)
                dve(lambda e: e.tensor_tensor(out=oh1, in0=oh1, in1=oh2, op=ALU.add), RB, RB)
                dve(lambda e: e.tensor_reduce(out=p1, in_=oh1, axis=AX.X, op=ALU.add), RB, RB)
                dve(lambda e: e.reciprocal(out=p1, in_=p1), RB, RB)
                dve(lambda e: e.tensor_tensor(out=p1, in0=p1, in1=ggt, op=ALU.mult), RB, RB)
                dve(lambda e: e.tensor_tensor(out=oh1, in0=oh1, in1=p1.broadcast_to([128, NT, 8]), op=ALU.mult), RB, RB)
                dve(lambda e: e.tensor_tensor(out=gate[:].rearrange("p n (g e) -> p n g e", g=4),
                                              in0=oh1[:, :, None, :].broadcast_to([128, NT, 4, 8]), in1=gohb, op=ALU.mult),
                    RB, [gate])
                for ex in range(32):
                    bi = ex % 2
                    wsl, wbl = wst, web[bi]
                    mk.dma("sp", wsl[0][:].rearrange("p (k n) -> p k n", k=8),
                           w1_d[l, ex].rearrange("(k p) n -> p k n", p=128), writes=[wsl[0]])
                    mk.dma("sp", wsl[1][:].rearrange("p (k n) -> p k n", k=8),
                           w3_d[l, ex].rearrange("(k p) n -> p k n", p=128), writes=[wsl[1]])
                    mk.dma("sp", wsl[2][:].rearrange("p (k n) -> p k n", k=2),
                           w2_d[l, ex].rearrange("(k p) n -> p k n", p=128), writes=[wsl[2]])
                    for j in range(2):
                        for k in range(8):
                            pool(lambda e, j=j, k=k: e.tensor_scalar(out=wbl[j][:, k * 256:(k + 1) * 256],
                                                                    in0=wsl[j][:, k * 256:(k + 1) * 256],
                                                                    scalar1=pvec[:, 16 + k:17 + k], scalar2=None,
                                                                    op0=ALU.mult), [wsl[j], pvec], [wbl[j]])
                    pool(lambda e: e.tensor_copy(out=wbl[2][:], in_=wsl[2][:]), [wsl[2]], [wbl[2]])
                    for g in range(4):
                        hg = hh[g % 2]
                        for c in range(2):
                            p1_ = ps1()
                            p3_ = ps1()
                            for j, pp in ((0, p1_), (1, p3_)):
                                for k in range(8):
                                    pe(lambda e, j=j, pp=pp, k=k, c=c: e.matmul(
                                        pp[:], lhsT=wbl[j][:, k * 256 + c * 128:k * 256 + (c + 1) * 128],
                                        rhs=xnT[:, k, g * 512:(g + 1) * 512], start=(k == 0), stop=(k == 7)),
                                       [wbl[j], xnT], [pp])
                            s1t = s1[c]
                            act(lambda e, s1t=s1t, p1_=p1_: e.activation(out=s1t[:], in_=p1_[:], func=AF.Silu), [p1_], [s1t])
                            dve(lambda e, s1t=s1t, p3_=p3_, c=c, hg=hg: e.tensor_tensor(out=hg[:, c, :], in0=s1t[:],
                                                                                     in1=p3_[:], op=ALU.mult),
                                [s1t, p3_], [hg])
                        for t in range(4):
                            n = g * 4 + t
                            py_ = ps2()
                            for hf in range(2):
                                for c in range(2):
                                    pe(lambda e, hf=hf, c=c, t=t, hg=hg, py_=py_: e.matmul(
                                        py_[:, hf * 512:(hf + 1) * 512], lhsT=hg[:, c, t * 128:(t + 1) * 128],
                                        rhs=wbl[2][:, c * 1024 + hf * 512:c * 1024 + (hf + 1) * 512], start=(c == 0),
                                        stop=(c == 1)), [hg, wbl[2]], [py_])
                            xt = xres[n]
                            dve(lambda e, xt=xt, py_=py_, n=n, ex=ex: e.scalar_tensor_tensor(
                                out=xt[:], in0=py_[:], scalar=gate[:, n, ex:ex + 1], in1=xt[:], op0=ALU.mult, op1=ALU.add),
                                [py_, gate, xt], [xt])
                for n in range(NT):
                    if l < n_layers - 1:
                        mk.dma("sp", xs_d[n * 128:(n + 1) * 128, :], xres[n][:], reads=[xres[n]], writes=[xsb[n]],
                               stream="xs_st")
                    else:
                        mk.dma("sp", out_d[sq, n * 128:(n + 1) * 128, :], xres[n][:], reads=[xres[n]],
                               writes=[Buf("outd")], stream="out")

        mk.barrier()
    return nc, mk


def _host_inputs(inputs):
    f = lambda k: np.ascontiguousarray(np.asarray(inputs[k], dtype=np.float32))
    x = f("x")
    pos = np.ascontiguousarray(np.asarray(inputs["positions"]).astype(np.int32))
    pvec = np.zeros((L, 128, 64), np.float32)
    pvec[:, :, 0:8] = f("attn_norm").reshape(L, 8, 128).transpose(0, 2, 1)
    pvec[:, :, 8:16] = f("mix_out_norm").reshape(L, 8, 128).transpose(0, 2, 1)
    pvec[:, :, 16:24] = f("ffn_norm").reshape(L, 8, 128).transpose(0, 2, 1)
    pvec[:, :, 24:26] = f("a_q_norm").reshape(L, 2, 128).transpose(0, 2, 1)
    pvec[:, :, 26:27] = f("a_kv_norm").reshape(L, 1, 128).transpose(0, 2, 1)
    pvec[:, :, 27:31] = f("c_b_s").transpose(0, 2, 1)
    convw = np.zeros((L, 96, 40), np.float32)
    convw[:, :, 0:32] = f("m_conv_w").reshape(L, 4, 8, 96).transpose(0, 3, 2, 1).reshape(L, 96, 32)
    convw[:, :, 32:40] = f("m_conv_b").reshape(L, 8, 96).transpose(0, 2, 1)
    rrow = np.zeros((L, 496), np.float32)
    rrow[:, 0:8] = f("m_gate_bias")
    rrow[:, 8:104] = f("a_q_head_norm")
    rrow[:, 104:200] = f("a_k_head_norm")
    rrow[:, 200:456] = f("c_v_norm")
    rrow[:, 456:460] = f("b_group")
    rrow[:, 460:492] = f("b_expert")
    rvec = np.ascontiguousarray(np.broadcast_to(rrow[:, None, :], (L, 128, 496)))
    consts = np.zeros((128, 528), np.float32)
    p = np.arange(128)
    consts[:, 0:128] = np.eye(128, dtype=np.float32)
    consts[:, 128:256] = (p[:, None] <= p[None, :]).astype(np.float32)
    consts[:, 256:384] = (p[:, None] >= p[None, :]).astype(np.float32)
    consts[:, 384:512] = 1.0
    consts[:, 512:528] = (1.0 / (10000.0 ** (np.arange(0, 32, 2, dtype=np.float32) / 32.0))).astype(np.float32)[None, :]
    shared = {
        "w_in": f("w_in"), "w_out": f("w_out"), "a_w_uq": f("a_w_uq"), "a_w_ukv": f("a_w_ukv"), "c_w_s": f("c_w_s"),
        "w_group": f("w_group"), "w_expert": f("w_expert"), "w1": f("w1"), "w3": f("w3"), "w2": f("w2"),
        "pvec": pvec, "convw": convw, "rvec": rvec, "consts": consts,
    }
    return x, pos, shared


def kernel(**inputs):
    x, pos, shared = _host_inputs(inputs)
    B = x.shape[0]
    per = B // NCORES
    nc, _ = build(n_seq=per, n_layers=L)
    in_maps = []
    for c in range(NCORES):
        m = dict(shared)
        m["x"] = np.ascontiguousarray(x[c * per:(c + 1) * per])
        pc = pos[c * per:(c + 1) * per].reshape(per, NT, 128).transpose(0, 2, 1)
        m["posT"] = np.ascontiguousarray(pc)
        in_maps.append(m)
    res = run_bass_kernel_spmd(nc, in_maps, core_ids=list(range(NCORES)))
    return np.concatenate([r["out"] for r in res.results], axis=0).astype(np.float32)
```

```python
import math
from contextlib import ExitStack
import numpy as np
import concourse.bass as bass
import concourse.mybir as mybir
from concourse.bass_utils import run_bass_kernel_spmd

F32 = mybir.dt.float32
BF16 = mybir.dt.bfloat16
I32 = mybir.dt.int32
AF = mybir.ActivationFunctionType
ALU = mybir.AluOpType
AX = mybir.AxisListType

NCORES = 8
S = 2048
D = 1024
NT = 16
L = 4
DIN = 2472
O_MQ, O_MK, O_MV, O_MO, O_MI, O_MF, O_CQ, O_CKV, O_KR, O_CU, O_CV = (
    0, 384, 768, 1152, 1536, 1540, 1544, 1800, 1928, 1960, 2216)
EPS = 1e-6


class Buf:
    __slots__ = ("name", "w", "r")

    def __init__(self, name):
        self.name = name
        self.w = None
        self.r = []


class Tl:
    def __init__(self, t, name):
        self.t = t
        self.b = Buf(name)

    def __getitem__(self, k):
        return self.t[k]


class _Rec:
    def __init__(self):
        self.call = None

    def __getattr__(self, name):
        def f(*a, **k):
            self.call = (name, a, k)
            return self
        return f


class MK:
    ENGS = ("pe", "dve", "act", "pool", "sp")

    def __init__(self, nc, stack):
        self.nc = nc
        self.stack = stack
        self.ops = {e: [] for e in self.ENGS}
        self.sems = {}
        self.count = {}
        self.seen = {e: {} for e in self.ENGS}
        for e in self.ENGS:
            self._sem("E_" + e)
        self.nops = 0

    def _emit(self, e, waits, fn, sem, inc):
        call = None
        if fn is not None:
            rec = _Rec()
            fn(rec)
            call = rec.call
        self.ops[e].append((waits, call, sem, inc))

    def emit(self):
        with self.nc.Block() as block:
            def mkbody(e):
                def body(eng):
                    for waits, call, sem, inc in self.ops[e]:
                        for s_, v in waits:
                            eng.wait_ge(s_, v)
                        if call is not None:
                            name, a, k = call
                            getattr(eng, name)(*a, **k).then_inc(sem, inc)
                return body
            block.tensor(mkbody("pe"))
            block.vector(mkbody("dve"))
            block.scalar(mkbody("act"))
            block.gpsimd(mkbody("pool"))
            block.sync(mkbody("sp"))

    def _sem(self, key):
        if key not in self.sems:
            self.sems[key] = self.stack.enter_context(self.nc.semaphore(key))
            self.count[key] = 0
        return self.sems[key]

    def sb(self, name, shape, dt):
        return Tl(self.stack.enter_context(self.nc.sbuf_tensor("sb_" + name, list(shape), dt)), name)

    def ps(self, name, shape, dt):
        return Tl(self.stack.enter_context(self.nc.psum_tensor(name, list(shape), dt)), name)

    def _waits(self, e, reads, writes):
        need = {}

        def add(ev, raw):
            if ev is None:
                return
            k, v, src = ev
            if src == e and (e == "pe" or not raw):
                return
            if need.get(k, 0) < v:
                need[k] = v
        for b in reads:
            add(b.w, True)
        for b in writes:
            add(b.w, False)
            for ev in b.r:
                add(ev, False)
        out = []
        seen = self.seen[e]
        for k, v in need.items():
            if seen.get(k, 0) < v:
                seen[k] = v
                out.append((self.sems[k], v))
        return out

    def _reg(self, ev, reads, writes):
        for b in reads:
            b.r.append(ev)
        for b in writes:
            b.w = ev
            b.r = []

    def op(self, e, fn, reads=(), writes=()):
        reads = [getattr(x, "b", x) for x in reads]
        writes = [getattr(x, "b", x) for x in writes]
        waits = self._waits(e, reads, writes)
        key = "E_" + e
        self.count[key] += 1
        ev = (key, self.count[key], e)
        self._reg(ev, reads, writes)
        self._emit(e, waits, fn, self.sems[key], 1)
        self.nops += 1

    def dma(self, q, out, in_, reads=(), writes=(), stream=None):
        reads = [getattr(x, "b", x) for x in reads]
        writes = [getattr(x, "b", x) for x in writes]
        waits = self._waits(q, reads, writes)
        key = "D_" + (stream or (writes[0].name if writes else reads[0].name))
        self._sem(key)
        self.count[key] += 16
        ev = (key, self.count[key], "dma")
        self._reg(ev, reads, writes)
        self._emit(q, waits, lambda eng: eng.dma_start(out=out, in_=in_), self.sems[key], 16)
        self.nops += 1

    def barrier(self):
        for e in self.ENGS:
            waits = []
            for k, v in self.count.items():
                if v > 0 and self.seen[e].get(k, 0) < v and k != "E_" + e:
                    self.seen[e][k] = v
                    waits.append((self.sems[k], v))
            self._emit(e, waits, None, None, 0)


def build(n_seq=4, n_layers=4, dbg=False, do_moe=True, n_exp=32):
    nc = bass.Bass("TRN2", target_bir_lowering=False)

    def din(name, shape, dt=F32):
        return nc.dram_tensor(name, list(shape), dt, kind="ExternalInput").ap()

    x_d = din("x", [n_seq, S, D])
    pos_d = din("posT", [n_seq, 128, NT], I32)
    w_in_d = din("w_in", [L, D, DIN])
    w_out_d = din("w_out", [L, D, D])
    w_uq_d = din("a_w_uq", [L, 256, 576])
    w_ukv_d = din("a_w_ukv", [L, 128, 768])
    c_ws_d = din("c_w_s", [L, 4, 128, 128])
    w_g_d = din("w_group", [L, D, 4])
    w_e_d = din("w_expert", [L, D, 32])
    w1_d = din("w1", [L, 32, D, 256])
    w3_d = din("w3", [L, 32, D, 256])
    w2_d = din("w2", [L, 32, 256, D])
    pv_d = din("pvec", [L, 128, 64])
    cw_d = din("convw", [L, 96, 40])
    rv_d = din("rvec", [L, 128, 496])
    gv_d = din("gvec", [L, 128, 2048])
    c_d = din("consts", [128, 528])
    out_d = nc.dram_tensor("out", [n_seq, S, D], F32, kind="ExternalOutput").ap()
    dbg_d = {}
    if dbg:
        dbg_d["y"] = nc.dram_tensor("dbg_y", [S, D], F32, kind="ExternalOutput").ap()
        dbg_d["x1"] = nc.dram_tensor("dbg_x1", [S, D], F32, kind="ExternalOutput").ap()

    st = ExitStack()
    with st:
        mk = MK(nc, st)
        dve = lambda fn, r, w: mk.op("dve", fn, r, w)
        act = lambda fn, r, w: mk.op("act", fn, r, w)
        pe = lambda fn, r, w: mk.op("pe", fn, r, w)
        pool = lambda fn, r, w: mk.op("pool", fn, r, w)

        SZ = {F32: 4, BF16: 2, I32: 4}
        ARENA_F = 47104
        arena_t = st.enter_context(nc.sbuf_tensor("arena", [128, ARENA_F], F32))

        class Arena:
            def __init__(self):
                self.off = 0

            def __call__(self, name, shape, dt):
                free = 1
                for d_ in shape[1:]:
                    free *= d_
                n4 = (free * SZ[dt] + 3) // 4
                assert self.off + n4 <= ARENA_F, (name, self.off, n4)
                v = arena_t[0:shape[0], self.off:self.off + n4]
                self.off += n4
                if dt != F32:
                    v = v.bitcast(dt)
                v = v[:, 0:free]
                if len(shape) > 2:
                    names = "abcd"[:len(shape) - 1]
                    kw = {names[i]: shape[1 + i] for i in range(len(shape) - 1)}
                    v = v.rearrange("p (" + " ".join(names) + ") -> p " + " ".join(names), **kw)
                return Tl(v, name)

        cst = mk.sb("cst", [128, 528], F32)
        identb = mk.sb("identb", [128, 128], BF16)
        mask4b = mk.sb("mask4b", [128, 4, 128], F32)
        sc = mk.sb("sc", [128, 8], F32)
        cossin = mk.sb("cossin", [128, 2, NT, 16], F32)
        pvec = mk.sb("pvec", [128, 64], F32)
        rvec = mk.sb("rvec", [128, 496], F32)
        convw = mk.sb("convw", [96, 40], F32)
        posi = mk.sb("posi", [128, NT], I32)
        posf = mk.sb("posf", [128, NT], F32)
        stt = mk.sb("stt", [128, 8], F32)
        hst = mk.sb("hst", [128, 3, 14], F32)
        g_t = mk.sb("g_t", [128, 8, 4], F32)
        gains = mk.sb("gains", [128, 2, D], F32)
        A = Arena()
        xtile = [A(f"xtile{i}", [128, D], F32) for i in range(2)]
        convd = A("convd", [96, 8, 4, 96], BF16)
        win_b = A("win_b", [128, 8, DIN], BF16)
        wout_b = A("wout_b", [128, 8, D], BF16)
        wuq_b = A("wuq_b", [128, 2, 576], BF16)
        wukv_b = A("wukv_b", [128, 768], BF16)
        wsT_b = A("wsT_b", [128, 4, 128], BF16)
        stage2 = A("stage2", [128, 1152], F32)
        KT = A("KT", [96, 6, S], BF16)
        Vaug = A("Vaug", [128, NT, 6, 65], BF16)
        junk = A("junk", [128, D], F32)
        hb = A("hb", [128, D], BF16)
        hTt = A("hTt", [128, 8, 128], BF16)
        xqk = A("xqk", [96, 8, 131], BF16)
        esil = A("esil", [96, 8, 128], F32)
        qkT = A("qkT", [96, 8, 128], BF16)
        Sm = A("Sm", [128, 4, 128], BF16)
        ktok = A("ktok", [128, 4, 96], BF16)
        Vt = A("Vt", [128, 4, 97], BF16)
        Cf = A("Cf", [96, 4, 97], F32)
        Cb = A("Cb", [96, 4, 97], BF16)
        og = A("og", [128, 384], F32)
        hm = A("hm", [128, 4, 96], F32)
        cT = A("cT", [128, 3, 128], BF16)
        qs = A("qs", [128, 6, 96], F32)
        ksv = A("ksv", [128, 6, 128], F32)
        kr = A("kr", [128, 6, 32], F32)
        krope = A("krope", [128, 32], F32)
        rt = A("rt", [128, 4, 6, 16], F32)
        qb = A("qb", [128, 6, 96], BF16)
        kb = A("kb", [128, 6, 96], BF16)
        QT = A("QT", [96, 6, 128], BF16)
        PT = [A(f"PT{i}", [128, 4, 128], BF16) for i in range(2)]
        ha = A("ha", [128, 6, 64], F32)
        ga = A("ga", [128, 512], F32)
        gb_ = A("gb_", [128, 512], F32)
        guv = A("guv", [128, 512], F32)
        vn = A("vn", [128, 256], BF16)
        hc = A("hc", [128, 256], F32)
        ycat = A("ycat", [128, D], BF16)
        yT = A("yT", [128, 8, 128], BF16)
        mixer_bytes = A.off * 4
        junk_mix = junk
        A = Arena()
        xres = [A(f"xres{n}", [128, D], F32) for n in range(NT)]
        xnT = A("xnT", [128, 8, S], BF16)
        xnf = A("xnf", [128, D], F32)
        xh = A("xh", [128, D], BF16)
        xl = A("xl", [128, D], BF16)
        xlT = A("xlT", [128, 8, 128], BF16)
        wrh = A("wrh", [128, 8, 36], BF16)
        wrl = A("wrl", [128, 8, 36], BF16)
        wr = A("wr", [128, 8, 36], F32)
        rl = A("rl", [128, NT, 36], F32)
        rtmp = A("rtmp", [128, 12, NT, 8], F32)
        t32t = A("t32t", [128, NT, 32], F32)
        ropet = A("ropet", [128, 7, NT, 16], F32)
        gate = A("gate", [128, NT, 32], F32)
        web = [[A(f"web{i}_{j}", [128, 2048], BF16) for j in range(3)] for i in range(2)]
        s1 = [A(f"s1_{i}", [128, 512], BF16) for i in range(2)]
        hh = [A(f"hh{i}", [128, 2, 512], BF16) for i in range(2)]
        mjunk = A("mjunk", [128, D], F32)
        moe_bytes = A.off * 4
        xs_d = nc.dram_tensor("xs_scratch", [S, D], F32, kind="Internal").ap()
        xsb = [Buf(f"xs{n}") for n in range(NT)]

        PS = [mk.ps(f"ps{i}", [128, 512], F32) for i in range(4)]
        PP = [mk.ps(f"pp{i}", [128, 1024], F32) for i in range(2)]
        ctr = {"s": 0, "p": 0}

        def ps1(exclude=None):
            ctr["s"] += 1
            if PS[ctr["s"] % 4] is exclude:
                ctr["s"] += 1
            return PS[ctr["s"] % 4]

        def ps2():
            ctr["p"] += 1
            return PP[ctr["p"] % 2]

        def v3(ap, a):
            return ap.rearrange("p (a b) -> p a b", a=a)

        mk.dma("sp", cst[:], c_d, writes=[cst])
        ident = cst[:, 0:128]
        maskle = cst[:, 128:256]
        maskge = cst[:, 256:384]
        ones = cst[:, 384:512]
        invf = cst[:, 512:528]
        dve(lambda e: e.tensor_copy(out=identb[:], in_=ident), [cst], [identb])
        for h in range(4):
            dve(lambda e, h=h: e.tensor_copy(out=mask4b[:, h, :], in_=maskle), [cst], [mask4b])
        pool(lambda e: e.memset(sc[:, 0:1], EPS), [], [sc])
        pool(lambda e: e.memset(sc[:, 1:2], 1.0), [], [sc])
        pool(lambda e: e.memset(sc[:, 2:3], EPS / 4), [], [sc])
        pool(lambda e: e.memset(sc[:, 3:4], 0.0), [], [sc])
        c_eps, c_one, c_eps4 = sc[:, 0:1], sc[:, 1:2], sc[:, 2:3]

        def rsqrt(out, in_, scale, bias, r, w):
            act(lambda e: e.activation(out=out, in_=in_, func=AF.Ln, scale=scale, bias=bias), r + [sc], w)
            act(lambda e: e.activation(out=out, in_=out, func=AF.Exp, scale=-0.5), w, w)

        for sq in range(n_seq):
            mk.barrier()
            mk.dma("sp", posi[:], pos_d[sq], writes=[posi])
            dve(lambda e: e.tensor_copy(out=posf[:], in_=posi[:]), [posi], [posf])
            ang = ropet[:, 0, :, :]
            dve(lambda e: e.tensor_tensor(out=ang, in0=posf[:, :, None].broadcast_to([128, NT, 16]),
                                          in1=invf[:, None, :].broadcast_to([128, NT, 16]), op=ALU.mult),
                [posf, cst], [ropet])
            for ci, shift in ((0, math.pi / 2), (1, 0.0)):
                a2 = ropet[:, 1, :, :]
                ki = ropet[:, 2, :, :]
                kf = ropet[:, 3, :, :]
                m1 = ropet[:, 4, :, :]
                RP = [ropet]
                dve(lambda e: e.tensor_scalar(out=a2, in0=ang, scalar1=shift, scalar2=None, op0=ALU.add), RP, RP)
                dve(lambda e: e.tensor_scalar(out=ki.bitcast(I32), in0=a2, scalar1=1.0 / (2 * math.pi), scalar2=None,
                                              op0=ALU.mult), RP, RP)
                dve(lambda e: e.tensor_copy(out=kf, in_=ki.bitcast(I32)), RP, RP)
                dve(lambda e: e.scalar_tensor_tensor(out=a2, in0=kf, scalar=-2 * math.pi, in1=a2, op0=ALU.mult,
                                                     op1=ALU.add), RP, RP)
                dve(lambda e: e.tensor_scalar(out=m1, in0=a2, scalar1=math.pi, scalar2=-2 * math.pi, op0=ALU.is_gt,
                                              op1=ALU.mult), RP, RP)
                dve(lambda e: e.tensor_tensor(out=a2, in0=a2, in1=m1, op=ALU.add), RP, RP)
                dve(lambda e: e.tensor_scalar(out=m1, in0=a2, scalar1=-math.pi, scalar2=2 * math.pi, op0=ALU.is_lt,
                                              op1=ALU.mult), RP, RP)
                dve(lambda e: e.tensor_tensor(out=a2, in0=a2, in1=m1, op=ALU.add), RP, RP)
                act(lambda e: e.activation(out=cossin[:, ci, :, :], in_=a2, func=AF.Sin), RP, [cossin])

            for l in range(n_layers):
                mk.barrier()
                junk = junk_mix
                pool(lambda e: e.memset(Vaug[:], 1.0), [], [Vaug])
                mk.dma("sp", pvec[:], pv_d[l], writes=[pvec])
                mk.dma("sp", rvec[:], rv_d[l], writes=[rvec])
                mk.dma("sp", convw[:], cw_d[l], writes=[convw])
                mk.dma("sp", gains[:].rearrange("p a b -> p (a b)"), gv_d[l], writes=[gains])
                for kc in range(8):
                    mk.dma("pool", win_b[:, kc, :], w_in_d[l, kc * 128:(kc + 1) * 128, :], writes=[win_b])
                for kc in range(8):
                    mk.dma("sp", stage2[:, 0:D], w_out_d[l, kc * 128:(kc + 1) * 128, :], writes=[stage2])
                    act(lambda e, kc=kc: e.activation(out=wout_b[:, kc, :], in_=stage2[:, 0:D], func=AF.Identity,
                                                      scale=pvec[:, 8 + kc:9 + kc]), [stage2, pvec], [wout_b])
                for c in range(2):
                    mk.dma("sp", stage2[:, 0:576], w_uq_d[l, c * 128:(c + 1) * 128, :], writes=[stage2])
                    act(lambda e, c=c: e.activation(out=wuq_b[:, c, :], in_=stage2[:, 0:576], func=AF.Identity,
                                                    scale=pvec[:, 24 + c:25 + c]), [stage2, pvec], [wuq_b])
                mk.dma("sp", stage2[:, 0:768], w_ukv_d[l], writes=[stage2])
                act(lambda e: e.activation(out=wukv_b[:], in_=stage2[:, 0:768], func=AF.Identity, scale=pvec[:, 26:27]),
                    [stage2, pvec], [wukv_b])
                mk.dma("sp", stage2[:, 0:512].rearrange("p (g s) -> p g s", g=4),
                       c_ws_d[l].rearrange("g t s -> t g s"), writes=[stage2])
                pool(lambda e: e.tensor_tensor(out=hb[:, 0:512].rearrange("p (g s) -> p g s", g=4),
                                               in0=stage2[:, 0:512].rearrange("p (g s) -> p g s", g=4),
                                               in1=maskge[:, None, :].broadcast_to([128, 4, 128]), op=ALU.mult),
                     [stage2, cst], [hb])
                pw = ps1()
                pwb = pw[:].bitcast(BF16)
                for g in range(4):
                    pe(lambda e, g=g: e.transpose(out=pwb[:, g * 128:(g + 1) * 128], in_=hb[:, g * 128:(g + 1) * 128],
                                                  identity=identb[:]), [hb, identb], [pw])
                dve(lambda e: e.tensor_copy(out=wsT_b[:].rearrange("p g t -> p (g t)"), in_=pwb[:, 0:512]), [pw], [wsT_b])
                for ch in range(8):
                    for j in range(4):
                        pool(lambda e, ch=ch, j=j: e.tensor_scalar(out=convd[:, ch, j, :], in0=cst[0:96, 0:96],
                                                                  scalar1=convw[:, ch * 4 + j:ch * 4 + j + 1],
                                                                  scalar2=None, op0=ALU.mult), [cst, convw], [convd])
                pool(lambda e: e.tensor_scalar(out=rvec[:, 0:4], in0=rvec[:, 0:4], scalar1=-0.5 * math.log(96.0),
                                               scalar2=None, op0=ALU.add), [rvec], [rvec])
                pool(lambda e: e.memset(Cf[:], 0.0), [], [Cf])
                pool(lambda e: e.memset(Cb[:], 0.0), [], [Cb])
                pool(lambda e: e.memset(xqk[:], 0.0), [], [xqk])
                gbias = rvec[:, 0:8]
                qhg = rvec[:, 8:104]
                khg = rvec[:, 104:200]
                cvg = rvec[:, 200:456]
                bgb = rvec[:, 456:460]
                beb = rvec[:, 460:492]
                bsb = pvec[:, 27:31]
                convb = convw[:, 32:40]

                for n in range(NT):
                    xt = xtile[n % 2]
                    tsl = slice(n * 128, (n + 1) * 128)
                    if l == 0:
                        mk.dma("sp", xt[:], x_d[sq, tsl, :], writes=[xt])
                    else:
                        mk.dma("sp", xt[:], xs_d[tsl, :], reads=[xsb[n]], writes=[xt])
                    act(lambda e: e.activation(out=junk[:], in_=xt[:], func=AF.Square, accum_out=stt[:, 0:1]),
                        [xt], [junk, stt])
                    rsqrt(stt[:, 1:2], stt[:, 0:1], 1.0 / D, c_eps, [stt], [stt])
                    dve(lambda e: e.scalar_tensor_tensor(out=hb[:], in0=xt[:], scalar=stt[:, 1:2], in1=gains[:, 0, :],
                                                         op0=ALU.mult, op1=ALU.mult), [xt, stt, gains], [hb])
                    p0 = ps1()
                    p0b = p0[:].bitcast(BF16)
                    for k in range(8):
                        pe(lambda e, k=k: e.transpose(out=p0b[:, k * 128:(k + 1) * 128], in_=hb[:, k * 128:(k + 1) * 128],
                                                      identity=identb[:]), [hb, identb], [p0])
                    act(lambda e: e.copy(out=hTt[:].rearrange("p k t -> p (k t)"), in_=p0b[:, 0:1024]), [p0], [hTt])

                    def proj_tok(c0, c1):
                        p = ps1()
                        for k in range(8):
                            pe(lambda e, k=k: e.matmul(p[:, 0:c1 - c0], lhsT=hTt[:, k, :], rhs=win_b[:, k, c0:c1],
                                                       start=(k == 0), stop=(k == 7)), [hTt, win_b], [p])
                        return p
                    pT2 = proj_tok(O_MO, O_MO + 392)
                    ipre, lf, einvb, ks, a_t, rec = (g_t[:, i, :] for i in range(6))
                    dve(lambda e: e.tensor_tensor(out=ipre, in0=pT2[:, 384:388], in1=gbias[:, 0:4], op=ALU.add),
                        [pT2, rvec], [g_t])
                    dve(lambda e: e.tensor_tensor(out=lf, in0=pT2[:, 388:392], in1=gbias[:, 4:8], op=ALU.add),
                        [pT2, rvec], [g_t])
                    act(lambda e: e.activation(out=lf, in_=lf, func=AF.Exp, scale=-1.0), [g_t], [g_t])
                    act(lambda e: e.activation(out=lf, in_=lf, func=AF.Ln, bias=c_one), [g_t, sc], [g_t])
                    pg = ps1()
                    pe(lambda e: e.matmul(pg[:, 0:4], lhsT=maskle, rhs=lf, start=True, stop=True), [cst, g_t], [pg])
                    pe(lambda e: e.matmul(pg[:, 4:8], lhsT=ones, rhs=lf, start=True, stop=True), [cst, g_t], [pg])
                    act(lambda e: e.activation(out=einvb, in_=pg[:, 0:4], func=AF.Exp), [pg], [g_t])
                    dve(lambda e: e.tensor_tensor(out=ks, in0=ipre, in1=pg[:, 0:4], op=ALU.add), [pg, g_t], [g_t])
                    act(lambda e: e.activation(out=ks, in_=ks, func=AF.Exp), [g_t], [g_t])
                    act(lambda e: e.activation(out=a_t, in_=pg[:, 4:8], func=AF.Exp, scale=-1.0), [pg], [g_t])
                    act(lambda e: e.activation(out=og[:], in_=pT2[:, 0:384], func=AF.Exp, scale=-1.0), [pT2], [og])
                    dve(lambda e: e.tensor_scalar(out=og[:], in0=og[:], scalar1=1.0, scalar2=None, op0=ALU.add), [og], [og])
                    dve(lambda e: e.reciprocal(out=og[:], in_=og[:]), [og], [og])
                    pT1 = proj_tok(O_MV, O_MV + 384)
                    for h in range(4):
                        dve(lambda e, h=h: e.tensor_scalar(out=Vt[:, h, 0:96], in0=pT1[:, h * 96:(h + 1) * 96],
                                                           scalar1=ks[:, h:h + 1], scalar2=None, op0=ALU.mult),
                            [pT1, g_t], [Vt])
                    dve(lambda e: e.tensor_copy(out=Vt[:, :, 96], in_=ks), [g_t], [Vt])
                    pq = ps2()
                    for ch in range(8):
                        for k in range(8):
                            pe(lambda e, ch=ch, k=k: e.matmul(pq[0:96, ch * 128:(ch + 1) * 128],
                                                              lhsT=win_b[:, k, ch * 96:(ch + 1) * 96], rhs=hTt[:, k, :],
                                                              start=(k == 0), stop=(k == 7)), [hTt, win_b], [pq])
                    act(lambda e: e.copy(out=xqk[:, :, 3:131], in_=v3(pq[0:96, :], 8)), [pq], [xqk])
                    pc = ps2()
                    for ch in range(8):
                        for j in range(4):
                            pe(lambda e, ch=ch, j=j: e.matmul(pc[0:96, ch * 128:(ch + 1) * 128], lhsT=convd[:, ch, j, :],
                                                              rhs=xqk[:, ch, j:j + 128], start=(j == 0), stop=(j == 3)),
                               [convd, xqk], [pc])
                    for ch in range(8):
                        act(lambda e, ch=ch: e.activation(out=esil[:, ch, :], in_=pc[0:96, ch * 128:(ch + 1) * 128],
                                                          func=AF.Identity, bias=convb[:, ch:ch + 1]),
                            [pc, convw], [esil])
                    act(lambda e: e.activation(out=junk[0:96, :], in_=esil[:].rearrange("p a b -> p (a b)"),
                                               func=AF.Exp, scale=-1.0), [esil], [junk])
                    dve(lambda e: e.tensor_scalar(out=junk[0:96, :], in0=junk[0:96, :], scalar1=1.0, scalar2=None,
                                                  op0=ALU.add), [junk], [junk])
                    dve(lambda e: e.reciprocal(out=junk[0:96, :], in_=junk[0:96, :]), [junk], [junk])
                    dve(lambda e: e.tensor_tensor(out=qkT[:].rearrange("p a b -> p (a b)"),
                                                  in0=esil[:].rearrange("p a b -> p (a b)"), in1=junk[0:96, :],
                                                  op=ALU.mult), [esil, junk], [qkT])
                    pool(lambda e: e.tensor_copy(out=xqk[:, :, 0:3], in_=xqk[:, :, 128:131]), [xqk], [xqk])

                    pS = ps1()
                    for h in range(4):
                        pe(lambda e, h=h: e.matmul(pS[:, h * 128:(h + 1) * 128], lhsT=qkT[:, 4 + h, :], rhs=qkT[:, h, :],
                                                   start=True, stop=True), [qkT], [pS])
                    dve(lambda e: e.tensor_tensor(out=Sm[:], in0=v3(pS[:], 4), in1=mask4b[:], op=ALU.mult),
                        [pS, mask4b], [Sm])
                    pk = ps1()
                    pkb = pk[:].bitcast(BF16)
                    for h in range(4):
                        pe(lambda e, h=h: e.transpose(out=pkb[:, h * 96:(h + 1) * 96], in_=qkT[:, 4 + h, :],
                                                      identity=identb[0:96, 0:96]), [qkT, identb], [pk])
                    act(lambda e: e.copy(out=ktok[:].rearrange("p a b -> p (a b)"), in_=pkb[:, 0:384]), [pk], [ktok])
                    pN = ps1()
                    for h in range(4):
                        pe(lambda e, h=h: e.matmul(pN[:, h * 97:(h + 1) * 97], lhsT=Sm[:, h, :], rhs=Vt[:, h, :],
                                                   start=True, stop=False), [Sm, Vt], [pN])
                        pe(lambda e, h=h: e.matmul(pN[:, h * 97:(h + 1) * 97], lhsT=qkT[:, h, :], rhs=Cb[:, h, :],
                                                   start=False, stop=True), [qkT, Cb], [pN])
                    pD = ps1()
                    for h in range(4):
                        pe(lambda e, h=h: e.matmul(pD[0:96, h * 97:(h + 1) * 97], lhsT=ktok[:, h, :], rhs=Vt[:, h, :],
                                                   start=True, stop=True), [ktok, Vt], [pD])
                    dve(lambda e: e.tensor_tensor(out=Cf[:].rearrange("p a b -> p (a b)"),
                                                  in0=Cf[:].rearrange("p a b -> p (a b)"), in1=pD[0:96, 0:388],
                                                  op=ALU.add), [Cf, pD], [Cf])
                    for h in range(4):
                        dve(lambda e, h=h: e.tensor_scalar(out=Cf[:, h, :], in0=Cf[:, h, :], scalar1=a_t[0:96, h:h + 1],
                                                           scalar2=None, op0=ALU.mult), [Cf, g_t], [Cf])
                    act(lambda e: e.copy(out=Cb[:], in_=Cf[:]), [Cf], [Cb])
                    pNv = pN[:, 0:388].rearrange("p (a b) -> p a b", a=4)
                    act(lambda e: e.activation(out=rec, in_=pNv[:, :, 96], func=AF.Abs), [pN], [g_t])
                    dve(lambda e: e.tensor_tensor(out=rec, in0=rec, in1=einvb, op=ALU.max), [g_t], [g_t])
                    dve(lambda e: e.reciprocal(out=rec, in_=rec), [g_t], [g_t])
                    dve(lambda e: e.tensor_tensor(out=hm[:], in0=pNv[:, :, 0:96],
                                                  in1=rec[:, :, None].broadcast_to([128, 4, 96]), op=ALU.mult),
                        [pN, g_t], [hm])
                    dve(lambda e: e.tensor_tensor(out=hm[:].rearrange("p a b -> p (a b)"),
                                                  in0=hm[:].rearrange("p a b -> p (a b)"), in1=og[:], op=ALU.mult),
                        [hm, og], [hm])

                    pT3 = proj_tok(O_CQ, O_CQ + 416)
                    act(lambda e: e.activation(out=junk[:, 0:256], in_=pT3[:, 0:256], func=AF.Square,
                                               accum_out=stt[:, 2:3]), [pT3], [junk, stt])
                    act(lambda e: e.activation(out=junk[:, 256:384], in_=pT3[:, 256:384], func=AF.Square,
                                               accum_out=stt[:, 3:4]), [pT3], [junk, stt])
                    act(lambda e: e.copy(out=krope[:], in_=pT3[:, 384:416]), [pT3], [krope])
                    act(lambda e: e.activation(out=junk[:, 384:416], in_=pT3[:, 384:416], func=AF.Square,
                                               accum_out=stt[:, 6:7]), [pT3], [junk, stt])
                    rsqrt(stt[:, 4:5], stt[:, 2:3], 1.0 / 256, c_eps, [stt], [stt])
                    rsqrt(stt[:, 5:6], stt[:, 3:4], 1.0 / 128, c_eps, [stt], [stt])
                    pC = ps1()
                    for c in range(3):
                        for k in range(8):
                            pe(lambda e, c=c, k=k: e.matmul(pC[:, c * 128:(c + 1) * 128],
                                                            lhsT=win_b[:, k, O_CQ + c * 128:O_CQ + (c + 1) * 128],
                                                            rhs=hTt[:, k, :], start=(k == 0), stop=(k == 7)),
                               [hTt, win_b], [pC])
                    act(lambda e: e.copy(out=cT[:].rearrange("p a b -> p (a b)"), in_=pC[:, 0:384]), [pC], [cT])
                    pQ = ps2()
                    for j in range(2):
                        for c in range(2):
                            pe(lambda e, j=j, c=c: e.matmul(pQ[:, j * 512:j * 512 + 288], lhsT=cT[:, c, :],
                                                            rhs=wuq_b[:, c, j * 288:(j + 1) * 288],
                                                            start=(c == 0), stop=(c == 1)), [cT, wuq_b], [pQ])
                    pK = ps2()
                    for j in range(2):
                        pe(lambda e, j=j: e.matmul(pK[:, j * 512:j * 512 + 384], lhsT=cT[:, 2, :],
                                                   rhs=wukv_b[:, j * 384:(j + 1) * 384], start=True, stop=True),
                           [cT, wukv_b], [pK])
                    for j in range(2):
                        dve(lambda e, j=j: e.tensor_scalar(out=qs[:, 3 * j:3 * j + 3, :].rearrange("p a b -> p (a b)"),
                                                           in0=pQ[:, j * 512:j * 512 + 288], scalar1=stt[:, 4:5],
                                                           scalar2=None, op0=ALU.mult), [pQ, stt], [qs])
                        dve(lambda e, j=j: e.tensor_scalar(out=ksv[:, 3 * j:3 * j + 3, :].rearrange("p a b -> p (a b)"),
                                                           in0=pK[:, j * 512:j * 512 + 384], scalar1=stt[:, 5:6],
                                                           scalar2=None, op0=ALU.mult), [pK, stt], [ksv])
                    jq = junk[:, 0:576].rearrange("p (a b) -> p a b", a=6)
                    act(lambda e: e.activation(out=jq, in_=qs[:], func=AF.Square), [qs], [junk])
                    dve(lambda e: e.reduce_sum(out=hst[:, 0, 0:6], in_=jq, axis=AX.X), [junk], [hst])
                    rsqrt(hst[:, 1, 0:6], hst[:, 0, 0:6], 1.0 / 96, c_eps, [hst], [hst])
                    dve(lambda e: e.tensor_tensor(out=qs[:], in0=qs[:], in1=hst[:, 1, 0:6, None].broadcast_to([128, 6, 96]),
                                                  op=ALU.mult), [qs, hst], [qs])
                    dve(lambda e: e.tensor_tensor(out=qs[:], in0=qs[:], in1=qhg[:, None, :].broadcast_to([128, 6, 96]),
                                                  op=ALU.mult), [qs, rvec], [qs])
                    jk = junk[:, 0:384].rearrange("p (a b) -> p a b", a=6)
                    act(lambda e: e.activation(out=jk, in_=ksv[:, :, 0:64], func=AF.Square), [ksv], [junk])
                    dve(lambda e: e.reduce_sum(out=hst[:, 0, 6:12], in_=jk, axis=AX.X), [junk], [hst])
                    dve(lambda e: e.tensor_scalar(out=hst[:, 0, 6:12], in0=hst[:, 0, 6:12], scalar1=stt[:, 6:7],
                                                  scalar2=None, op0=ALU.add), [hst, stt], [hst])
                    rsqrt(hst[:, 1, 6:12], hst[:, 0, 6:12], 1.0 / 96, c_eps, [hst], [hst])
                    rk = hst[:, 1, 6:12]
                    dve(lambda e: e.tensor_tensor(out=jk, in0=ksv[:, :, 0:64], in1=rk[:, :, None].broadcast_to([128, 6, 64]),
                                                  op=ALU.mult), [ksv, hst], [junk])
                    dve(lambda e: e.tensor_tensor(out=kb[:, :, 0:64], in0=jk,
                                                  in1=khg[:, None, 0:64].broadcast_to([128, 6, 64]), op=ALU.mult),
                        [junk, rvec], [kb])
                    dve(lambda e: e.tensor_tensor(out=kr[:], in0=krope[:, None, :].broadcast_to([128, 6, 32]),
                                                  in1=rk[:, :, None].broadcast_to([128, 6, 32]), op=ALU.mult),
                        [krope, hst], [kr])
                    dve(lambda e: e.tensor_tensor(out=kr[:], in0=kr[:], in1=khg[:, None, 64:96].broadcast_to([128, 6, 32]),
                                                  op=ALU.mult), [kr, rvec], [kr])
                    cosb = cossin[:, 0, n, None, :].broadcast_to([128, 6, 16])
                    sinb = cossin[:, 1, n, None, :].broadcast_to([128, 6, 16])

                    def rope(src1, src2, dst1, dst2, rb, wb):
                        t = [rt[:, i, :, :] for i in range(4)]
                        dve(lambda e: e.tensor_tensor(out=t[0], in0=src1, in1=cosb, op=ALU.mult), rb + [cossin], [rt])
                        dve(lambda e: e.tensor_tensor(out=t[1], in0=src2, in1=sinb, op=ALU.mult), rb + [cossin], [rt])
                        dve(lambda e: e.tensor_tensor(out=t[2], in0=src1, in1=sinb, op=ALU.mult), rb + [cossin], [rt])
                        dve(lambda e: e.tensor_tensor(out=t[3], in0=src2, in1=cosb, op=ALU.mult), rb + [cossin], [rt])
                        dve(lambda e: e.tensor_tensor(out=dst1, in0=t[0], in1=t[1], op=ALU.subtract), [rt], wb)
                        dve(lambda e: e.tensor_tensor(out=dst2, in0=t[2], in1=t[3], op=ALU.add), [rt], wb)
                    rope(qs[:, :, 64:80], qs[:, :, 80:96], qb[:, :, 64:80], qb[:, :, 80:96], [qs], [qb])
                    dve(lambda e: e.tensor_copy(out=qb[:, :, 0:64], in_=qs[:, :, 0:64]), [qs], [qb])
                    rope(kr[:, :, 0:16], kr[:, :, 16:32], kb[:, :, 64:80], kb[:, :, 80:96], [kr], [kb])
                    act(lambda e: e.copy(out=Vaug[:, n, :, 0:64], in_=ksv[:, :, 64:128]), [ksv], [Vaug])
                    pqt = ps1()
                    pqtb = pqt[:].bitcast(BF16)
                    for h in range(6):
                        pe(lambda e, h=h: e.transpose(out=pqtb[0:96, h * 128:(h + 1) * 128], in_=qb[:, h, :],
                                                      identity=identb[:]), [qb, identb], [pqt])
                    act(lambda e: e.copy(out=QT[:].rearrange("p a b -> p (a b)"), in_=pqtb[0:96, 0:768]), [pqt], [QT])
                    pkt = ps1()
                    pktb = pkt[:].bitcast(BF16)
                    for h in range(6):
                        pe(lambda e, h=h: e.transpose(out=pktb[0:96, h * 128:(h + 1) * 128], in_=kb[:, h, :],
                                                      identity=identb[:]), [kb, identb], [pkt])
                    act(lambda e: e.copy(out=KT[:, :, tsl], in_=pktb[0:96, 0:768].rearrange("p (a b) -> p a b", a=6)),
                        [pkt], [KT])
                    pO = ps1()
                    cnt = 0
                    for h in range(6):
                        for j0 in range(0, n + 1, 4):
                            nb = min(4, n + 1 - j0)
                            pa = ps1(exclude=pO)
                            for jj in range(nb):
                                j = j0 + jj
                                pe(lambda e, h=h, j=j, jj=jj: e.matmul(pa[:, jj * 128:(jj + 1) * 128],
                                                                       lhsT=KT[:, h, j * 128:(j + 1) * 128], rhs=QT[:, h, :],
                                                                       start=True, stop=True), [KT, QT], [pa])
                            pt = PT[cnt % 2]
                            cnt += 1
                            act(lambda e, nb=nb, pt=pt, pa=pa: e.activation(
                                out=pt[:, 0:nb, :].rearrange("p a b -> p (a b)"), in_=pa[:, 0:nb * 128], func=AF.Exp,
                                scale=96.0 ** -0.5), [pa], [pt])
                            if j0 + nb == n + 1:
                                pool(lambda e, nb=nb, pt=pt: e.tensor_tensor(out=pt[:, nb - 1, :], in0=pt[:, nb - 1, :],
                                                                            in1=maskle, op=ALU.mult), [pt, cst], [pt])
                            for jj in range(nb):
                                j = j0 + jj
                                pe(lambda e, h=h, j=j, jj=jj, pt=pt: e.matmul(pO[:, h * 65:(h + 1) * 65], lhsT=pt[:, jj, :],
                                                                              rhs=Vaug[:, j, h, :], start=(j == 0),
                                                                              stop=(j == n)), [pt, Vaug], [pO])
                    pOv = pO[:, 0:390].rearrange("p (a b) -> p a b", a=6)
                    dve(lambda e: e.reciprocal(out=hst[:, 2, 0:6], in_=pOv[:, :, 64]), [pO], [hst])
                    dve(lambda e: e.tensor_tensor(out=ha[:], in0=pOv[:, :, 0:64],
                                                  in1=hst[:, 2, 0:6, None].broadcast_to([128, 6, 64]), op=ALU.mult),
                        [pO, hst], [ha])

                    pT4 = proj_tok(O_CU, O_CU + 512)
                    act(lambda e: e.activation(out=ga[:], in_=pT4[:], func=AF.Square), [pT4], [ga])
                    dve(lambda e: e.tensor_scalar(out=ga[:], in0=ga[:], scalar1=0.044715, scalar2=1.0, op0=ALU.mult,
                                                  op1=ALU.add), [ga], [ga])
                    dve(lambda e: e.tensor_tensor(out=ga[:], in0=ga[:], in1=pT4[:], op=ALU.mult), [ga, pT4], [ga])
                    act(lambda e: e.activation(out=gb_[:], in_=ga[:], func=AF.Exp, scale=-2.0 * math.sqrt(2.0 / math.pi)),
                        [ga], [gb_])
                    dve(lambda e: e.tensor_scalar(out=gb_[:], in0=gb_[:], scalar1=1.0, scalar2=None, op0=ALU.add),
                        [gb_], [gb_])
                    dve(lambda e: e.reciprocal(out=gb_[:], in_=gb_[:]), [gb_], [gb_])
                    dve(lambda e: e.tensor_tensor(out=guv[:], in0=gb_[:], in1=pT4[:], op=ALU.mult), [gb_, pT4], [guv])
                    gv = guv[:, 256:512].rearrange("p (a b) -> p a b", a=4)
                    jv = junk[:, 0:256].rearrange("p (a b) -> p a b", a=4)
                    act(lambda e: e.activation(out=jv, in_=gv, func=AF.Square), [guv], [junk])
                    dve(lambda e: e.reduce_sum(out=g_t[:, 6, :], in_=jv, axis=AX.X), [junk], [g_t])
                    rsqrt(g_t[:, 7, :], g_t[:, 6, :], 1.0 / 64, c_eps, [g_t], [g_t])
                    dve(lambda e: e.tensor_tensor(out=vn[:].rearrange("p (a b) -> p a b", a=4), in0=gv,
                                                  in1=g_t[:, 7, :, None].broadcast_to([128, 4, 64]), op=ALU.mult),
                        [guv, g_t], [vn])
                    pM = ps1()
                    for g in range(4):
                        pe(lambda e, g=g: e.matmul(pM[:, g * 64:(g + 1) * 64], lhsT=wsT_b[:, g, :],
                                                   rhs=vn[:, g * 64:(g + 1) * 64], start=True, stop=True), [wsT_b, vn], [pM])
                    dve(lambda e: e.tensor_tensor(out=hc[:], in0=pM[:, 0:256], in1=cvg, op=ALU.mult), [pM, rvec], [hc])
                    hcv = hc[:].rearrange("p (a b) -> p a b", a=4)
                    dve(lambda e: e.tensor_tensor(out=hcv, in0=hcv, in1=bsb[:, :, None].broadcast_to([128, 4, 64]),
                                                  op=ALU.add), [hc, pvec], [hc])
                    dve(lambda e: e.tensor_tensor(out=hc[:], in0=hc[:], in1=guv[:, 0:256], op=ALU.mult), [hc, guv], [hc])

                    jm = junk[:, 0:384].rearrange("p (a b) -> p a b", a=4)
                    act(lambda e: e.activation(out=jm, in_=hm[:], func=AF.Square), [hm], [junk])
                    dve(lambda e: e.reduce_sum(out=hst[:, 0, 0:4], in_=jm, axis=AX.X), [junk], [hst])
                    ja = junk[:, 384:768].rearrange("p (a b) -> p a b", a=6)
                    act(lambda e: e.activation(out=ja, in_=ha[:], func=AF.Square), [ha], [junk])
                    dve(lambda e: e.reduce_sum(out=hst[:, 0, 4:10], in_=ja, axis=AX.X), [junk], [hst])
                    jc = junk[:, 768:1024].rearrange("p (a b) -> p a b", a=4)
                    act(lambda e: e.activation(out=jc, in_=hcv, func=AF.Square), [hc], [junk])
                    dve(lambda e: e.reduce_sum(out=hst[:, 0, 10:14], in_=jc, axis=AX.X), [junk], [hst])
                    rsqrt(hst[:, 1, 0:4], hst[:, 0, 0:4], 1.0 / 96, c_eps, [hst], [hst])
                    rsqrt(hst[:, 1, 4:14], hst[:, 0, 4:14], 1.0 / 64, c_eps, [hst], [hst])
                    dve(lambda e: e.tensor_tensor(out=ycat[:, 0:384].rearrange("p (a b) -> p a b", a=4), in0=hm[:],
                                                  in1=hst[:, 1, 0:4, None].broadcast_to([128, 4, 96]), op=ALU.mult),
                        [hm, hst], [ycat])
                    dve(lambda e: e.tensor_tensor(out=ycat[:, 384:768].rearrange("p (a b) -> p a b", a=6), in0=ha[:],
                                                  in1=hst[:, 1, 4:10, None].broadcast_to([128, 6, 64]), op=ALU.mult),
                        [ha, hst], [ycat])
                    dve(lambda e: e.tensor_tensor(out=ycat[:, 768:1024].rearrange("p (a b) -> p a b", a=4), in0=hcv,
                                                  in1=hst[:, 1, 10:14, None].broadcast_to([128, 4, 64]), op=ALU.mult),
                        [hc, hst], [ycat])
                    if dbg and sq == 0 and l == 0:
                        dve(lambda e: e.tensor_copy(out=junk[:], in_=ycat[:]), [ycat], [junk])
                        mk.dma("sp", dbg_d["y"][tsl, :], junk[:], reads=[junk], writes=[Buf("dbgy")], stream="dbg")
                    py = ps1()
                    pyb = py[:].bitcast(BF16)
                    for k in range(8):
                        pe(lambda e, k=k: e.transpose(out=pyb[:, k * 128:(k + 1) * 128], in_=ycat[:, k * 128:(k + 1) * 128],
                                                      identity=identb[:]), [ycat, identb], [py])
                    act(lambda e: e.copy(out=yT[:].rearrange("p a b -> p (a b)"), in_=pyb[:, 0:1024]), [py], [yT])
                    po = ps2()
                    for hf in range(2):
                        for k in range(8):
                            pe(lambda e, hf=hf, k=k: e.matmul(po[:, hf * 512:(hf + 1) * 512], lhsT=yT[:, k, :],
                                                              rhs=wout_b[:, k, hf * 512:(hf + 1) * 512],
                                                              start=(k == 0), stop=(k == 7)), [yT, wout_b], [po])
                    dve(lambda e: e.tensor_tensor(out=xt[:], in0=xt[:], in1=po[:], op=ALU.add), [xt, po], [xt])
                    if dbg and sq == 0 and l == 0:
                        mk.dma("sp", dbg_d["x1"][tsl, :], xt[:], reads=[xt], writes=[Buf("dbgx1")], stream="dbg")
                    if do_moe or l < n_layers - 1:
                        mk.dma("sp", xs_d[tsl, :], xt[:], reads=[xt], writes=[xsb[n]], stream="xs_st")
                    else:
                        mk.dma("sp", out_d[sq, tsl, :], xt[:], reads=[xt], writes=[Buf("outd")], stream="out")

                if not do_moe:
                    continue
                mk.barrier()
                junk = mjunk
                for n in range(NT):
                    mk.dma("sp", xres[n][:], xs_d[n * 128:(n + 1) * 128, :], reads=[xsb[n]], writes=[xres[n]])
                mk.dma("sp", wr[:, :, 0:4], w_g_d[l].rearrange("(k p) n -> p k n", p=128), writes=[wr])
                mk.dma("sp", wr[:, :, 4:36], w_e_d[l].rearrange("(k p) n -> p k n", p=128), writes=[wr])
                dve(lambda e: e.tensor_copy(out=wrh[:], in_=wr[:]), [wr], [wrh])
                dve(lambda e: e.tensor_tensor(out=wrl[:], in0=wr[:], in1=wrh[:], op=ALU.subtract), [wr, wrh], [wrl])
                for n in range(NT):
                    xt = xres[n]
                    act(lambda e: e.activation(out=junk[:], in_=xt[:], func=AF.Square, accum_out=stt[:, 0:1]),
                        [xt], [junk, stt])
                    rsqrt(stt[:, 1:2], stt[:, 0:1], 1.0 / D, c_eps, [stt], [stt])
                    dve(lambda e: e.scalar_tensor_tensor(out=xnf[:], in0=xt[:], scalar=stt[:, 1:2], in1=gains[:, 1, :],
                                                         op0=ALU.mult, op1=ALU.mult), [xt, stt, gains], [xnf])
                    act(lambda e: e.copy(out=xh[:], in_=xnf[:]), [xnf], [xh])
                    dve(lambda e: e.tensor_tensor(out=xl[:], in0=xnf[:], in1=xh[:], op=ALU.subtract), [xnf, xh], [xl])
                    for src, dst3 in ((xh, xnT[:, :, n * 128:(n + 1) * 128]), (xl, xlT[:])):
                        pt_ = ps1()
                        ptb = pt_[:].bitcast(BF16)
                        for k in range(8):
                            pe(lambda e, k=k, src=src, ptb=ptb: e.transpose(out=ptb[:, k * 128:(k + 1) * 128],
                                                                            in_=src[:, k * 128:(k + 1) * 128],
                                                                            identity=identb[:]), [src, identb], [pt_])
                        if src is xh:
                            act(lambda e, dst3=dst3, ptb=ptb: e.copy(out=dst3, in_=v3(ptb[:, 0:1024], 8)), [pt_], [xnT])
                        else:
                            dve(lambda e, dst3=dst3, ptb=ptb: e.tensor_copy(out=dst3, in_=v3(ptb[:, 0:1024], 8)),
                                [pt_], [xlT])
                    pr = ps1()
                    terms = []
                    for k in range(8):
                        terms += [(xnT[:, k, n * 128:(n + 1) * 128], wrh[:, k, :], xnT),
                                  (xlT[:, k, :], wrh[:, k, :], xlT),
                                  (xnT[:, k, n * 128:(n + 1) * 128], wrl[:, k, :], xnT)]
                    for i, (lt, rh, lb) in enumerate(terms):
                        pe(lambda e, lt=lt, rh=rh, i=i: e.matmul(pr[:, 0:36], lhsT=lt, rhs=rh, start=(i == 0),
                                                                 stop=(i == len(terms) - 1)), [lb, wrh, wrl], [pr])
                    act(lambda e, n=n: e.copy(out=rl[:, n, :], in_=pr[:, 0:36]), [pr], [rl])
                R = lambda i, w: rtmp[:, i, :, 0:w]
                gl = rl[:, :, 0:4]
                el = rl[:, :, 4:36]
                glb, gmx, goh, gex, gsm, ggt = R(0, 4), R(1, 1), R(2, 4), R(3, 4), R(4, 1), R(5, 1)
                RB = [rtmp, t32t]
                dve(lambda e: e.tensor_tensor(out=glb, in0=gl, in1=bgb[:, None, :].broadcast_to([128, NT, 4]), op=ALU.add),
                    [rl, rvec], RB)
                dve(lambda e: e.tensor_reduce(out=gmx, in_=glb, axis=AX.X, op=ALU.max), RB, RB)
                dve(lambda e: e.tensor_tensor(out=goh, in0=glb, in1=gmx.broadcast_to([128, NT, 4]), op=ALU.is_ge), RB, RB)
                dve(lambda e: e.tensor_reduce(out=gmx, in_=gl, axis=AX.X, op=ALU.max), [rl], RB)
                dve(lambda e: e.tensor_tensor(out=gex, in0=gl, in1=gmx.broadcast_to([128, NT, 4]), op=ALU.subtract),
                    [rl, rtmp], RB)
                act(lambda e: e.activation(out=gex, in_=gex, func=AF.Exp), RB, RB)
                dve(lambda e: e.tensor_reduce(out=gsm, in_=gex, axis=AX.X, op=ALU.add), RB, RB)
                dve(lambda e: e.tensor_tensor(out=gex, in0=gex, in1=goh, op=ALU.mult), RB, RB)
                dve(lambda e: e.tensor_reduce(out=ggt, in_=gex, axis=AX.X, op=ALU.add), RB, RB)
                dve(lambda e: e.reciprocal(out=gsm, in_=gsm), RB, RB)
                dve(lambda e: e.tensor_tensor(out=ggt, in0=ggt, in1=gsm, op=ALU.mult), RB, RB)
                t32, elb8, el8 = t32t[:], R(7, 8), R(8, 8)
                t32v = t32.rearrange("p n (g e) -> p n g e", g=4)
                gohb = goh[:, :, :, None].broadcast_to([128, NT, 4, 8])
                dve(lambda e: e.tensor_tensor(out=t32v, in0=el.rearrange("p n (g e) -> p n g e", g=4), in1=gohb,
                                              op=ALU.mult), [rl, rtmp], RB)
                dve(lambda e: e.tensor_reduce(out=el8, in_=t32.rearrange("p n (g e) -> p n e g", g=4), axis=AX.X,
                                              op=ALU.add), RB, RB)
                dve(lambda e: e.tensor_tensor(out=t32v, in0=beb[:, None, :].broadcast_to([128, NT, 32]).rearrange(
                    "p n (g e) -> p n g e", g=4), in1=gohb, op=ALU.mult), [rvec, rtmp], RB)
                dve(lambda e: e.tensor_reduce(out=elb8, in_=t32.rearrange("p n (g e) -> p n e g", g=4), axis=AX.X,
                                              op=ALU.add), RB, RB)
                dve(lambda e: e.tensor_tensor(out=elb8, in0=elb8, in1=el8, op=ALU.add), RB, RB)
                emx, eex, oh1, oh2, p1, p2 = R(9, 1), R(10, 8), R(11, 8), R(6, 8), R(1, 1), R(4, 1)
                dve(lambda e: e.tensor_reduce(out=emx, in_=el8, axis=AX.X, op=ALU.max), RB, RB)
                dve(lambda e: e.tensor_tensor(out=eex, in0=el8, in1=emx.broadcast_to([128, NT, 8]), op=ALU.subtract), RB, RB)
                act(lambda e: e.activation(out=eex, in_=eex, func=AF.Exp), RB, RB)
                dve(lambda e: e.tensor_reduce(out=emx, in_=elb8, axis=AX.X, op=ALU.max), RB, RB)
                dve(lambda e: e.tensor_tensor(out=oh1, in0=elb8, in1=emx.broadcast_to([128, NT, 8]), op=ALU.is_ge), RB, RB)
                dve(lambda e: e.scalar_tensor_tensor(out=elb8, in0=oh1, scalar=-1e30, in1=elb8, op0=ALU.mult, op1=ALU.add),
                    RB, RB)
                dve(lambda e: e.tensor_reduce(out=emx, in_=elb8, axis=AX.X, op=ALU.max), RB, RB)
                dve(lambda e: e.tensor_tensor(out=oh2, in0=elb8, in1=emx.broadcast_to([128, NT, 8]), op=ALU.is_ge), RB, RB)
                dve(lambda e: e.tensor_tensor(out=oh1, in0=oh1, in1=eex, op=ALU.mult), RB, RB)
                dve(lambda e: e.tensor_tensor(out=oh2, in0=oh2, in1=eex, op=ALU.mult), RB, RB)
                dve(lambda e: e.tensor_tensor(out=oh1, in0=oh1, in1=oh2, op=ALU.add), RB, RB)
                dve(lambda e: e.tensor_reduce(out=p1, in_=oh1, axis=AX.X, op=ALU.add), RB, RB)
                dve(lambda e: e.reciprocal(out=p1, in_=p1), RB, RB)
                dve(lambda e: e.tensor_tensor(out=p1, in0=p1, in1=ggt, op=ALU.mult), RB, RB)
                dve(lambda e: e.tensor_tensor(out=oh1, in0=oh1, in1=p1.broadcast_to([128, NT, 8]), op=ALU.mult), RB, RB)
                dve(lambda e: e.tensor_tensor(out=gate[:].rearrange("p n (g e) -> p n g e", g=4),
                                              in0=oh1[:, :, None, :].broadcast_to([128, NT, 4, 8]), in1=gohb, op=ALU.mult),
                    RB, [gate])
                for ex in range(n_exp):
                    bi = ex % 2
                    wbl = web[bi]
                    mk.dma("pool", wbl[0][:].rearrange("p (k n) -> p k n", k=8),
                           w1_d[l, ex].rearrange("(k p) n -> p k n", p=128), writes=[wbl[0]])
                    mk.dma("pool", wbl[1][:].rearrange("p (k n) -> p k n", k=8),
                           w3_d[l, ex].rearrange("(k p) n -> p k n", p=128), writes=[wbl[1]])
                    mk.dma("pool", wbl[2][:].rearrange("p (k n) -> p k n", k=2),
                           w2_d[l, ex].rearrange("(k p) n -> p k n", p=128), writes=[wbl[2]])
                    for g in range(4):
                        hg = hh[g % 2]
                        for c in range(2):
                            p1_ = ps1()
                            p3_ = ps1()
                            for j, pp in ((0, p1_), (1, p3_)):
                                for k in range(8):
                                    pe(lambda e, j=j, pp=pp, k=k, c=c: e.matmul(
                                        pp[:], lhsT=wbl[j][:, k * 256 + c * 128:k * 256 + (c + 1) * 128],
                                        rhs=xnT[:, k, g * 512:(g + 1) * 512], start=(k == 0), stop=(k == 7)),
                                       [wbl[j], xnT], [pp])
                            s1t = s1[c]
                            act(lambda e, s1t=s1t, p1_=p1_: e.activation(out=s1t[:], in_=p1_[:], func=AF.Silu), [p1_], [s1t])
                            dve(lambda e, s1t=s1t, p3_=p3_, c=c, hg=hg: e.tensor_tensor(out=hg[:, c, :], in0=s1t[:],
                                                                                     in1=p3_[:], op=ALU.mult),
                                [s1t, p3_], [hg])
                        for t in range(4):
                            n = g * 4 + t
                            py_ = ps2()
                            for hf in range(2):
                                for c in range(2):
                                    pe(lambda e, hf=hf, c=c, t=t, hg=hg, py_=py_: e.matmul(
                                        py_[:, hf * 512:(hf + 1) * 512], lhsT=hg[:, c, t * 128:(t + 1) * 128],
                                        rhs=wbl[2][:, c * 1024 + hf * 512:c * 1024 + (hf + 1) * 512], start=(c == 0),
                                        stop=(c == 1)), [hg, wbl[2]], [py_])
                            xt = xres[n]
                            dve(lambda e, xt=xt, py_=py_, n=n, ex=ex: e.scalar_tensor_tensor(
                                out=xt[:], in0=py_[:], scalar=gate[:, n, ex:ex + 1], in1=xt[:], op0=ALU.mult, op1=ALU.add),
                                [py_, gate, xt], [xt])
                for n in range(NT):
                    if l < n_layers - 1:
                        mk.dma("sp", xs_d[n * 128:(n + 1) * 128, :], xres[n][:], reads=[xres[n]], writes=[xsb[n]],
                               stream="xs_st")
                    else:
                        mk.dma("sp", out_d[sq, n * 128:(n + 1) * 128, :], xres[n][:], reads=[xres[n]],
                               writes=[Buf("outd")], stream="out")

        mk.barrier()
        mk.emit()
    return nc, mk


def _host_inputs(inputs):
    f = lambda k: np.ascontiguousarray(np.asarray(inputs[k], dtype=np.float32))
    x = f("x")
    pos = np.ascontiguousarray(np.asarray(inputs["positions"]).astype(np.int32))
    pvec = np.zeros((L, 128, 64), np.float32)
    pvec[:, :, 0:8] = f("attn_norm").reshape(L, 8, 128).transpose(0, 2, 1)
    pvec[:, :, 8:16] = f("mix_out_norm").reshape(L, 8, 128).transpose(0, 2, 1)
    pvec[:, :, 16:24] = f("ffn_norm").reshape(L, 8, 128).transpose(0, 2, 1)
    pvec[:, :, 24:26] = f("a_q_norm").reshape(L, 2, 128).transpose(0, 2, 1)
    pvec[:, :, 26:27] = f("a_kv_norm").reshape(L, 1, 128).transpose(0, 2, 1)
    pvec[:, :, 27:31] = f("c_b_s").transpose(0, 2, 1)
    convw = np.zeros((L, 96, 40), np.float32)
    convw[:, :, 0:32] = f("m_conv_w").reshape(L, 4, 8, 96).transpose(0, 3, 2, 1).reshape(L, 96, 32)
    convw[:, :, 32:40] = f("m_conv_b").reshape(L, 8, 96).transpose(0, 2, 1)
    rrow = np.zeros((L, 496), np.float32)
    rrow[:, 0:8] = f("m_gate_bias")
    rrow[:, 8:104] = f("a_q_head_norm")
    rrow[:, 104:200] = f("a_k_head_norm")
    rrow[:, 200:456] = f("c_v_norm")
    rrow[:, 456:460] = f("b_group")
    rrow[:, 460:492] = f("b_expert")
    rvec = np.ascontiguousarray(np.broadcast_to(rrow[:, None, :], (L, 128, 496)))
    grow = np.concatenate([f("attn_norm"), f("ffn_norm")], axis=1)
    gvec = np.ascontiguousarray(np.broadcast_to(grow[:, None, :], (L, 128, 2048)))
    consts = np.zeros((128, 528), np.float32)
    p = np.arange(128)
    consts[:, 0:128] = np.eye(128, dtype=np.float32)
    consts[:, 128:256] = (p[:, None] <= p[None, :]).astype(np.float32)
    consts[:, 256:384] = (p[:, None] >= p[None, :]).astype(np.float32)
    consts[:, 384:512] = 1.0
    consts[:, 512:528] = (1.0 / (10000.0 ** (np.arange(0, 32, 2, dtype=np.float32) / 32.0))).astype(np.float32)[None, :]
    shared = {
        "w_in": f("w_in"), "w_out": f("w_out"), "a_w_uq": f("a_w_uq"), "a_w_ukv": f("a_w_ukv"), "c_w_s": f("c_w_s"),
        "w_group": f("w_group"), "w_expert": f("w_expert"), "w1": f("w1"), "w3": f("w3"), "w2": f("w2"),
        "pvec": pvec, "convw": convw, "rvec": rvec, "gvec": gvec, "consts": consts,
    }
    return x, pos, shared


def kernel(**inputs):
    x, pos, shared = _host_inputs(inputs)
    B = x.shape[0]
    per = B // NCORES
    nc, _ = build(n_seq=per, n_layers=L)
    in_maps = []
    for c in range(NCORES):
        m = dict(shared)
        m["x"] = np.ascontiguousarray(x[c * per:(c + 1) * per])
        pc = pos[c * per:(c + 1) * per].reshape(per, NT, 128).transpose(0, 2, 1)
        m["posT"] = np.ascontiguousarray(pc)
        in_maps.append(m)
    res = run_bass_kernel_spmd(nc, in_maps, core_ids=list(range(NCORES)))
    return np.concatenate([r["out"] for r in res.results], axis=0).astype(np.float32)
```

```python
import math
from contextlib import ExitStack
import numpy as np
import concourse.bass as bass
import concourse.mybir as mybir
from concourse.bass_utils import run_bass_kernel_spmd

F32 = mybir.dt.float32
BF16 = mybir.dt.bfloat16
I32 = mybir.dt.int32
AF = mybir.ActivationFunctionType
ALU = mybir.AluOpType
AX = mybir.AxisListType

NCORES = 8
S = 2048
D = 1024
NT = 16
L = 4
DIN = 2472
O_MQ, O_MK, O_MV, O_MO, O_MI, O_MF, O_CQ, O_CKV, O_KR, O_CU, O_CV = (
    0, 384, 768, 1152, 1536, 1540, 1544, 1800, 1928, 1960, 2216)
EPS = 1e-6


class Buf:
    __slots__ = ("name", "w", "r")

    def __init__(self, name):
        self.name = name
        self.w = None
        self.r = []


class Tl:
    def __init__(self, t, name):
        self.t = t
        self.b = Buf(name)

    def __getitem__(self, k):
        return self.t[k]


class _Rec:
    def __init__(self):
        self.call = None

    def __getattr__(self, name):
        def f(*a, **k):
            self.call = (name, a, k)
            return self
        return f


class MK:
    ENGS = ("pe", "dve", "act", "pool", "sp")

    def __init__(self, nc, stack):
        self.nc = nc
        self.stack = stack
        self.ops = {e: [] for e in self.ENGS}
        self.sems = {}
        self.count = {}
        self.seen = {e: {} for e in self.ENGS}
        for e in self.ENGS:
            self._sem("E_" + e)
        self.nops = 0
        self.stream = None

    def _emit(self, e, waits, fn, sem, inc):
        self.ops[e].append((waits, None, sem, inc))

    def emit(self):
        with self.nc.Block() as block:
            def mkbody(e):
                def body(eng):
                    for waits, call, sem, inc in self.ops[e]:
                        for s_, v in waits:
                            eng.wait_ge(s_, v)
                        if call is not None:
                            name, a, k = call
                            getattr(eng, name)(*a, **k).then_inc(sem, inc)
                return body
            block.tensor(mkbody("pe"))
            block.vector(mkbody("dve"))
            block.scalar(mkbody("act"))
            block.gpsimd(mkbody("pool"))
            block.sync(mkbody("sp"))

    def _sem(self, key):
        if key not in self.sems:
            self.sems[key] = self.stack.enter_context(self.nc.semaphore(key))
            self.count[key] = 0
        return self.sems[key]

    def sb(self, name, shape, dt):
        return Tl(self.stack.enter_context(self.nc.sbuf_tensor("sb_" + name, list(shape), dt)), name)

    def ps(self, name, shape, dt):
        return Tl(self.stack.enter_context(self.nc.psum_tensor(name, list(shape), dt)), name)

    def _waits(self, e, reads, writes):
        need = {}

        def add(ev, raw):
            if ev is None:
                return
            k, v, src = ev
            if src == e and (e == "pe" or not raw):
                return
            if need.get(k, 0) < v:
                need[k] = v
        for b in reads:
            add(b.w, True)
        for b in writes:
            add(b.w, False)
            for ev in b.r:
                add(ev, False)
        out = []
        seen = self.seen[e]
        for k, v in need.items():
            if seen.get(k, 0) < v:
                seen[k] = v
                out.append((self.sems[k], v))
        return out

    def _reg(self, ev, reads, writes):
        for b in reads:
            b.r.append(ev)
        for b in writes:
            b.w = ev
            b.r = []

    def op(self, e, fn, reads=(), writes=()):
        rec = _Rec()
        fn(rec)
        reads = [getattr(x, "b", x) for x in reads]
        writes = [getattr(x, "b", x) for x in writes]
        if self.stream is not None:
            self.stream.append((e, rec.call, reads, writes, None))
        else:
            self._op_call(e, rec.call, reads, writes, None)

    def dma(self, q, out, in_, reads=(), writes=(), stream=None):
        reads = [getattr(x, "b", x) for x in reads]
        writes = [getattr(x, "b", x) for x in writes]
        key = "D_" + (stream or (writes[0].name if writes else reads[0].name))
        call = ("dma_start", (), dict(out=out, in_=in_))
        if self.stream is not None:
            self.stream.append((q, call, reads, writes, key))
        else:
            self._op_call(q, call, reads, writes, key)

    def _op_call(self, e, call, reads, writes, dkey):
        waits = self._waits(e, reads, writes)
        if dkey is None:
            key, inc, src = "E_" + e, 1, e
        else:
            key, inc, src = dkey, 16, "dma"
            self._sem(key)
        self.count[key] += inc
        ev = (key, self.count[key], src)
        self._reg(ev, reads, writes)
        self.ops[e].append((waits, call, self.sems[key], inc))
        self.nops += 1

    def merge(self, lists):
        idx = [0] * len(lists)
        while True:
            best, bf = -1, 2.0
            for i, l_ in enumerate(lists):
                if idx[i] < len(l_):
                    f_ = idx[i] / len(l_)
                    if f_ < bf:
                        best, bf = i, f_
            if best < 0:
                break
            self._op_call(*lists[best][idx[best]])
            idx[best] += 1

    def barrier(self):
        for e in self.ENGS:
            waits = []
            for k, v in self.count.items():
                if v > 0 and self.seen[e].get(k, 0) < v and k != "E_" + e:
                    self.seen[e][k] = v
                    waits.append((self.sems[k], v))
            self._emit(e, waits, None, None, 0)


def build(n_seq=4, n_layers=4, dbg=False, do_moe=True, n_exp=32):
    nc = bass.Bass("TRN2", target_bir_lowering=False)

    def din(name, shape, dt=F32):
        return nc.dram_tensor(name, list(shape), dt, kind="ExternalInput").ap()

    x_d = din("x", [n_seq, S, D])
    pos_d = din("posT", [n_seq, 128, NT], I32)
    w_in_d = din("w_in", [L, D, DIN])
    w_out_d = din("w_out", [L, D, D])
    w_uq_d = din("a_w_uq", [L, 256, 576])
    w_ukv_d = din("a_w_ukv", [L, 128, 768])
    c_ws_d = din("c_w_s", [L, 4, 128, 128])
    w_g_d = din("w_group", [L, D, 4])
    w_e_d = din("w_expert", [L, D, 32])
    w1_d = din("w1", [L, 32, D, 256])
    w3_d = din("w3", [L, 32, D, 256])
    w2_d = din("w2", [L, 32, 256, D])
    pv_d = din("pvec", [L, 128, 64])
    cw_d = din("convw", [L, 96, 40])
    rv_d = din("rvec", [L, 128, 496])
    gv_d = din("gvec", [L, 128, 2048])
    c_d = din("consts", [128, 528])
    out_d = nc.dram_tensor("out", [n_seq, S, D], F32, kind="ExternalOutput").ap()
    dbg_d = {}
    if dbg:
        dbg_d["y"] = nc.dram_tensor("dbg_y", [S, D], F32, kind="ExternalOutput").ap()
        dbg_d["x1"] = nc.dram_tensor("dbg_x1", [S, D], F32, kind="ExternalOutput").ap()

    st = ExitStack()
    with st:
        mk = MK(nc, st)
        dve = lambda fn, r, w: mk.op("dve", fn, r, w)
        act = lambda fn, r, w: mk.op("act", fn, r, w)
        pe = lambda fn, r, w: mk.op("pe", fn, r, w)
        pool = lambda fn, r, w: mk.op("pool", fn, r, w)

        SZ = {F32: 4, BF16: 2, I32: 4}
        ARENA_F = 47104
        arena_t = st.enter_context(nc.sbuf_tensor("arena", [128, ARENA_F], F32))

        class Arena:
            def __init__(self):
                self.off = 0

            def __call__(self, name, shape, dt):
                free = 1
                for d_ in shape[1:]:
                    free *= d_
                n4 = (free * SZ[dt] + 3) // 4
                assert self.off + n4 <= ARENA_F, (name, self.off, n4)
                v = arena_t[0:shape[0], self.off:self.off + n4]
                self.off += n4
                if dt != F32:
                    v = v.bitcast(dt)
                v = v[:, 0:free]
                if len(shape) > 2:
                    names = "abcd"[:len(shape) - 1]
                    kw = {names[i]: shape[1 + i] for i in range(len(shape) - 1)}
                    v = v.rearrange("p (" + " ".join(names) + ") -> p " + " ".join(names), **kw)
                return Tl(v, name)

        cst = mk.sb("cst", [128, 528], F32)
        identb = mk.sb("identb", [128, 128], BF16)
        mask4b = mk.sb("mask4b", [128, 4, 128], F32)
        sc = mk.sb("sc", [128, 8], F32)
        cossin = mk.sb("cossin", [128, 2, NT, 16], F32)
        pvec = mk.sb("pvec", [128, 64], F32)
        rvec = mk.sb("rvec", [128, 496], F32)
        convw = mk.sb("convw", [96, 40], F32)
        posi = mk.sb("posi", [128, NT], I32)
        posf = mk.sb("posf", [128, NT], F32)
        stt = mk.sb("stt", [128, 8], F32)
        hst = mk.sb("hst", [128, 3, 14], F32)
        g_t = mk.sb("g_t", [128, 8, 4], F32)
        sttB = mk.sb("sttB", [128, 8], F32)
        gains = mk.sb("gains", [128, 2, D], F32)
        A = Arena()
        xtile = [A(f"xtile{i}", [128, D], F32) for i in range(2)]
        convd = A("convd", [96, 8, 4, 96], BF16)
        win_b = A("win_b", [128, 8, DIN], BF16)
        wout_b = A("wout_b", [128, 8, D], BF16)
        wuq_b = A("wuq_b", [128, 2, 576], BF16)
        wukv_b = A("wukv_b", [128, 768], BF16)
        wsT_b = A("wsT_b", [128, 4, 128], BF16)
        stage2 = A("stage2", [128, 1152], F32)
        KT = A("KT", [96, 6, S], BF16)
        Vaug = A("Vaug", [128, NT, 6, 65], BF16)
        junk = A("junk", [128, D], F32)
        hb = A("hb", [128, D], BF16)
        hTt = A("hTt", [128, 8, 128], BF16)
        xqk = A("xqk", [96, 8, 131], BF16)
        esil = A("esil", [96, 8, 128], F32)
        qkT = A("qkT", [96, 8, 128], BF16)
        Sm = A("Sm", [128, 4, 128], BF16)
        ktok = A("ktok", [128, 4, 96], BF16)
        Vt = A("Vt", [128, 4, 97], BF16)
        Cf = A("Cf", [96, 4, 97], F32)
        Cb = A("Cb", [96, 4, 97], BF16)
        og = A("og", [128, 384], F32)
        hm = A("hm", [128, 4, 96], F32)
        cT = A("cT", [128, 3, 128], BF16)
        qs = A("qs", [128, 6, 96], F32)
        ksv = A("ksv", [128, 6, 128], F32)
        kr = A("kr", [128, 6, 32], F32)
        krope = A("krope", [128, 32], F32)
        rt = A("rt", [128, 4, 6, 16], F32)
        qb = A("qb", [128, 6, 96], BF16)
        kb = A("kb", [128, 6, 96], BF16)
        QT = A("QT", [96, 6, 128], BF16)
        PT = [A(f"PT{i}", [128, 4, 128], BF16) for i in range(3)]
        ha = A("ha", [128, 6, 64], F32)
        ga = A("ga", [128, 512], F32)
        gb_ = A("gb_", [128, 512], F32)
        guv = A("guv", [128, 512], F32)
        vn = A("vn", [128, 256], BF16)
        hc = A("hc", [128, 256], F32)
        ycat = A("ycat", [128, D], BF16)
        yT = A("yT", [128, 8, 128], BF16)
        junkB = A("junkB", [128, D], F32)
        mixer_bytes = A.off * 4
        junk_mix = junk
        A = Arena()
        xres = [A(f"xres{n}", [128, D], F32) for n in range(NT)]
        xnT = A("xnT", [128, 8, S], BF16)
        xnf = A("xnf", [128, D], F32)
        xh = A("xh", [128, D], BF16)
        xl = A("xl", [128, D], BF16)
        xlT = A("xlT", [128, 8, 128], BF16)
        wrh = A("wrh", [128, 8, 36], BF16)
        wrl = A("wrl", [128, 8, 36], BF16)
        wr = A("wr", [128, 8, 36], F32)
        rl = A("rl", [128, NT, 36], F32)
        rtmp = A("rtmp", [128, 12, NT, 8], F32)
        t32t = A("t32t", [128, NT, 32], F32)
        ropet = A("ropet", [128, 7, NT, 16], F32)
        gate = A("gate", [128, NT, 32], F32)
        web = [[A(f"web{i}_{j}", [128, 2048], BF16) for j in range(3)] for i in range(2)]
        s1 = [A(f"s1_{i}", [128, 512], BF16) for i in range(2)]
        hh = [A(f"hh{i}", [128, 2, 512], BF16) for i in range(2)]
        mjunk = A("mjunk", [128, D], F32)
        moe_bytes = A.off * 4
        xs_d = nc.dram_tensor("xs_scratch", [S, D], F32, kind="Internal").ap()
        xsb = [Buf(f"xs{n}") for n in range(NT)]

        PS = [mk.ps(f"ps{i}", [128, 512], F32) for i in range(4)]
        PP = [mk.ps(f"pp{i}", [128, 1024], F32) for i in range(2)]
        ctr = {"s": 0, "p": 0}

        PPh = [Tl(PP[i][:, j * 512:(j + 1) * 512], f"pp{i}h{j}") for i in range(2) for j in range(2)]
        pools = {"A": [PS, 0], "B": [PPh, 0]}
        cur = {"pool": "A"}

        def ps1(exclude=None):
            pl = pools[cur["pool"]]
            pl[1] += 1
            if pl[0][pl[1] % 4] is exclude:
                pl[1] += 1
            return pl[0][pl[1] % 4]

        def ps2():
            ctr["p"] += 1
            return PP[ctr["p"] % 2]

        def v3(ap, a):
            return ap.rearrange("p (a b) -> p a b", a=a)

        mk.dma("sp", cst[:], c_d, writes=[cst])
        ident = cst[:, 0:128]
        maskle = cst[:, 128:256]
        maskge = cst[:, 256:384]
        ones = cst[:, 384:512]
        invf = cst[:, 512:528]
        dve(lambda e: e.tensor_copy(out=identb[:], in_=ident), [cst], [identb])
        for h in range(4):
            dve(lambda e, h=h: e.tensor_copy(out=mask4b[:, h, :], in_=maskle), [cst], [mask4b])
        pool(lambda e: e.memset(sc[:, 0:1], EPS), [], [sc])
        pool(lambda e: e.memset(sc[:, 1:2], 1.0), [], [sc])
        pool(lambda e: e.memset(sc[:, 2:3], EPS / 4), [], [sc])
        pool(lambda e: e.memset(sc[:, 3:4], 0.0), [], [sc])
        c_eps, c_one, c_eps4 = sc[:, 0:1], sc[:, 1:2], sc[:, 2:3]

        def rsqrt(out, in_, scale, bias, r, w):
            act(lambda e: e.activation(out=out, in_=in_, func=AF.Ln, scale=scale, bias=bias), r + [sc], w)
            act(lambda e: e.activation(out=out, in_=out, func=AF.Exp, scale=-0.5), w, w)

        for sq in range(n_seq):
            mk.barrier()
            mk.dma("sp", posi[:], pos_d[sq], writes=[posi])
            dve(lambda e: e.tensor_copy(out=posf[:], in_=posi[:]), [posi], [posf])
            ang = ropet[:, 0, :, :]
            dve(lambda e: e.tensor_tensor(out=ang, in0=posf[:, :, None].broadcast_to([128, NT, 16]),
                                          in1=invf[:, None, :].broadcast_to([128, NT, 16]), op=ALU.mult),
                [posf, cst], [ropet])
            for ci, shift in ((0, math.pi / 2), (1, 0.0)):
                a2 = ropet[:, 1, :, :]
                ki = ropet[:, 2, :, :]
                kf = ropet[:, 3, :, :]
                m1 = ropet[:, 4, :, :]
                RP = [ropet]
                dve(lambda e: e.tensor_scalar(out=a2, in0=ang, scalar1=shift, scalar2=None, op0=ALU.add), RP, RP)
                dve(lambda e: e.tensor_scalar(out=ki.bitcast(I32), in0=a2, scalar1=1.0 / (2 * math.pi), scalar2=None,
                                              op0=ALU.mult), RP, RP)
                dve(lambda e: e.tensor_copy(out=kf, in_=ki.bitcast(I32)), RP, RP)
                dve(lambda e: e.scalar_tensor_tensor(out=a2, in0=kf, scalar=-2 * math.pi, in1=a2, op0=ALU.mult,
                                                     op1=ALU.add), RP, RP)
                dve(lambda e: e.tensor_scalar(out=m1, in0=a2, scalar1=math.pi, scalar2=-2 * math.pi, op0=ALU.is_gt,
                                              op1=ALU.mult), RP, RP)
                dve(lambda e: e.tensor_tensor(out=a2, in0=a2, in1=m1, op=ALU.add), RP, RP)
                dve(lambda e: e.tensor_scalar(out=m1, in0=a2, scalar1=-math.pi, scalar2=2 * math.pi, op0=ALU.is_lt,
                                              op1=ALU.mult), RP, RP)
                dve(lambda e: e.tensor_tensor(out=a2, in0=a2, in1=m1, op=ALU.add), RP, RP)
                act(lambda e: e.activation(out=cossin[:, ci, :, :], in_=a2, func=AF.Sin), RP, [cossin])

            for l in range(n_layers):
                mk.barrier()
                junk = junk_mix
                pool(lambda e: e.memset(Vaug[:], 1.0), [], [Vaug])
                mk.dma("sp", pvec[:], pv_d[l], writes=[pvec])
                mk.dma("sp", rvec[:], rv_d[l], writes=[rvec])
                mk.dma("sp", convw[:], cw_d[l], writes=[convw])
                mk.dma("sp", gains[:].rearrange("p a b -> p (a b)"), gv_d[l], writes=[gains])
                for kc in range(8):
                    mk.dma("pool", win_b[:, kc, :], w_in_d[l, kc * 128:(kc + 1) * 128, :], writes=[win_b])
                for kc in range(8):
                    mk.dma("sp", stage2[:, 0:D], w_out_d[l, kc * 128:(kc + 1) * 128, :], writes=[stage2])
                    act(lambda e, kc=kc: e.activation(out=wout_b[:, kc, :], in_=stage2[:, 0:D], func=AF.Identity,
                                                      scale=pvec[:, 8 + kc:9 + kc]), [stage2, pvec], [wout_b])
                for c in range(2):
                    mk.dma("sp", stage2[:, 0:576], w_uq_d[l, c * 128:(c + 1) * 128, :], writes=[stage2])
                    act(lambda e, c=c: e.activation(out=wuq_b[:, c, :], in_=stage2[:, 0:576], func=AF.Identity,
                                                    scale=pvec[:, 24 + c:25 + c]), [stage2, pvec], [wuq_b])
                mk.dma("sp", stage2[:, 0:768], w_ukv_d[l], writes=[stage2])
                act(lambda e: e.activation(out=wukv_b[:], in_=stage2[:, 0:768], func=AF.Identity, scale=pvec[:, 26:27]),
                    [stage2, pvec], [wukv_b])
                mk.dma("sp", stage2[:, 0:512].rearrange("p (g s) -> p g s", g=4),
                       c_ws_d[l].rearrange("g t s -> t g s"), writes=[stage2])
                pool(lambda e: e.tensor_tensor(out=hb[:, 0:512].rearrange("p (g s) -> p g s", g=4),
                                               in0=stage2[:, 0:512].rearrange("p (g s) -> p g s", g=4),
                                               in1=maskge[:, None, :].broadcast_to([128, 4, 128]), op=ALU.mult),
                     [stage2, cst], [hb])
                pw = ps1()
                pwb = pw[:].bitcast(BF16)
                for g in range(4):
                    pe(lambda e, g=g: e.transpose(out=pwb[:, g * 128:(g + 1) * 128], in_=hb[:, g * 128:(g + 1) * 128],
                                                  identity=identb[:]), [hb, identb], [pw])
                dve(lambda e: e.tensor_copy(out=wsT_b[:].rearrange("p g t -> p (g t)"), in_=pwb[:, 0:512]), [pw], [wsT_b])
                for ch in range(8):
                    for j in range(4):
                        pool(lambda e, ch=ch, j=j: e.tensor_scalar(out=convd[:, ch, j, :], in0=cst[0:96, 0:96],
                                                                  scalar1=convw[:, ch * 4 + j:ch * 4 + j + 1],
                                                                  scalar2=None, op0=ALU.mult), [cst, convw], [convd])
                pool(lambda e: e.tensor_scalar(out=rvec[:, 0:4], in0=rvec[:, 0:4], scalar1=-0.5 * math.log(96.0),
                                               scalar2=None, op0=ALU.add), [rvec], [rvec])
                pool(lambda e: e.memset(Cf[:], 0.0), [], [Cf])
                pool(lambda e: e.memset(Cb[:], 0.0), [], [Cb])
                pool(lambda e: e.memset(xqk[:], 0.0), [], [xqk])
                gbias = rvec[:, 0:8]
                qhg = rvec[:, 8:104]
                khg = rvec[:, 104:200]
                cvg = rvec[:, 200:456]
                bgb = rvec[:, 456:460]
                beb = rvec[:, 460:492]
                bsb = pvec[:, 27:31]
                convb = convw[:, 32:40]

                for n in range(NT):
                    xt = xtile[n % 2]
                    tsl = slice(n * 128, (n + 1) * 128)
                    if l == 0:
                        mk.dma("sp", xt[:], x_d[sq, tsl, :], writes=[xt])
                    else:
                        mk.dma("sp", xt[:], xs_d[tsl, :], reads=[xsb[n]], writes=[xt])
                    act(lambda e: e.activation(out=junk[:], in_=xt[:], func=AF.Square, accum_out=stt[:, 0:1]),
                        [xt], [junk, stt])
                    rsqrt(stt[:, 1:2], stt[:, 0:1], 1.0 / D, c_eps, [stt], [stt])
                    dve(lambda e: e.scalar_tensor_tensor(out=hb[:], in0=xt[:], scalar=stt[:, 1:2], in1=gains[:, 0, :],
                                                         op0=ALU.mult, op1=ALU.mult), [xt, stt, gains], [hb])
                    p0 = ps1()
                    p0b = p0[:].bitcast(BF16)
                    for k in range(8):
                        pe(lambda e, k=k: e.transpose(out=p0b[:, k * 128:(k + 1) * 128], in_=hb[:, k * 128:(k + 1) * 128],
                                                      identity=identb[:]), [hb, identb], [p0])
                    act(lambda e: e.copy(out=hTt[:].rearrange("p k t -> p (k t)"), in_=p0b[:, 0:1024]), [p0], [hTt])

                    L1, L2 = [], []
                    mk.stream = L1
                    cur["pool"] = "A"
                    def proj_tok(c0, c1):
                        p = ps1()
                        for k in range(8):
                            pe(lambda e, k=k: e.matmul(p[:, 0:c1 - c0], lhsT=hTt[:, k, :], rhs=win_b[:, k, c0:c1],
                                                       start=(k == 0), stop=(k == 7)), [hTt, win_b], [p])
                        return p
                    pT2 = proj_tok(O_MO, O_MO + 392)
                    ipre, lf, einvb, ks, a_t, rec = (g_t[:, i, :] for i in range(6))
                    dve(lambda e: e.tensor_tensor(out=ipre, in0=pT2[:, 384:388], in1=gbias[:, 0:4], op=ALU.add),
                        [pT2, rvec], [g_t])
                    dve(lambda e: e.tensor_tensor(out=lf, in0=pT2[:, 388:392], in1=gbias[:, 4:8], op=ALU.add),
                        [pT2, rvec], [g_t])
                    act(lambda e: e.activation(out=lf, in_=lf, func=AF.Exp, scale=-1.0), [g_t], [g_t])
                    act(lambda e: e.activation(out=lf, in_=lf, func=AF.Ln, bias=c_one), [g_t, sc], [g_t])
                    pg = ps1()
                    pe(lambda e: e.matmul(pg[:, 0:4], lhsT=maskle, rhs=lf, start=True, stop=True), [cst, g_t], [pg])
                    pe(lambda e: e.matmul(pg[:, 4:8], lhsT=ones, rhs=lf, start=True, stop=True), [cst, g_t], [pg])
                    act(lambda e: e.activation(out=einvb, in_=pg[:, 0:4], func=AF.Exp), [pg], [g_t])
                    dve(lambda e: e.tensor_tensor(out=ks, in0=ipre, in1=pg[:, 0:4], op=ALU.add), [pg, g_t], [g_t])
                    act(lambda e: e.activation(out=ks, in_=ks, func=AF.Exp), [g_t], [g_t])
                    act(lambda e: e.activation(out=a_t, in_=pg[:, 4:8], func=AF.Exp, scale=-1.0), [pg], [g_t])
                    act(lambda e: e.activation(out=og[:], in_=pT2[:, 0:384], func=AF.Exp, scale=-1.0), [pT2], [og])
                    act(lambda e: e.activation(out=og[:], in_=og[:], func=AF.Ln, bias=c_one), [og, sc], [og])
                    act(lambda e: e.activation(out=og[:], in_=og[:], func=AF.Exp, scale=-1.0), [og], [og])
                    pT1 = proj_tok(O_MV, O_MV + 384)
                    for h in range(4):
                        dve(lambda e, h=h: e.tensor_scalar(out=Vt[:, h, 0:96], in0=pT1[:, h * 96:(h + 1) * 96],
                                                           scalar1=ks[:, h:h + 1], scalar2=None, op0=ALU.mult),
                            [pT1, g_t], [Vt])
                    dve(lambda e: e.tensor_copy(out=Vt[:, :, 96], in_=ks), [g_t], [Vt])
                    pq = [ps1(), ps1()]
                    for ch in range(8):
                        for k in range(8):
                            pe(lambda e, ch=ch, k=k: e.matmul(pq[ch // 4][0:96, (ch % 4) * 128:(ch % 4 + 1) * 128],
                                                              lhsT=win_b[:, k, ch * 96:(ch + 1) * 96], rhs=hTt[:, k, :],
                                                              start=(k == 0), stop=(k == 7)), [hTt, win_b], [pq[ch // 4]])
                    for hq in range(2):
                        act(lambda e, hq=hq: e.copy(out=xqk[:, hq * 4:hq * 4 + 4, 3:131], in_=v3(pq[hq][0:96, :], 4)),
                            [pq[hq]], [xqk])
                    pc = [ps1(), ps1()]
                    for ch in range(8):
                        for j in range(4):
                            pe(lambda e, ch=ch, j=j: e.matmul(pc[ch // 4][0:96, (ch % 4) * 128:(ch % 4 + 1) * 128],
                                                              lhsT=convd[:, ch, j, :], rhs=xqk[:, ch, j:j + 128],
                                                              start=(j == 0), stop=(j == 3)), [convd, xqk], [pc[ch // 4]])
                    for ch in range(8):
                        act(lambda e, ch=ch: e.activation(out=esil[:, ch, :],
                                                          in_=pc[ch // 4][0:96, (ch % 4) * 128:(ch % 4 + 1) * 128],
                                                          func=AF.Identity, bias=convb[:, ch:ch + 1]),
                            [pc[ch // 4], convw], [esil])
                    act(lambda e: e.activation(out=junk[0:96, :], in_=esil[:].rearrange("p a b -> p (a b)"),
                                               func=AF.Exp, scale=-1.0), [esil], [junk])
                    act(lambda e: e.activation(out=junk[0:96, :], in_=junk[0:96, :], func=AF.Ln, bias=c_one[0:96, :]),
                        [junk, sc], [junk])
                    act(lambda e: e.activation(out=junk[0:96, :], in_=junk[0:96, :], func=AF.Exp, scale=-1.0),
                        [junk], [junk])
                    dve(lambda e: e.tensor_tensor(out=qkT[:].rearrange("p a b -> p (a b)"),
                                                  in0=esil[:].rearrange("p a b -> p (a b)"), in1=junk[0:96, :],
                                                  op=ALU.mult), [esil, junk], [qkT])
                    pool(lambda e: e.tensor_copy(out=xqk[:, :, 0:3], in_=xqk[:, :, 128:131]), [xqk], [xqk])

                    pS = ps1()
                    for h in range(4):
                        pe(lambda e, h=h: e.matmul(pS[:, h * 128:(h + 1) * 128], lhsT=qkT[:, 4 + h, :], rhs=qkT[:, h, :],
                                                   start=True, stop=True), [qkT], [pS])
                    dve(lambda e: e.tensor_tensor(out=Sm[:], in0=v3(pS[:], 4), in1=mask4b[:], op=ALU.mult),
                        [pS, mask4b], [Sm])
                    pk = ps1()
                    pkb = pk[:].bitcast(BF16)
                    for h in range(4):
                        pe(lambda e, h=h: e.transpose(out=pkb[:, h * 96:(h + 1) * 96], in_=qkT[:, 4 + h, :],
                                                      identity=identb[0:96, 0:96]), [qkT, identb], [pk])
                    act(lambda e: e.copy(out=ktok[:].rearrange("p a b -> p (a b)"), in_=pkb[:, 0:384]), [pk], [ktok])
                    pN = ps1()
                    for h in range(4):
                        pe(lambda e, h=h: e.matmul(pN[:, h * 97:(h + 1) * 97], lhsT=Sm[:, h, :], rhs=Vt[:, h, :],
                                                   start=True, stop=False), [Sm, Vt], [pN])
                        pe(lambda e, h=h: e.matmul(pN[:, h * 97:(h + 1) * 97], lhsT=qkT[:, h, :], rhs=Cb[:, h, :],
                                                   start=False, stop=True), [qkT, Cb], [pN])
                    pD = ps1()
                    for h in range(4):
                        pe(lambda e, h=h: e.matmul(pD[0:96, h * 97:(h + 1) * 97], lhsT=ktok[:, h, :], rhs=Vt[:, h, :],
                                                   start=True, stop=True), [ktok, Vt], [pD])
                    dve(lambda e: e.tensor_tensor(out=Cf[:].rearrange("p a b -> p (a b)"),
                                                  in0=Cf[:].rearrange("p a b -> p (a b)"), in1=pD[0:96, 0:388],
                                                  op=ALU.add), [Cf, pD], [Cf])
                    for h in range(4):
                        dve(lambda e, h=h: e.tensor_scalar(out=Cf[:, h, :], in0=Cf[:, h, :], scalar1=a_t[0:96, h:h + 1],
                                                           scalar2=None, op0=ALU.mult), [Cf, g_t], [Cf])
                    act(lambda e: e.copy(out=Cb[:], in_=Cf[:]), [Cf], [Cb])
                    pNv = pN[:, 0:388].rearrange("p (a b) -> p a b", a=4)
                    act(lambda e: e.activation(out=rec, in_=pNv[:, :, 96], func=AF.Abs), [pN], [g_t])
                    dve(lambda e: e.tensor_tensor(out=rec, in0=rec, in1=einvb, op=ALU.max), [g_t], [g_t])
                    dve(lambda e: e.reciprocal(out=rec, in_=rec), [g_t], [g_t])
                    dve(lambda e: e.tensor_tensor(out=hm[:], in0=pNv[:, :, 0:96],
                                                  in1=rec[:, :, None].broadcast_to([128, 4, 96]), op=ALU.mult),
                        [pN, g_t], [hm])
                    dve(lambda e: e.tensor_tensor(out=hm[:].rearrange("p a b -> p (a b)"),
                                                  in0=hm[:].rearrange("p a b -> p (a b)"), in1=og[:], op=ALU.mult),
                        [hm, og], [hm])

                    mk.stream = L2
                    cur["pool"] = "B"
                    pT3 = proj_tok(O_CQ, O_CQ + 416)
                    act(lambda e: e.activation(out=junkB[:, 0:256], in_=pT3[:, 0:256], func=AF.Square,
                                               accum_out=sttB[:, 2:3]), [pT3], [junkB, sttB])
                    act(lambda e: e.activation(out=junkB[:, 256:384], in_=pT3[:, 256:384], func=AF.Square,
                                               accum_out=sttB[:, 3:4]), [pT3], [junkB, sttB])
                    act(lambda e: e.copy(out=krope[:], in_=pT3[:, 384:416]), [pT3], [krope])
                    act(lambda e: e.activation(out=junkB[:, 384:416], in_=pT3[:, 384:416], func=AF.Square,
                                               accum_out=sttB[:, 6:7]), [pT3], [junkB, sttB])
                    rsqrt(sttB[:, 4:5], sttB[:, 2:3], 1.0 / 256, c_eps, [sttB], [sttB])
                    rsqrt(sttB[:, 5:6], sttB[:, 3:4], 1.0 / 128, c_eps, [sttB], [sttB])
                    pC = ps1()
                    for c in range(3):
                        for k in range(8):
                            pe(lambda e, c=c, k=k: e.matmul(pC[:, c * 128:(c + 1) * 128],
                                                            lhsT=win_b[:, k, O_CQ + c * 128:O_CQ + (c + 1) * 128],
                                                            rhs=hTt[:, k, :], start=(k == 0), stop=(k == 7)),
                               [hTt, win_b], [pC])
                    act(lambda e: e.copy(out=cT[:].rearrange("p a b -> p (a b)"), in_=pC[:, 0:384]), [pC], [cT])
                    pQ = [ps1(), ps1()]
                    for j in range(2):
                        for c in range(2):
                            pe(lambda e, j=j, c=c: e.matmul(pQ[j][:, 0:288], lhsT=cT[:, c, :],
                                                            rhs=wuq_b[:, c, j * 288:(j + 1) * 288],
                                                            start=(c == 0), stop=(c == 1)), [cT, wuq_b], [pQ[j]])
                    pK = [ps1(), ps1()]
                    for j in range(2):
                        pe(lambda e, j=j: e.matmul(pK[j][:, 0:384], lhsT=cT[:, 2, :],
                                                   rhs=wukv_b[:, j * 384:(j + 1) * 384], start=True, stop=True),
                           [cT, wukv_b], [pK[j]])
                    for j in range(2):
                        dve(lambda e, j=j: e.tensor_scalar(out=qs[:, 3 * j:3 * j + 3, :].rearrange("p a b -> p (a b)"),
                                                           in0=pQ[j][:, 0:288], scalar1=sttB[:, 4:5],
                                                           scalar2=None, op0=ALU.mult), [pQ[j], sttB], [qs])
                        dve(lambda e, j=j: e.tensor_scalar(out=ksv[:, 3 * j:3 * j + 3, :].rearrange("p a b -> p (a b)"),
                                                           in0=pK[j][:, 0:384], scalar1=sttB[:, 5:6],
                                                           scalar2=None, op0=ALU.mult), [pK[j], sttB], [ksv])
                    jq = junkB[:, 0:576].rearrange("p (a b) -> p a b", a=6)
                    act(lambda e: e.activation(out=jq, in_=qs[:], func=AF.Square), [qs], [junkB])
                    dve(lambda e: e.reduce_sum(out=hst[:, 0, 0:6], in_=jq, axis=AX.X), [junkB], [hst])
                    rsqrt(hst[:, 1, 0:6], hst[:, 0, 0:6], 1.0 / 96, c_eps, [hst], [hst])
                    dve(lambda e: e.tensor_tensor(out=qs[:], in0=qs[:], in1=hst[:, 1, 0:6, None].broadcast_to([128, 6, 96]),
                                                  op=ALU.mult), [qs, hst], [qs])
                    dve(lambda e: e.tensor_tensor(out=qs[:], in0=qs[:], in1=qhg[:, None, :].broadcast_to([128, 6, 96]),
                                                  op=ALU.mult), [qs, rvec], [qs])
                    jk = junkB[:, 0:384].rearrange("p (a b) -> p a b", a=6)
                    act(lambda e: e.activation(out=jk, in_=ksv[:, :, 0:64], func=AF.Square), [ksv], [junkB])
                    dve(lambda e: e.reduce_sum(out=hst[:, 0, 6:12], in_=jk, axis=AX.X), [junkB], [hst])
                    dve(lambda e: e.tensor_scalar(out=hst[:, 0, 6:12], in0=hst[:, 0, 6:12], scalar1=sttB[:, 6:7],
                                                  scalar2=None, op0=ALU.add), [hst, sttB], [hst])
                    rsqrt(hst[:, 1, 6:12], hst[:, 0, 6:12], 1.0 / 96, c_eps, [hst], [hst])
                    rk = hst[:, 1, 6:12]
                    dve(lambda e: e.tensor_tensor(out=jk, in0=ksv[:, :, 0:64], in1=rk[:, :, None].broadcast_to([128, 6, 64]),
                                                  op=ALU.mult), [ksv, hst], [junkB])
                    dve(lambda e: e.tensor_tensor(out=kb[:, :, 0:64], in0=jk,
                                                  in1=khg[:, None, 0:64].broadcast_to([128, 6, 64]), op=ALU.mult),
                        [junkB, rvec], [kb])
                    dve(lambda e: e.tensor_tensor(out=kr[:], in0=krope[:, None, :].broadcast_to([128, 6, 32]),
                                                  in1=rk[:, :, None].broadcast_to([128, 6, 32]), op=ALU.mult),
                        [krope, hst], [kr])
                    dve(lambda e: e.tensor_tensor(out=kr[:], in0=kr[:], in1=khg[:, None, 64:96].broadcast_to([128, 6, 32]),
                                                  op=ALU.mult), [kr, rvec], [kr])
                    cosb = cossin[:, 0, n, None, :].broadcast_to([128, 6, 16])
                    sinb = cossin[:, 1, n, None, :].broadcast_to([128, 6, 16])

                    def rope(src1, src2, dst1, dst2, rb, wb):
                        t = [rt[:, i, :, :] for i in range(4)]
                        dve(lambda e: e.tensor_tensor(out=t[0], in0=src1, in1=cosb, op=ALU.mult), rb + [cossin], [rt])
                        dve(lambda e: e.tensor_tensor(out=t[1], in0=src2, in1=sinb, op=ALU.mult), rb + [cossin], [rt])
                        dve(lambda e: e.tensor_tensor(out=t[2], in0=src1, in1=sinb, op=ALU.mult), rb + [cossin], [rt])
                        dve(lambda e: e.tensor_tensor(out=t[3], in0=src2, in1=cosb, op=ALU.mult), rb + [cossin], [rt])
                        dve(lambda e: e.tensor_tensor(out=dst1, in0=t[0], in1=t[1], op=ALU.subtract), [rt], wb)
                        dve(lambda e: e.tensor_tensor(out=dst2, in0=t[2], in1=t[3], op=ALU.add), [rt], wb)
                    rope(qs[:, :, 64:80], qs[:, :, 80:96], qb[:, :, 64:80], qb[:, :, 80:96], [qs], [qb])
                    dve(lambda e: e.tensor_copy(out=qb[:, :, 0:64], in_=qs[:, :, 0:64]), [qs], [qb])
                    rope(kr[:, :, 0:16], kr[:, :, 16:32], kb[:, :, 64:80], kb[:, :, 80:96], [kr], [kb])
                    act(lambda e: e.copy(out=Vaug[:, n, :, 0:64], in_=ksv[:, :, 64:128]), [ksv], [Vaug])
                    pqt = ps1()
                    pqtb = pqt[:].bitcast(BF16)
                    for h in range(6):
                        pe(lambda e, h=h: e.transpose(out=pqtb[0:96, h * 128:(h + 1) * 128], in_=qb[:, h, :],
                                                      identity=identb[:]), [qb, identb], [pqt])
                    act(lambda e: e.copy(out=QT[:].rearrange("p a b -> p (a b)"), in_=pqtb[0:96, 0:768]), [pqt], [QT])
                    pkt = ps1()
                    pktb = pkt[:].bitcast(BF16)
                    for h in range(6):
                        pe(lambda e, h=h: e.transpose(out=pktb[0:96, h * 128:(h + 1) * 128], in_=kb[:, h, :],
                                                      identity=identb[:]), [kb, identb], [pkt])
                    act(lambda e: e.copy(out=KT[:, :, tsl], in_=pktb[0:96, 0:768].rearrange("p (a b) -> p a b", a=6)),
                        [pkt], [KT])
                    pO = ps1()
                    groups = [(h, j0, min(4, n + 1 - j0)) for h in range(6) for j0 in range(0, n + 1, 4)]

                    def att_scores(h, j0, nb):
                        pa = ps1(exclude=pO)
                        for jj in range(nb):
                            j = j0 + jj
                            pe(lambda e, h=h, j=j, jj=jj, pa=pa: e.matmul(pa[:, jj * 128:(jj + 1) * 128],
                                                                          lhsT=KT[:, h, j * 128:(j + 1) * 128],
                                                                          rhs=QT[:, h, :], start=True, stop=True),
                               [KT, QT], [pa])
                        return pa

                    pa_next = att_scores(*groups[0])
                    for gi, (h, j0, nb) in enumerate(groups):
                        pa = pa_next
                        if gi + 1 < len(groups):
                            pa_next = att_scores(*groups[gi + 1])
                        pt = PT[gi % 3]
                        act(lambda e, nb=nb, pt=pt, pa=pa: e.activation(
                            out=pt[:, 0:nb, :].rearrange("p a b -> p (a b)"), in_=pa[:, 0:nb * 128], func=AF.Exp,
                            scale=96.0 ** -0.5), [pa], [pt])
                        if j0 + nb == n + 1:
                            pool(lambda e, nb=nb, pt=pt: e.tensor_tensor(out=pt[:, nb - 1, :], in0=pt[:, nb - 1, :],
                                                                        in1=maskle, op=ALU.mult), [pt, cst], [pt])
                        for jj in range(nb):
                            j = j0 + jj
                            pe(lambda e, h=h, j=j, jj=jj, pt=pt: e.matmul(pO[:, h * 65:(h + 1) * 65], lhsT=pt[:, jj, :],
                                                                          rhs=Vaug[:, j, h, :], start=(j == 0),
                                                                          stop=(j == n)), [pt, Vaug], [pO])
                    pOv = pO[:, 0:390].rearrange("p (a b) -> p a b", a=6)
                    dve(lambda e: e.reciprocal(out=hst[:, 2, 0:6], in_=pOv[:, :, 64]), [pO], [hst])
                    dve(lambda e: e.tensor_tensor(out=ha[:], in0=pOv[:, :, 0:64],
                                                  in1=hst[:, 2, 0:6, None].broadcast_to([128, 6, 64]), op=ALU.mult),
                        [pO, hst], [ha])

                    mk.stream = L1
                    cur["pool"] = "A"
                    pT4 = proj_tok(O_CU, O_CU + 512)
                    act(lambda e: e.activation(out=ga[:], in_=pT4[:], func=AF.Square), [pT4], [ga])
                    dve(lambda e: e.tensor_scalar(out=ga[:], in0=ga[:], scalar1=0.044715, scalar2=1.0, op0=ALU.mult,
                                                  op1=ALU.add), [ga], [ga])
                    dve(lambda e: e.tensor_tensor(out=ga[:], in0=ga[:], in1=pT4[:], op=ALU.mult), [ga, pT4], [ga])
                    act(lambda e: e.activation(out=gb_[:], in_=ga[:], func=AF.Exp, scale=-2.0 * math.sqrt(2.0 / math.pi)),
                        [ga], [gb_])
                    act(lambda e: e.activation(out=gb_[:], in_=gb_[:], func=AF.Ln, bias=c_one), [gb_, sc], [gb_])
                    act(lambda e: e.activation(out=gb_[:], in_=gb_[:], func=AF.Exp, scale=-1.0), [gb_], [gb_])
                    dve(lambda e: e.tensor_tensor(out=guv[:], in0=gb_[:], in1=pT4[:], op=ALU.mult), [gb_, pT4], [guv])
                    gv = guv[:, 256:512].rearrange("p (a b) -> p a b", a=4)
                    jv = junk[:, 0:256].rearrange("p (a b) -> p a b", a=4)
                    act(lambda e: e.activation(out=jv, in_=gv, func=AF.Square), [guv], [junk])
                    dve(lambda e: e.reduce_sum(out=g_t[:, 6, :], in_=jv, axis=AX.X), [junk], [g_t])
                    rsqrt(g_t[:, 7, :], g_t[:, 6, :], 1.0 / 64, c_eps, [g_t], [g_t])
                    dve(lambda e: e.tensor_tensor(out=vn[:].rearrange("p (a b) -> p a b", a=4), in0=gv,
                                                  in1=g_t[:, 7, :, None].broadcast_to([128, 4, 64]), op=ALU.mult),
                        [guv, g_t], [vn])
                    pM = ps1()
                    for g in range(4):
                        pe(lambda e, g=g: e.matmul(pM[:, g * 64:(g + 1) * 64], lhsT=wsT_b[:, g, :],
                                                   rhs=vn[:, g * 64:(g + 1) * 64], start=True, stop=True), [wsT_b, vn], [pM])
                    dve(lambda e: e.tensor_tensor(out=hc[:], in0=pM[:, 0:256], in1=cvg, op=ALU.mult), [pM, rvec], [hc])
                    hcv = hc[:].rearrange("p (a b) -> p a b", a=4)
                    dve(lambda e: e.tensor_tensor(out=hcv, in0=hcv, in1=bsb[:, :, None].broadcast_to([128, 4, 64]),
                                                  op=ALU.add), [hc, pvec], [hc])
                    dve(lambda e: e.tensor_tensor(out=hc[:], in0=hc[:], in1=guv[:, 0:256], op=ALU.mult), [hc, guv], [hc])

                    mk.stream = None
                    mk.merge([L1, L2])
                    jm = junk[:, 0:384].rearrange("p (a b) -> p a b", a=4)
                    act(lambda e: e.activation(out=jm, in_=hm[:], func=AF.Square), [hm], [junk])
                    dve(lambda e: e.reduce_sum(out=hst[:, 0, 0:4], in_=jm, axis=AX.X), [junk], [hst])
                    ja = junk[:, 384:768].rearrange("p (a b) -> p a b", a=6)
                    act(lambda e: e.activation(out=ja, in_=ha[:], func=AF.Square), [ha], [junk])
                    dve(lambda e: e.reduce_sum(out=hst[:, 0, 4:10], in_=ja, axis=AX.X), [junk], [hst])
                    jc = junk[:, 768:1024].rearrange("p (a b) -> p a b", a=4)
                    act(lambda e: e.activation(out=jc, in_=hcv, func=AF.Square), [hc], [junk])
                    dve(lambda e: e.reduce_sum(out=hst[:, 0, 10:14], in_=jc, axis=AX.X), [junk], [hst])
                    rsqrt(hst[:, 1, 0:4], hst[:, 0, 0:4], 1.0 / 96, c_eps, [hst], [hst])
                    rsqrt(hst[:, 1, 4:14], hst[:, 0, 4:14], 1.0 / 64, c_eps, [hst], [hst])
                    dve(lambda e: e.tensor_tensor(out=ycat[:, 0:384].rearrange("p (a b) -> p a b", a=4), in0=hm[:],
                                                  in1=hst[:, 1, 0:4, None].broadcast_to([128, 4, 96]), op=ALU.mult),
                        [hm, hst], [ycat])
                    dve(lambda e: e.tensor_tensor(out=ycat[:, 384:768].rearrange("p (a b) -> p a b", a=6), in0=ha[:],
                                                  in1=hst[:, 1, 4:10, None].broadcast_to([128, 6, 64]), op=ALU.mult),
                        [ha, hst], [ycat])
                    dve(lambda e: e.tensor_tensor(out=ycat[:, 768:1024].rearrange("p (a b) -> p a b", a=4), in0=hcv,
                                                  in1=hst[:, 1, 10:14, None].broadcast_to([128, 4, 64]), op=ALU.mult),
                        [hc, hst], [ycat])
                    if dbg and sq == 0 and l == 0:
                        dve(lambda e: e.tensor_copy(out=junk[:], in_=ycat[:]), [ycat], [junk])
                        mk.dma("sp", dbg_d["y"][tsl, :], junk[:], reads=[junk], writes=[Buf("dbgy")], stream="dbg")
                    py = ps1()
                    pyb = py[:].bitcast(BF16)
                    for k in range(8):
                        pe(lambda e, k=k: e.transpose(out=pyb[:, k * 128:(k + 1) * 128], in_=ycat[:, k * 128:(k + 1) * 128],
                                                      identity=identb[:]), [ycat, identb], [py])
                    act(lambda e: e.copy(out=yT[:].rearrange("p a b -> p (a b)"), in_=pyb[:, 0:1024]), [py], [yT])
                    po = [ps1(), ps1()]
                    for hf in range(2):
                        for k in range(8):
                            pe(lambda e, hf=hf, k=k: e.matmul(po[hf][:], lhsT=yT[:, k, :],
                                                              rhs=wout_b[:, k, hf * 512:(hf + 1) * 512],
                                                              start=(k == 0), stop=(k == 7)), [yT, wout_b], [po[hf]])
                    for hf in range(2):
                        dve(lambda e, hf=hf: e.tensor_tensor(out=xt[:, hf * 512:(hf + 1) * 512],
                                                             in0=xt[:, hf * 512:(hf + 1) * 512], in1=po[hf][:], op=ALU.add),
                            [xt, po[hf]], [xt])
                    if dbg and sq == 0 and l == 0:
                        mk.dma("sp", dbg_d["x1"][tsl, :], xt[:], reads=[xt], writes=[Buf("dbgx1")], stream="dbg")
                    if do_moe or l < n_layers - 1:
                        mk.dma("sp", xs_d[tsl, :], xt[:], reads=[xt], writes=[xsb[n]], stream="xs_st")
                    else:
                        mk.dma("sp", out_d[sq, tsl, :], xt[:], reads=[xt], writes=[Buf("outd")], stream="out")

                if not do_moe:
                    continue
                mk.barrier()
                junk = mjunk
                for n in range(NT):
                    mk.dma("sp", xres[n][:], xs_d[n * 128:(n + 1) * 128, :], reads=[xsb[n]], writes=[xres[n]])
                mk.dma("sp", wr[:, :, 0:4], w_g_d[l].rearrange("(k p) n -> p k n", p=128), writes=[wr])
                mk.dma("sp", wr[:, :, 4:36], w_e_d[l].rearrange("(k p) n -> p k n", p=128), writes=[wr])
                dve(lambda e: e.tensor_copy(out=wrh[:], in_=wr[:]), [wr], [wrh])
                dve(lambda e: e.tensor_tensor(out=wrl[:], in0=wr[:], in1=wrh[:], op=ALU.subtract), [wr, wrh], [wrl])
                for n in range(NT):
                    xt = xres[n]
                    act(lambda e: e.activation(out=junk[:], in_=xt[:], func=AF.Square, accum_out=stt[:, 0:1]),
                        [xt], [junk, stt])
                    rsqrt(stt[:, 1:2], stt[:, 0:1], 1.0 / D, c_eps, [stt], [stt])
                    dve(lambda e: e.scalar_tensor_tensor(out=xnf[:], in0=xt[:], scalar=stt[:, 1:2], in1=gains[:, 1, :],
                                                         op0=ALU.mult, op1=ALU.mult), [xt, stt, gains], [xnf])
                    act(lambda e: e.copy(out=xh[:], in_=xnf[:]), [xnf], [xh])
                    dve(lambda e: e.tensor_tensor(out=xl[:], in0=xnf[:], in1=xh[:], op=ALU.subtract), [xnf, xh], [xl])
                    for src, dst3 in ((xh, xnT[:, :, n * 128:(n + 1) * 128]), (xl, xlT[:])):
                        pt_ = ps1()
                        ptb = pt_[:].bitcast(BF16)
                        for k in range(8):
                            pe(lambda e, k=k, src=src, ptb=ptb: e.transpose(out=ptb[:, k * 128:(k + 1) * 128],
                                                                            in_=src[:, k * 128:(k + 1) * 128],
                                                                            identity=identb[:]), [src, identb], [pt_])
                        if src is xh:
                            act(lambda e, dst3=dst3, ptb=ptb: e.copy(out=dst3, in_=v3(ptb[:, 0:1024], 8)), [pt_], [xnT])
                        else:
                            dve(lambda e, dst3=dst3, ptb=ptb: e.tensor_copy(out=dst3, in_=v3(ptb[:, 0:1024], 8)),
                                [pt_], [xlT])
                    pr = ps1()
                    terms = []
                    for k in range(8):
                        terms += [(xnT[:, k, n * 128:(n + 1) * 128], wrh[:, k, :], xnT),
                                  (xlT[:, k, :], wrh[:, k, :], xlT),
                                  (xnT[:, k, n * 128:(n + 1) * 128], wrl[:, k, :], xnT)]
                    for i, (lt, rh, lb) in enumerate(terms):
                        pe(lambda e, lt=lt, rh=rh, i=i: e.matmul(pr[:, 0:36], lhsT=lt, rhs=rh, start=(i == 0),
                                                                 stop=(i == len(terms) - 1)), [lb, wrh, wrl], [pr])
                    act(lambda e, n=n: e.copy(out=rl[:, n, :], in_=pr[:, 0:36]), [pr], [rl])
                R = lambda i, w: rtmp[:, i, :, 0:w]
                gl = rl[:, :, 0:4]
                el = rl[:, :, 4:36]
                glb, gmx, goh, gex, gsm, ggt = R(0, 4), R(1, 1), R(2, 4), R(3, 4), R(4, 1), R(5, 1)
                RB = [rtmp, t32t]
                dve(lambda e: e.tensor_tensor(out=glb, in0=gl, in1=bgb[:, None, :].broadcast_to([128, NT, 4]), op=ALU.add),
                    [rl, rvec], RB)
                dve(lambda e: e.tensor_reduce(out=gmx, in_=glb, axis=AX.X, op=ALU.max), RB, RB)
                dve(lambda e: e.tensor_tensor(out=goh, in0=glb, in1=gmx.broadcast_to([128, NT, 4]), op=ALU.is_ge), RB, RB)
                dve(lambda e: e.tensor_reduce(out=gmx, in_=gl, axis=AX.X, op=ALU.max), [rl], RB)
                dve(lambda e: e.tensor_tensor(out=gex, in0=gl, in1=gmx.broadcast_to([128, NT, 4]), op=ALU.subtract),
                    [rl, rtmp], RB)
                act(lambda e: e.activation(out=gex, in_=gex, func=AF.Exp), RB, RB)
                dve(lambda e: e.tensor_reduce(out=gsm, in_=gex, axis=AX.X, op=ALU.add), RB, RB)
                dve(lambda e: e.tensor_tensor(out=gex, in0=gex, in1=goh, op=ALU.mult), RB, RB)
                dve(lambda e: e.tensor_reduce(out=ggt, in_=gex, axis=AX.X, op=ALU.add), RB, RB)
                dve(lambda e: e.reciprocal(out=gsm, in_=gsm), RB, RB)
                dve(lambda e: e.tensor_tensor(out=ggt, in0=ggt, in1=gsm, op=ALU.mult), RB, RB)
                t32, elb8, el8 = t32t[:], R(7, 8), R(8, 8)
                t32v = t32.rearrange("p n (g e) -> p n g e", g=4)
                gohb = goh[:, :, :, None].broadcast_to([128, NT, 4, 8])
                dve(lambda e: e.tensor_tensor(out=t32v, in0=el.rearrange("p n (g e) -> p n g e", g=4), in1=gohb,
                                              op=ALU.mult), [rl, rtmp], RB)
                dve(lambda e: e.tensor_reduce(out=el8, in_=t32.rearrange("p n (g e) -> p n e g", g=4), axis=AX.X,
                                              op=ALU.add), RB, RB)
                dve(lambda e: e.tensor_tensor(out=t32v, in0=beb[:, None, :].broadcast_to([128, NT, 32]).rearrange(
                    "p n (g e) -> p n g e", g=4), in1=gohb, op=ALU.mult), [rvec, rtmp], RB)
                dve(lambda e: e.tensor_reduce(out=elb8, in_=t32.rearrange("p n (g e) -> p n e g", g=4), axis=AX.X,
                                              op=ALU.add), RB, RB)
                dve(lambda e: e.tensor_tensor(out=elb8, in0=elb8, in1=el8, op=ALU.add), RB, RB)
                emx, eex, oh1, oh2, p1, p2 = R(9, 1), R(10, 8), R(11, 8), R(6, 8), R(1, 1), R(4, 1)
                dve(lambda e: e.tensor_reduce(out=emx, in_=el8, axis=AX.X, op=ALU.max), RB, RB)
                dve(lambda e: e.tensor_tensor(out=eex, in0=el8, in1=emx.broadcast_to([128, NT, 8]), op=ALU.subtract), RB, RB)
                act(lambda e: e.activation(out=eex, in_=eex, func=AF.Exp), RB, RB)
                dve(lambda e: e.tensor_reduce(out=emx, in_=elb8, axis=AX.X, op=ALU.max), RB, RB)
                dve(lambda e: e.tensor_tensor(out=oh1, in0=elb8, in1=emx.broadcast_to([128, NT, 8]), op=ALU.is_ge), RB, RB)
                dve(lambda e: e.scalar_tensor_tensor(out=elb8, in0=oh1, scalar=-1e30, in1=elb8, op0=ALU.mult, op1=ALU.add),
                    RB, RB)
                dve(lambda e: e.tensor_reduce(out=emx, in_=elb8, axis=AX.X, op=ALU.max), RB, RB)
                dve(lambda e: e.tensor_tensor(out=oh2, in0=elb8, in1=emx.broadcast_to([128, NT, 8]), op=ALU.is_ge), RB, RB)
                dve(lambda e: e.tensor_tensor(out=oh1, in0=oh1, in1=eex, op=ALU.mult), RB, RB)
                dve(lambda e: e.tensor_tensor(out=oh2, in0=oh2, in1=eex, op=ALU.mult), RB, RB)
                dve(lambda e: e.tensor_tensor(out=oh1, in0=oh1, in1=oh2, op=ALU.add), RB, RB)
                dve(lambda e: e.tensor_reduce(out=p1, in_=oh1, axis=AX.X, op=ALU.add), RB, RB)
                dve(lambda e: e.reciprocal(out=p1, in_=p1), RB, RB)
                dve(lambda e: e.tensor_tensor(out=p1, in0=p1, in1=ggt, op=ALU.mult), RB, RB)
                dve(lambda e: e.tensor_tensor(out=oh1, in0=oh1, in1=p1.broadcast_to([128, NT, 8]), op=ALU.mult), RB, RB)
                dve(lambda e: e.tensor_tensor(out=gate[:].rearrange("p n (g e) -> p n g e", g=4),
                                              in0=oh1[:, :, None, :].broadcast_to([128, NT, 4, 8]), in1=gohb, op=ALU.mult),
                    RB, [gate])
                for ex in range(n_exp):
                    bi = ex % 2
                    wbl = web[bi]
                    mk.dma("pool", wbl[0][:].rearrange("p (k n) -> p k n", k=8),
                           w1_d[l, ex].rearrange("(k p) n -> p k n", p=128), writes=[wbl[0]])
                    mk.dma("pool", wbl[1][:].rearrange("p (k n) -> p k n", k=8),
                           w3_d[l, ex].rearrange("(k p) n -> p k n", p=128), writes=[wbl[1]])
                    mk.dma("pool", wbl[2][:].rearrange("p (k n) -> p k n", k=2),
                           w2_d[l, ex].rearrange("(k p) n -> p k n", p=128), writes=[wbl[2]])
                    for g in range(4):
                        hg = hh[g % 2]
                        for c in range(2):
                            p1_ = ps1()
                            p3_ = ps1()
                            for j, pp in ((0, p1_), (1, p3_)):
                                for k in range(8):
                                    pe(lambda e, j=j, pp=pp, k=k, c=c: e.matmul(
                                        pp[:], lhsT=wbl[j][:, k * 256 + c * 128:k * 256 + (c + 1) * 128],
                                        rhs=xnT[:, k, g * 512:(g + 1) * 512], start=(k == 0), stop=(k == 7)),
                                       [wbl[j], xnT], [pp])
                            s1t = s1[c]
                            act(lambda e, s1t=s1t, p1_=p1_: e.activation(out=s1t[:], in_=p1_[:], func=AF.Silu), [p1_], [s1t])
                            dve(lambda e, s1t=s1t, p3_=p3_, c=c, hg=hg: e.tensor_tensor(out=hg[:, c, :], in0=s1t[:],
                                                                                     in1=p3_[:], op=ALU.mult),
                                [s1t, p3_], [hg])
                        for t in range(4):
                            n = g * 4 + t
                            py_ = ps2()
                            for hf in range(2):
                                for c in range(2):
                                    pe(lambda e, hf=hf, c=c, t=t, hg=hg, py_=py_: e.matmul(
                                        py_[:, hf * 512:(hf + 1) * 512], lhsT=hg[:, c, t * 128:(t + 1) * 128],
                                        rhs=wbl[2][:, c * 1024 + hf * 512:c * 1024 + (hf + 1) * 512], start=(c == 0),
                                        stop=(c == 1)), [hg, wbl[2]], [py_])
                            xt = xres[n]
                            dve(lambda e, xt=xt, py_=py_, n=n, ex=ex: e.scalar_tensor_tensor(
                                out=xt[:], in0=py_[:], scalar=gate[:, n, ex:ex + 1], in1=xt[:], op0=ALU.mult, op1=ALU.add),
                                [py_, gate, xt], [xt])
                for n in range(NT):
                    if l < n_layers - 1:
                        mk.dma("sp", xs_d[n * 128:(n + 1) * 128, :], xres[n][:], reads=[xres[n]], writes=[xsb[n]],
                               stream="xs_st")
                    else:
                        mk.dma("sp", out_d[sq, n * 128:(n + 1) * 128, :], xres[n][:], reads=[xres[n]],
                               writes=[Buf("outd")], stream="out")

        mk.barrier()
        mk.emit()
    return nc, mk


def _host_inputs(inputs):
    f = lambda k: np.ascontiguousarray(np.asarray(inputs[k], dtype=np.float32))
    x = f("x")
    pos = np.ascontiguousarray(np.asarray(inputs["positions"]).astype(np.int32))
    pvec = np.zeros((L, 128, 64), np.float32)
    pvec[:, :, 0:8] = f("attn_norm").reshape(L, 8, 128).transpose(0, 2, 1)
    pvec[:, :, 8:16] = f("mix_out_norm").reshape(L, 8, 128).transpose(0, 2, 1)
    pvec[:, :, 16:24] = f("ffn_norm").reshape(L, 8, 128).transpose(0, 2, 1)
    pvec[:, :, 24:26] = f("a_q_norm").reshape(L, 2, 128).transpose(0, 2, 1)
    pvec[:, :, 26:27] = f("a_kv_norm").reshape(L, 1, 128).transpose(0, 2, 1)
    pvec[:, :, 27:31] = f("c_b_s").transpose(0, 2, 1)
    convw = np.zeros((L, 96, 40), np.float32)
    convw[:, :, 0:32] = f("m_conv_w").reshape(L, 4, 8, 96).transpose(0, 3, 2, 1).reshape(L, 96, 32)
    convw[:, :, 32:40] = f("m_conv_b").reshape(L, 8, 96).transpose(0, 2, 1)
    rrow = np.zeros((L, 496), np.float32)
    rrow[:, 0:8] = f("m_gate_bias")
    rrow[:, 8:104] = f("a_q_head_norm")
    rrow[:, 104:200] = f("a_k_head_norm")
    rrow[:, 200:456] = f("c_v_norm")
    rrow[:, 456:460] = f("b_group")
    rrow[:, 460:492] = f("b_expert")
    rvec = np.ascontiguousarray(np.broadcast_to(rrow[:, None, :], (L, 128, 496)))
    grow = np.concatenate([f("attn_norm"), f("ffn_norm")], axis=1)
    gvec = np.ascontiguousarray(np.broadcast_to(grow[:, None, :], (L, 128, 2048)))
    consts = np.zeros((128, 528), np.float32)
    p = np.arange(128)
    consts[:, 0:128] = np.eye(128, dtype=np.float32)
    consts[:, 128:256] = (p[:, None] <= p[None, :]).astype(np.float32)
    consts[:, 256:384] = (p[:, None] >= p[None, :]).astype(np.float32)
    consts[:, 384:512] = 1.0
    consts[:, 512:528] = (1.0 / (10000.0 ** (np.arange(0, 32, 2, dtype=np.float32) / 32.0))).astype(np.float32)[None, :]
    shared = {
        "w_in": f("w_in"), "w_out": f("w_out"), "a_w_uq": f("a_w_uq"), "a_w_ukv": f("a_w_ukv"), "c_w_s": f("c_w_s"),
        "w_group": f("w_group"), "w_expert": f("w_expert"), "w1": f("w1"), "w3": f("w3"), "w2": f("w2"),
        "pvec": pvec, "convw": convw, "rvec": rvec, "gvec": gvec, "consts": consts,
    }
    return x, pos, shared


def kernel(**inputs):
    x, pos, shared = _host_inputs(inputs)
    B = x.shape[0]
    per = B // NCORES
    nc, _ = build(n_seq=per, n_layers=L)
    in_maps = []
    for c in range(NCORES):
        m = dict(shared)
        m["x"] = np.ascontiguousarray(x[c * per:(c + 1) * per])
        pc = pos[c * per:(c + 1) * per].reshape(per, NT, 128).transpose(0, 2, 1)
        m["posT"] = np.ascontiguousarray(pc)
        in_maps.append(m)
    res = run_bass_kernel_spmd(nc, in_maps, core_ids=list(range(NCORES)))
    return np.concatenate([r["out"] for r in res.results], axis=0).astype(np.float32)
```

```python
import math
from contextlib import ExitStack
import numpy as np
import concourse.bass as bass
import concourse.mybir as mybir
from concourse.bass_utils import run_bass_kernel_spmd

F32 = mybir.dt.float32
BF16 = mybir.dt.bfloat16
I32 = mybir.dt.int32
AF = mybir.ActivationFunctionType
ALU = mybir.AluOpType
AX = mybir.AxisListType

NCORES = 8
S = 2048
D = 1024
NT = 16
L = 4
DIN = 2472
O_MQ, O_MK, O_MV, O_MO, O_MI, O_MF, O_CQ, O_CKV, O_KR, O_CU, O_CV = (
    0, 384, 768, 1152, 1536, 1540, 1544, 1800, 1928, 1960, 2216)
EPS = 1e-6


class Buf:
    __slots__ = ("name", "w", "r")

    def __init__(self, name):
        self.name = name
        self.w = None
        self.r = []


class Tl:
    def __init__(self, t, name):
        self.t = t
        self.b = Buf(name)

    def __getitem__(self, k):
        return self.t[k]


class _Rec:
    def __init__(self):
        self.call = None

    def __getattr__(self, name):
        def f(*a, **k):
            self.call = (name, a, k)
            return self
        return f


class MK:
    ENGS = ("pe", "dve", "act", "pool", "sp")

    def __init__(self, nc, stack):
        self.nc = nc
        self.stack = stack
        self.ops = {e: [] for e in self.ENGS}
        self.sems = {}
        self.count = {}
        self.seen = {e: {} for e in self.ENGS}
        for e in self.ENGS:
            self._sem("E_" + e)
        self.nops = 0
        self.stream = None

    def _emit(self, e, waits, fn, sem, inc):
        self.ops[e].append((waits, None, sem, inc))

    def emit(self):
        with self.nc.Block() as block:
            def mkbody(e):
                def body(eng):
                    for waits, call, sem, inc in self.ops[e]:
                        for s_, v in waits:
                            eng.wait_ge(s_, v)
                        if call is not None:
                            name, a, k = call
                            getattr(eng, name)(*a, **k).then_inc(sem, inc)
                return body
            block.tensor(mkbody("pe"))
            block.vector(mkbody("dve"))
            block.scalar(mkbody("act"))
            block.gpsimd(mkbody("pool"))
            block.sync(mkbody("sp"))

    def _sem(self, key):
        if key not in self.sems:
            self.sems[key] = self.stack.enter_context(self.nc.semaphore(key))
            self.count[key] = 0
        return self.sems[key]

    def sb(self, name, shape, dt):
        return Tl(self.stack.enter_context(self.nc.sbuf_tensor("sb_" + name, list(shape), dt)), name)

    def ps(self, name, shape, dt):
        return Tl(self.stack.enter_context(self.nc.psum_tensor(name, list(shape), dt)), name)

    def _waits(self, e, reads, writes):
        need = {}

        def add(ev, raw):
            if ev is None:
                return
            k, v, src = ev
            if src == e and (e == "pe" or not raw):
                return
            if need.get(k, 0) < v:
                need[k] = v
        for b in reads:
            add(b.w, True)
        for b in writes:
            add(b.w, False)
            for ev in b.r:
                add(ev, False)
        out = []
        seen = self.seen[e]
        for k, v in need.items():
            if seen.get(k, 0) < v:
                seen[k] = v
                out.append((self.sems[k], v))
        return out

    def _reg(self, ev, reads, writes):
        for b in reads:
            b.r.append(ev)
        for b in writes:
            b.w = ev
            b.r = []

    def op(self, e, fn, reads=(), writes=()):
        rec = _Rec()
        fn(rec)
        reads = [getattr(x, "b", x) for x in reads]
        writes = [getattr(x, "b", x) for x in writes]
        if self.stream is not None:
            self.stream.append((e, rec.call, reads, writes, None))
        else:
            self._op_call(e, rec.call, reads, writes, None)

    def dma(self, q, out, in_, reads=(), writes=(), stream=None):
        reads = [getattr(x, "b", x) for x in reads]
        writes = [getattr(x, "b", x) for x in writes]
        key = "D_" + (stream or (writes[0].name if writes else reads[0].name))
        call = ("dma_start", (), dict(out=out, in_=in_))
        if self.stream is not None:
            self.stream.append((q, call, reads, writes, key))
        else:
            self._op_call(q, call, reads, writes, key)

    def _op_call(self, e, call, reads, writes, dkey):
        waits = self._waits(e, reads, writes)
        if dkey is None:
            key, inc, src = "E_" + e, 1, e
        else:
            key, inc, src = dkey, 16, "dma"
            self._sem(key)
        self.count[key] += inc
        ev = (key, self.count[key], src)
        self._reg(ev, reads, writes)
        self.ops[e].append((waits, call, self.sems[key], inc))
        self.nops += 1

    def merge(self, lists):
        idx = [0] * len(lists)
        while True:
            best, bf = -1, 2.0
            for i, l_ in enumerate(lists):
                if idx[i] < len(l_):
                    f_ = idx[i] / len(l_)
                    if f_ < bf:
                        best, bf = i, f_
            if best < 0:
                break
            self._op_call(*lists[best][idx[best]])
            idx[best] += 1

    def barrier(self):
        for e in self.ENGS:
            waits = []
            for k, v in self.count.items():
                if v > 0 and self.seen[e].get(k, 0) < v and k != "E_" + e:
                    self.seen[e][k] = v
                    waits.append((self.sems[k], v))
            self._emit(e, waits, None, None, 0)


def build(n_seq=4, n_layers=4, dbg=False, do_moe=True, n_exp=32):
    nc = bass.Bass("TRN2", target_bir_lowering=False)

    def din(name, shape, dt=F32):
        return nc.dram_tensor(name, list(shape), dt, kind="ExternalInput").ap()

    x_d = din("x", [n_seq, S, D])
    pos_d = din("posT", [n_seq, 128, NT], I32)
    w_in_d = din("w_in", [L, D, DIN])
    w_out_d = din("w_out", [L, D, D])
    w_uq_d = din("a_w_uq", [L, 256, 576])
    w_ukv_d = din("a_w_ukv", [L, 128, 768])
    c_ws_d = din("c_w_s", [L, 4, 128, 128])
    w_g_d = din("w_group", [L, D, 4])
    w_e_d = din("w_expert", [L, D, 32])
    w1_d = din("w1", [L, 32, D, 256])
    w3_d = din("w3", [L, 32, D, 256])
    w2_d = din("w2", [L, 32, 256, D])
    pv_d = din("pvec", [L, 128, 64])
    cw_d = din("convw", [L, 96, 40])
    rv_d = din("rvec", [L, 128, 496])
    gv_d = din("gvec", [L, 128, 2048])
    c_d = din("consts", [128, 528])
    out_d = nc.dram_tensor("out", [n_seq, S, D], F32, kind="ExternalOutput").ap()
    dbg_d = {}
    if dbg:
        dbg_d["y"] = nc.dram_tensor("dbg_y", [S, D], F32, kind="ExternalOutput").ap()
        dbg_d["x1"] = nc.dram_tensor("dbg_x1", [S, D], F32, kind="ExternalOutput").ap()

    st = ExitStack()
    with st:
        mk = MK(nc, st)
        dve = lambda fn, r, w: mk.op("dve", fn, r, w)
        act = lambda fn, r, w: mk.op("act", fn, r, w)
        pe = lambda fn, r, w: mk.op("pe", fn, r, w)
        pool = lambda fn, r, w: mk.op("pool", fn, r, w)

        SZ = {F32: 4, BF16: 2, I32: 4}
        ARENA_F = 47104
        arena_t = st.enter_context(nc.sbuf_tensor("arena", [128, ARENA_F], F32))

        class Arena:
            def __init__(self):
                self.off = 0

            def __call__(self, name, shape, dt):
                free = 1
                for d_ in shape[1:]:
                    free *= d_
                n4 = (free * SZ[dt] + 3) // 4
                assert self.off + n4 <= ARENA_F, (name, self.off, n4)
                v = arena_t[0:shape[0], self.off:self.off + n4]
                self.off += n4
                if dt != F32:
                    v = v.bitcast(dt)
                v = v[:, 0:free]
                if len(shape) > 2:
                    names = "abcd"[:len(shape) - 1]
                    kw = {names[i]: shape[1 + i] for i in range(len(shape) - 1)}
                    v = v.rearrange("p (" + " ".join(names) + ") -> p " + " ".join(names), **kw)
                return Tl(v, name)

        cst = mk.sb("cst", [128, 528], F32)
        identb = mk.sb("identb", [128, 128], BF16)
        mask4b = mk.sb("mask4b", [128, 4, 128], F32)
        sc = mk.sb("sc", [128, 8], F32)
        cossin = mk.sb("cossin", [128, 2, NT, 16], F32)
        pvec = mk.sb("pvec", [128, 64], F32)
        rvec = mk.sb("rvec", [128, 496], F32)
        convw = mk.sb("convw", [96, 40], F32)
        posi = mk.sb("posi", [128, NT], I32)
        posf = mk.sb("posf", [128, NT], F32)
        stt = mk.sb("stt", [128, 8], F32)
        hst = mk.sb("hst", [128, 3, 14], F32)
        g_t = mk.sb("g_t", [128, 8, 4], F32)
        sttB = mk.sb("sttB", [128, 8], F32)
        gains = mk.sb("gains", [128, 2, D], F32)
        A = Arena()
        xtile = [A(f"xtile{i}", [128, D], F32) for i in range(2)]
        convd = A("convd", [96, 8, 4, 96], BF16)
        win_b = A("win_b", [128, 8, DIN], BF16)
        wout_b = A("wout_b", [128, 8, D], BF16)
        wuq_b = A("wuq_b", [128, 2, 576], BF16)
        wukv_b = A("wukv_b", [128, 768], BF16)
        wsT_b = A("wsT_b", [128, 4, 128], BF16)
        stage2 = A("stage2", [128, 1152], F32)
        stage2B = A("stage2B", [128, 1152], F32)
        stg = [stage2, stage2B]
        KT = A("KT", [96, 6, S], BF16)
        Vaug = A("Vaug", [128, NT, 6, 65], BF16)
        junk = A("junk", [128, D], F32)
        hb = A("hb", [128, D], BF16)
        hTt = A("hTt", [128, 8, 128], BF16)
        xqk = A("xqk", [96, 8, 131], BF16)
        esil = A("esil", [96, 8, 128], F32)
        qkT = A("qkT", [96, 8, 128], BF16)
        Sm = A("Sm", [128, 4, 128], BF16)
        ktok = A("ktok", [128, 4, 96], BF16)
        Vt = A("Vt", [128, 4, 97], BF16)
        Cf = A("Cf", [96, 4, 97], F32)
        Cb = A("Cb", [96, 4, 97], BF16)
        og = A("og", [128, 384], F32)
        hm = A("hm", [128, 4, 96], F32)
        cT = A("cT", [128, 3, 128], BF16)
        qs = A("qs", [128, 6, 96], F32)
        ksv = A("ksv", [128, 6, 128], F32)
        kr = A("kr", [128, 6, 32], F32)
        krope = A("krope", [128, 32], F32)
        rt = A("rt", [128, 4, 6, 16], F32)
        qb = A("qb", [128, 6, 96], BF16)
        kb = A("kb", [128, 6, 96], BF16)
        QT = A("QT", [96, 6, 128], BF16)
        PT = [A(f"PT{i}", [128, 4, 128], BF16) for i in range(3)]
        ha = A("ha", [128, 6, 64], F32)
        ga = A("ga", [128, 512], F32)
        gb_ = A("gb_", [128, 512], F32)
        guv = A("guv", [128, 512], F32)
        vn = A("vn", [128, 256], BF16)
        hc = A("hc", [128, 256], F32)
        ycat = A("ycat", [128, D], BF16)
        yT = A("yT", [128, 8, 128], BF16)
        junkB = A("junkB", [128, D], F32)
        junkA2 = A("junkA2", [128, D], F32)
        hTtB = A("hTtB", [128, 8, 128], BF16)
        hT2 = [hTt, hTtB]
        mixer_bytes = A.off * 4
        junk_mix = junk
        A = Arena()
        xres = [A(f"xres{n}", [128, D], F32) for n in range(NT)]
        xnT = A("xnT", [128, 8, S], BF16)
        xnf = A("xnf", [128, D], F32)
        xh = A("xh", [128, D], BF16)
        xl = A("xl", [128, D], BF16)
        xlT = A("xlT", [128, 8, 128], BF16)
        wrh = A("wrh", [128, 8, 36], BF16)
        wrl = A("wrl", [128, 8, 36], BF16)
        wr = A("wr", [128, 8, 36], F32)
        rl = A("rl", [128, NT, 36], F32)
        rtmp = A("rtmp", [128, 12, NT, 8], F32)
        t32t = A("t32t", [128, NT, 32], F32)
        ropet = A("ropet", [128, 7, NT, 16], F32)
        gate = A("gate", [128, NT, 32], F32)
        web = [[A(f"web{i}_{j}", [128, 2048], BF16) for j in range(3)] for i in range(2)]
        s1 = [A(f"s1_{i}", [128, 512], BF16) for i in range(2)]
        hh = [A(f"hh{i}", [128, 2, 512], BF16) for i in range(2)]
        mjunk = A("mjunk", [128, D], F32)
        moe_bytes = A.off * 4
        xs_d = nc.dram_tensor("xs_scratch", [S, D], F32, kind="Internal").ap()
        xsb = [Buf(f"xs{n}") for n in range(NT)]

        PS = [mk.ps(f"ps{i}", [128, 512], F32) for i in range(4)]
        PP = [mk.ps(f"pp{i}", [128, 1024], F32) for i in range(2)]
        ctr = {"s": 0, "p": 0}

        PPh = [Tl(PP[i][:, j * 512:(j + 1) * 512], f"pp{i}h{j}") for i in range(2) for j in range(2)]
        pools = {"A": [PS, 0], "B": [PPh, 0]}
        cur = {"pool": "A"}

        def ps1(exclude=None):
            pl = pools[cur["pool"]]
            pl[1] += 1
            if pl[0][pl[1] % 4] is exclude:
                pl[1] += 1
            return pl[0][pl[1] % 4]

        def ps2():
            ctr["p"] += 1
            return PP[ctr["p"] % 2]

        def v3(ap, a):
            return ap.rearrange("p (a b) -> p a b", a=a)

        mk.dma("sp", cst[:], c_d, writes=[cst])
        ident = cst[:, 0:128]
        maskle = cst[:, 128:256]
        maskge = cst[:, 256:384]
        ones = cst[:, 384:512]
        invf = cst[:, 512:528]
        dve(lambda e: e.tensor_copy(out=identb[:], in_=ident), [cst], [identb])
        for h in range(4):
            dve(lambda e, h=h: e.tensor_copy(out=mask4b[:, h, :], in_=maskle), [cst], [mask4b])
        pool(lambda e: e.memset(sc[:, 0:1], EPS), [], [sc])
        pool(lambda e: e.memset(sc[:, 1:2], 1.0), [], [sc])
        pool(lambda e: e.memset(sc[:, 2:3], EPS / 4), [], [sc])
        pool(lambda e: e.memset(sc[:, 3:4], 0.0), [], [sc])
        c_eps, c_one, c_eps4 = sc[:, 0:1], sc[:, 1:2], sc[:, 2:3]

        def rsqrt(out, in_, scale, bias, r, w):
            act(lambda e: e.activation(out=out, in_=in_, func=AF.Ln, scale=scale, bias=bias), r + [sc], w)
            act(lambda e: e.activation(out=out, in_=out, func=AF.Exp, scale=-0.5), w, w)

        for sq in range(n_seq):
            mk.barrier()
            mk.dma("sp", posi[:], pos_d[sq], writes=[posi])
            dve(lambda e: e.tensor_copy(out=posf[:], in_=posi[:]), [posi], [posf])
            ang = ropet[:, 0, :, :]
            dve(lambda e: e.tensor_tensor(out=ang, in0=posf[:, :, None].broadcast_to([128, NT, 16]),
                                          in1=invf[:, None, :].broadcast_to([128, NT, 16]), op=ALU.mult),
                [posf, cst], [ropet])
            for ci, shift in ((0, math.pi / 2), (1, 0.0)):
                a2 = ropet[:, 1, :, :]
                ki = ropet[:, 2, :, :]
                kf = ropet[:, 3, :, :]
                m1 = ropet[:, 4, :, :]
                RP = [ropet]
                dve(lambda e: e.tensor_scalar(out=a2, in0=ang, scalar1=shift, scalar2=None, op0=ALU.add), RP, RP)
                dve(lambda e: e.tensor_scalar(out=ki.bitcast(I32), in0=a2, scalar1=1.0 / (2 * math.pi), scalar2=None,
                                              op0=ALU.mult), RP, RP)
                dve(lambda e: e.tensor_copy(out=kf, in_=ki.bitcast(I32)), RP, RP)
                dve(lambda e: e.scalar_tensor_tensor(out=a2, in0=kf, scalar=-2 * math.pi, in1=a2, op0=ALU.mult,
                                                     op1=ALU.add), RP, RP)
                dve(lambda e: e.tensor_scalar(out=m1, in0=a2, scalar1=math.pi, scalar2=-2 * math.pi, op0=ALU.is_gt,
                                              op1=ALU.mult), RP, RP)
                dve(lambda e: e.tensor_tensor(out=a2, in0=a2, in1=m1, op=ALU.add), RP, RP)
                dve(lambda e: e.tensor_scalar(out=m1, in0=a2, scalar1=-math.pi, scalar2=2 * math.pi, op0=ALU.is_lt,
                                              op1=ALU.mult), RP, RP)
                dve(lambda e: e.tensor_tensor(out=a2, in0=a2, in1=m1, op=ALU.add), RP, RP)
                act(lambda e: e.activation(out=cossin[:, ci, :, :], in_=a2, func=AF.Sin), RP, [cossin])

            for l in range(n_layers):
                mk.barrier()
                junk = junk_mix
                pool(lambda e: e.memset(Vaug[:], 1.0), [], [Vaug])
                mk.dma("sp", pvec[:], pv_d[l], writes=[pvec])
                mk.dma("sp", rvec[:], rv_d[l], writes=[rvec])
                mk.dma("sp", convw[:], cw_d[l], writes=[convw])
                mk.dma("sp", gains[:].rearrange("p a b -> p (a b)"), gv_d[l], writes=[gains])
                for kc in range(8):
                    mk.dma("pool", win_b[:, kc, :], w_in_d[l, kc * 128:(kc + 1) * 128, :], writes=[win_b])
                def late_prep():
                    for c in range(2):
                        sg = stg[c % 2]
                        mk.dma("sp", sg[:, 0:576], w_uq_d[l, c * 128:(c + 1) * 128, :], writes=[sg])
                        act(lambda e, c=c, sg=sg: e.activation(out=wuq_b[:, c, :], in_=sg[:, 0:576], func=AF.Identity,
                                                               scale=pvec[:, 24 + c:25 + c]), [sg, pvec], [wuq_b])
                    for kc in range(8):
                        sg = stg[kc % 2]
                        mk.dma("sp", sg[:, 0:D], w_out_d[l, kc * 128:(kc + 1) * 128, :], writes=[sg])
                        act(lambda e, kc=kc, sg=sg: e.activation(out=wout_b[:, kc, :], in_=sg[:, 0:D], func=AF.Identity,
                                                                 scale=pvec[:, 8 + kc:9 + kc]), [sg, pvec], [wout_b])
                    mk.dma("sp", stage2[:, 0:768], w_ukv_d[l], writes=[stage2])
                    act(lambda e: e.activation(out=wukv_b[:], in_=stage2[:, 0:768], func=AF.Identity, scale=pvec[:, 26:27]),
                        [stage2, pvec], [wukv_b])
                    mk.dma("sp", stage2[:, 0:512].rearrange("p (g s) -> p g s", g=4),
                           c_ws_d[l].rearrange("g t s -> t g s"), writes=[stage2])
                    dve(lambda e: e.tensor_tensor(out=hb[:, 0:512].rearrange("p (g s) -> p g s", g=4),
                                                   in0=stage2[:, 0:512].rearrange("p (g s) -> p g s", g=4),
                                                   in1=maskge[:, None, :].broadcast_to([128, 4, 128]), op=ALU.mult),
                         [stage2, cst], [hb])
                    pw = ps1()
                    pwb = pw[:].bitcast(BF16)
                    for g in range(4):
                        pe(lambda e, g=g: e.transpose(out=pwb[:, g * 128:(g + 1) * 128], in_=hb[:, g * 128:(g + 1) * 128],
                                                      identity=identb[:]), [hb, identb], [pw])
                    dve(lambda e: e.tensor_copy(out=wsT_b[:].rearrange("p g t -> p (g t)"), in_=pwb[:, 0:512]), [pw], [wsT_b])
                for ch in range(8):
                    for j in range(4):
                        dve(lambda e, ch=ch, j=j: e.tensor_scalar(out=convd[:, ch, j, :], in0=cst[0:96, 0:96],
                                                                  scalar1=convw[:, ch * 4 + j:ch * 4 + j + 1],
                                                                  scalar2=None, op0=ALU.mult), [cst, convw], [convd])
                dve(lambda e: e.tensor_scalar(out=rvec[:, 0:4], in0=rvec[:, 0:4], scalar1=-0.5 * math.log(96.0),
                                               scalar2=None, op0=ALU.add), [rvec], [rvec])
                pool(lambda e: e.memset(Cf[:], 0.0), [], [Cf])
                pool(lambda e: e.memset(Cb[:], 0.0), [], [Cb])
                pool(lambda e: e.memset(xqk[:], 0.0), [], [xqk])
                gbias = rvec[:, 0:8]
                qhg = rvec[:, 8:104]
                khg = rvec[:, 104:200]
                cvg = rvec[:, 200:456]
                bgb = rvec[:, 456:460]
                beb = rvec[:, 460:492]
                bsb = pvec[:, 27:31]
                convb = convw[:, 32:40]

                def phaseA(n):
                    xt = xtile[n % 2]
                    tsl = slice(n * 128, (n + 1) * 128)
                    hT_ = hT2[n % 2]
                    if l == 0:
                        mk.dma("sp", xt[:], x_d[sq, tsl, :], writes=[xt])
                    else:
                        mk.dma("sp", xt[:], xs_d[tsl, :], reads=[xsb[n]], writes=[xt])
                    act(lambda e: e.activation(out=junkA2[:], in_=xt[:], func=AF.Square, accum_out=stt[:, 0:1]),
                        [xt], [junkA2, stt])
                    rsqrt(stt[:, 1:2], stt[:, 0:1], 1.0 / D, c_eps, [stt], [stt])
                    dve(lambda e: e.scalar_tensor_tensor(out=hb[:], in0=xt[:], scalar=stt[:, 1:2], in1=gains[:, 0, :],
                                                         op0=ALU.mult, op1=ALU.mult), [xt, stt, gains], [hb])
                    p0 = ps1()
                    p0b = p0[:].bitcast(BF16)
                    for k in range(8):
                        pe(lambda e, k=k: e.transpose(out=p0b[:, k * 128:(k + 1) * 128], in_=hb[:, k * 128:(k + 1) * 128],
                                                      identity=identb[:]), [hb, identb], [p0])
                    act(lambda e: e.copy(out=hT_[:].rearrange("p k t -> p (k t)"), in_=p0b[:, 0:1024]), [p0], [hT_])

                cur["pool"] = "A"
                phaseA(0)
                late_prep()
                for n in range(NT):
                    xt = xtile[n % 2]
                    tsl = slice(n * 128, (n + 1) * 128)
                    hTt = hT2[n % 2]

                    L1, L2 = [], []
                    mk.stream = L1
                    cur["pool"] = "A"
                    def proj_tok(c0, c1):
                        p = ps1()
                        for k in range(8):
                            pe(lambda e, k=k: e.matmul(p[:, 0:c1 - c0], lhsT=hTt[:, k, :], rhs=win_b[:, k, c0:c1],
                                                       start=(k == 0), stop=(k == 7)), [hTt, win_b], [p])
                        return p
                    pT2 = proj_tok(O_MO, O_MO + 392)
                    ipre, lf, einvb, ks, a_t, rec = (g_t[:, i, :] for i in range(6))
                    dve(lambda e: e.tensor_tensor(out=ipre, in0=pT2[:, 384:388], in1=gbias[:, 0:4], op=ALU.add),
                        [pT2, rvec], [g_t])
                    dve(lambda e: e.tensor_tensor(out=lf, in0=pT2[:, 388:392], in1=gbias[:, 4:8], op=ALU.add),
                        [pT2, rvec], [g_t])
                    act(lambda e: e.activation(out=lf, in_=lf, func=AF.Exp, scale=-1.0), [g_t], [g_t])
                    act(lambda e: e.activation(out=lf, in_=lf, func=AF.Ln, bias=c_one), [g_t, sc], [g_t])
                    pg = ps1()
                    pe(lambda e: e.matmul(pg[:, 0:4], lhsT=maskle, rhs=lf, start=True, stop=True), [cst, g_t], [pg])
                    pe(lambda e: e.matmul(pg[:, 4:8], lhsT=ones, rhs=lf, start=True, stop=True), [cst, g_t], [pg])
                    act(lambda e: e.activation(out=einvb, in_=pg[:, 0:4], func=AF.Exp), [pg], [g_t])
                    dve(lambda e: e.tensor_tensor(out=ks, in0=ipre, in1=pg[:, 0:4], op=ALU.add), [pg, g_t], [g_t])
                    act(lambda e: e.activation(out=ks, in_=ks, func=AF.Exp), [g_t], [g_t])
                    act(lambda e: e.activation(out=a_t, in_=pg[:, 4:8], func=AF.Exp, scale=-1.0), [pg], [g_t])
                    act(lambda e: e.activation(out=og[:], in_=pT2[:, 0:384], func=AF.Exp, scale=-1.0), [pT2], [og])
                    act(lambda e: e.activation(out=og[:], in_=og[:], func=AF.Ln, bias=c_one), [og, sc], [og])
                    act(lambda e: e.activation(out=og[:], in_=og[:], func=AF.Exp, scale=-1.0), [og], [og])
                    pT1 = proj_tok(O_MV, O_MV + 384)
                    for h in range(4):
                        dve(lambda e, h=h: e.tensor_scalar(out=Vt[:, h, 0:96], in0=pT1[:, h * 96:(h + 1) * 96],
                                                           scalar1=ks[:, h:h + 1], scalar2=None, op0=ALU.mult),
                            [pT1, g_t], [Vt])
                    dve(lambda e: e.tensor_copy(out=Vt[:, :, 96], in_=ks), [g_t], [Vt])
                    pq = [ps1(), ps1()]
                    for ch in range(8):
                        for k in range(8):
                            pe(lambda e, ch=ch, k=k: e.matmul(pq[ch // 4][0:96, (ch % 4) * 128:(ch % 4 + 1) * 128],
                                                              lhsT=win_b[:, k, ch * 96:(ch + 1) * 96], rhs=hTt[:, k, :],
                                                              start=(k == 0), stop=(k == 7)), [hTt, win_b], [pq[ch // 4]])
                    for hq in range(2):
                        act(lambda e, hq=hq: e.copy(out=xqk[:, hq * 4:hq * 4 + 4, 3:131], in_=v3(pq[hq][0:96, :], 4)),
                            [pq[hq]], [xqk])
                    pc = [ps1(), ps1()]
                    for ch in range(8):
                        for j in range(4):
                            pe(lambda e, ch=ch, j=j: e.matmul(pc[ch // 4][0:96, (ch % 4) * 128:(ch % 4 + 1) * 128],
                                                              lhsT=convd[:, ch, j, :], rhs=xqk[:, ch, j:j + 128],
                                                              start=(j == 0), stop=(j == 3)), [convd, xqk], [pc[ch // 4]])
                    for ch in range(8):
                        act(lambda e, ch=ch: e.activation(out=esil[:, ch, :],
                                                          in_=pc[ch // 4][0:96, (ch % 4) * 128:(ch % 4 + 1) * 128],
                                                          func=AF.Identity, bias=convb[:, ch:ch + 1]),
                            [pc[ch // 4], convw], [esil])
                    act(lambda e: e.activation(out=junk[0:96, :], in_=esil[:].rearrange("p a b -> p (a b)"),
                                               func=AF.Exp, scale=-1.0), [esil], [junk])
                    act(lambda e: e.activation(out=junk[0:96, :], in_=junk[0:96, :], func=AF.Ln, bias=c_one[0:96, :]),
                        [junk, sc], [junk])
                    act(lambda e: e.activation(out=junk[0:96, :], in_=junk[0:96, :], func=AF.Exp, scale=-1.0),
                        [junk], [junk])
                    dve(lambda e: e.tensor_tensor(out=qkT[:].rearrange("p a b -> p (a b)"),
                                                  in0=esil[:].rearrange("p a b -> p (a b)"), in1=junk[0:96, :],
                                                  op=ALU.mult), [esil, junk], [qkT])
                    pool(lambda e: e.tensor_copy(out=xqk[:, :, 0:3], in_=xqk[:, :, 128:131]), [xqk], [xqk])

                    pS = ps1()
                    for h in range(4):
                        pe(lambda e, h=h: e.matmul(pS[:, h * 128:(h + 1) * 128], lhsT=qkT[:, 4 + h, :], rhs=qkT[:, h, :],
                                                   start=True, stop=True), [qkT], [pS])
                    dve(lambda e: e.tensor_tensor(out=Sm[:], in0=v3(pS[:], 4), in1=mask4b[:], op=ALU.mult),
                        [pS, mask4b], [Sm])
                    pk = ps1()
                    pkb = pk[:].bitcast(BF16)
                    for h in range(4):
                        pe(lambda e, h=h: e.transpose(out=pkb[:, h * 96:(h + 1) * 96], in_=qkT[:, 4 + h, :],
                                                      identity=identb[0:96, 0:96]), [qkT, identb], [pk])
                    act(lambda e: e.copy(out=ktok[:].rearrange("p a b -> p (a b)"), in_=pkb[:, 0:384]), [pk], [ktok])
                    pN = ps1()
                    for h in range(4):
                        pe(lambda e, h=h: e.matmul(pN[:, h * 97:(h + 1) * 97], lhsT=Sm[:, h, :], rhs=Vt[:, h, :],
                                                   start=True, stop=False), [Sm, Vt], [pN])
                        pe(lambda e, h=h: e.matmul(pN[:, h * 97:(h + 1) * 97], lhsT=qkT[:, h, :], rhs=Cb[:, h, :],
                                                   start=False, stop=True), [qkT, Cb], [pN])
                    pD = ps1()
                    for h in range(4):
                        pe(lambda e, h=h: e.matmul(pD[0:96, h * 97:(h + 1) * 97], lhsT=ktok[:, h, :], rhs=Vt[:, h, :],
                                                   start=True, stop=True), [ktok, Vt], [pD])
                    dve(lambda e: e.tensor_tensor(out=Cf[:].rearrange("p a b -> p (a b)"),
                                                  in0=Cf[:].rearrange("p a b -> p (a b)"), in1=pD[0:96, 0:388],
                                                  op=ALU.add), [Cf, pD], [Cf])
                    for h in range(4):
                        dve(lambda e, h=h: e.tensor_scalar(out=Cf[:, h, :], in0=Cf[:, h, :], scalar1=a_t[0:96, h:h + 1],
                                                           scalar2=None, op0=ALU.mult), [Cf, g_t], [Cf])
                    act(lambda e: e.copy(out=Cb[:], in_=Cf[:]), [Cf], [Cb])
                    pNv = pN[:, 0:388].rearrange("p (a b) -> p a b", a=4)
                    act(lambda e: e.activation(out=rec, in_=pNv[:, :, 96], func=AF.Abs), [pN], [g_t])
                    dve(lambda e: e.tensor_tensor(out=rec, in0=rec, in1=einvb, op=ALU.max), [g_t], [g_t])
                    dve(lambda e: e.reciprocal(out=rec, in_=rec), [g_t], [g_t])
                    dve(lambda e: e.tensor_tensor(out=hm[:], in0=pNv[:, :, 0:96],
                                                  in1=rec[:, :, None].broadcast_to([128, 4, 96]), op=ALU.mult),
                        [pN, g_t], [hm])
                    dve(lambda e: e.tensor_tensor(out=hm[:].rearrange("p a b -> p (a b)"),
                                                  in0=hm[:].rearrange("p a b -> p (a b)"), in1=og[:], op=ALU.mult),
                        [hm, og], [hm])

                    mk.stream = L2
                    cur["pool"] = "B"
                    pT3 = proj_tok(O_CQ, O_CQ + 416)
                    act(lambda e: e.activation(out=junkB[:, 0:256], in_=pT3[:, 0:256], func=AF.Square,
                                               accum_out=sttB[:, 2:3]), [pT3], [junkB, sttB])
                    act(lambda e: e.activation(out=junkB[:, 256:384], in_=pT3[:, 256:384], func=AF.Square,
                                               accum_out=sttB[:, 3:4]), [pT3], [junkB, sttB])
                    act(lambda e: e.copy(out=krope[:], in_=pT3[:, 384:416]), [pT3], [krope])
                    act(lambda e: e.activation(out=junkB[:, 384:416], in_=pT3[:, 384:416], func=AF.Square,
                                               accum_out=sttB[:, 6:7]), [pT3], [junkB, sttB])
                    rsqrt(sttB[:, 4:5], sttB[:, 2:3], 1.0 / 256, c_eps, [sttB], [sttB])
                    rsqrt(sttB[:, 5:6], sttB[:, 3:4], 1.0 / 128, c_eps, [sttB], [sttB])
                    pC = ps1()
                    for c in range(3):
                        for k in range(8):
                            pe(lambda e, c=c, k=k: e.matmul(pC[:, c * 128:(c + 1) * 128],
                                                            lhsT=win_b[:, k, O_CQ + c * 128:O_CQ + (c + 1) * 128],
                                                            rhs=hTt[:, k, :], start=(k == 0), stop=(k == 7)),
                               [hTt, win_b], [pC])
                    act(lambda e: e.copy(out=cT[:].rearrange("p a b -> p (a b)"), in_=pC[:, 0:384]), [pC], [cT])
                    pQ = [ps1(), ps1()]
                    for j in range(2):
                        for c in range(2):
                            pe(lambda e, j=j, c=c: e.matmul(pQ[j][:, 0:288], lhsT=cT[:, c, :],
                                                            rhs=wuq_b[:, c, j * 288:(j + 1) * 288],
                                                            start=(c == 0), stop=(c == 1)), [cT, wuq_b], [pQ[j]])
                    pK = [ps1(), ps1()]
                    for j in range(2):
                        pe(lambda e, j=j: e.matmul(pK[j][:, 0:384], lhsT=cT[:, 2, :],
                                                   rhs=wukv_b[:, j * 384:(j + 1) * 384], start=True, stop=True),
                           [cT, wukv_b], [pK[j]])
                    for j in range(2):
                        dve(lambda e, j=j: e.tensor_scalar(out=qs[:, 3 * j:3 * j + 3, :].rearrange("p a b -> p (a b)"),
                                                           in0=pQ[j][:, 0:288], scalar1=sttB[:, 4:5],
                                                           scalar2=None, op0=ALU.mult), [pQ[j], sttB], [qs])
                        dve(lambda e, j=j: e.tensor_scalar(out=ksv[:, 3 * j:3 * j + 3, :].rearrange("p a b -> p (a b)"),
                                                           in0=pK[j][:, 0:384], scalar1=sttB[:, 5:6],
                                                           scalar2=None, op0=ALU.mult), [pK[j], sttB], [ksv])
                    jq = junkB[:, 0:576].rearrange("p (a b) -> p a b", a=6)
                    act(lambda e: e.activation(out=jq, in_=qs[:], func=AF.Square), [qs], [junkB])
                    dve(lambda e: e.reduce_sum(out=hst[:, 0, 0:6], in_=jq, axis=AX.X), [junkB], [hst])
                    rsqrt(hst[:, 1, 0:6], hst[:, 0, 0:6], 1.0 / 96, c_eps, [hst], [hst])
                    dve(lambda e: e.tensor_tensor(out=qs[:], in0=qs[:], in1=hst[:, 1, 0:6, None].broadcast_to([128, 6, 96]),
                                                  op=ALU.mult), [qs, hst], [qs])
                    dve(lambda e: e.tensor_tensor(out=qs[:], in0=qs[:], in1=qhg[:, None, :].broadcast_to([128, 6, 96]),
                                                  op=ALU.mult), [qs, rvec], [qs])
                    jk = junkB[:, 0:384].rearrange("p (a b) -> p a b", a=6)
                    act(lambda e: e.activation(out=jk, in_=ksv[:, :, 0:64], func=AF.Square), [ksv], [junkB])
                    dve(lambda e: e.reduce_sum(out=hst[:, 0, 6:12], in_=jk, axis=AX.X), [junkB], [hst])
                    dve(lambda e: e.tensor_scalar(out=hst[:, 0, 6:12], in0=hst[:, 0, 6:12], scalar1=sttB[:, 6:7],
                                                  scalar2=None, op0=ALU.add), [hst, sttB], [hst])
                    rsqrt(hst[:, 1, 6:12], hst[:, 0, 6:12], 1.0 / 96, c_eps, [hst], [hst])
                    rk = hst[:, 1, 6:12]
                    dve(lambda e: e.tensor_tensor(out=jk, in0=ksv[:, :, 0:64], in1=rk[:, :, None].broadcast_to([128, 6, 64]),
                                                  op=ALU.mult), [ksv, hst], [junkB])
                    dve(lambda e: e.tensor_tensor(out=kb[:, :, 0:64], in0=jk,
                                                  in1=khg[:, None, 0:64].broadcast_to([128, 6, 64]), op=ALU.mult),
                        [junkB, rvec], [kb])
                    dve(lambda e: e.tensor_tensor(out=kr[:], in0=krope[:, None, :].broadcast_to([128, 6, 32]),
                                                  in1=rk[:, :, None].broadcast_to([128, 6, 32]), op=ALU.mult),
                        [krope, hst], [kr])
                    dve(lambda e: e.tensor_tensor(out=kr[:], in0=kr[:], in1=khg[:, None, 64:96].broadcast_to([128, 6, 32]),
                                                  op=ALU.mult), [kr, rvec], [kr])
                    cosb = cossin[:, 0, n, None, :].broadcast_to([128, 6, 16])
                    sinb = cossin[:, 1, n, None, :].broadcast_to([128, 6, 16])

                    def rope(src1, src2, dst1, dst2, rb, wb):
                        t = [rt[:, i, :, :] for i in range(4)]
                        dve(lambda e: e.tensor_tensor(out=t[0], in0=src1, in1=cosb, op=ALU.mult), rb + [cossin], [rt])
                        dve(lambda e: e.tensor_tensor(out=t[1], in0=src2, in1=sinb, op=ALU.mult), rb + [cossin], [rt])
                        dve(lambda e: e.tensor_tensor(out=t[2], in0=src1, in1=sinb, op=ALU.mult), rb + [cossin], [rt])
                        dve(lambda e: e.tensor_tensor(out=t[3], in0=src2, in1=cosb, op=ALU.mult), rb + [cossin], [rt])
                        dve(lambda e: e.tensor_tensor(out=dst1, in0=t[0], in1=t[1], op=ALU.subtract), [rt], wb)
                        dve(lambda e: e.tensor_tensor(out=dst2, in0=t[2], in1=t[3], op=ALU.add), [rt], wb)
                    rope(qs[:, :, 64:80], qs[:, :, 80:96], qb[:, :, 64:80], qb[:, :, 80:96], [qs], [qb])
                    dve(lambda e: e.tensor_copy(out=qb[:, :, 0:64], in_=qs[:, :, 0:64]), [qs], [qb])
                    rope(kr[:, :, 0:16], kr[:, :, 16:32], kb[:, :, 64:80], kb[:, :, 80:96], [kr], [kb])
                    act(lambda e: e.copy(out=Vaug[:, n, :, 0:64], in_=ksv[:, :, 64:128]), [ksv], [Vaug])
                    pqt = ps1()
                    pqtb = pqt[:].bitcast(BF16)
                    for h in range(6):
                        pe(lambda e, h=h: e.transpose(out=pqtb[0:96, h * 128:(h + 1) * 128], in_=qb[:, h, :],
                                                      identity=identb[:]), [qb, identb], [pqt])
                    act(lambda e: e.copy(out=QT[:].rearrange("p a b -> p (a b)"), in_=pqtb[0:96, 0:768]), [pqt], [QT])
                    pkt = ps1()
                    pktb = pkt[:].bitcast(BF16)
                    for h in range(6):
                        pe(lambda e, h=h: e.transpose(out=pktb[0:96, h * 128:(h + 1) * 128], in_=kb[:, h, :],
                                                      identity=identb[:]), [kb, identb], [pkt])
                    act(lambda e: e.copy(out=KT[:, :, tsl], in_=pktb[0:96, 0:768].rearrange("p (a b) -> p a b", a=6)),
                        [pkt], [KT])
                    pO = ps1()
                    groups = [(h, j0, min(4, n + 1 - j0)) for h in range(6) for j0 in range(0, n + 1, 4)]

                    def att_scores(h, j0, nb):
                        pa = ps1(exclude=pO)
                        for jj in range(nb):
                            j = j0 + jj
                            pe(lambda e, h=h, j=j, jj=jj, pa=pa: e.matmul(pa[:, jj * 128:(jj + 1) * 128],
                                                                          lhsT=KT[:, h, j * 128:(j + 1) * 128],
                                                                          rhs=QT[:, h, :], start=True, stop=True),
                               [KT, QT], [pa])
                        return pa

                    pa_next = att_scores(*groups[0])
                    for gi, (h, j0, nb) in enumerate(groups):
                        pa = pa_next
                        if gi + 1 < len(groups):
                            pa_next = att_scores(*groups[gi + 1])
                        pt = PT[gi % 3]
                        act(lambda e, nb=nb, pt=pt, pa=pa: e.activation(
                            out=pt[:, 0:nb, :].rearrange("p a b -> p (a b)"), in_=pa[:, 0:nb * 128], func=AF.Exp,
                            scale=96.0 ** -0.5), [pa], [pt])
                        if j0 + nb == n + 1:
                            pool(lambda e, nb=nb, pt=pt: e.tensor_tensor(out=pt[:, nb - 1, :], in0=pt[:, nb - 1, :],
                                                                        in1=maskle, op=ALU.mult), [pt, cst], [pt])
                        for jj in range(nb):
                            j = j0 + jj
                            pe(lambda e, h=h, j=j, jj=jj, pt=pt: e.matmul(pO[:, h * 65:(h + 1) * 65], lhsT=pt[:, jj, :],
                                                                          rhs=Vaug[:, j, h, :], start=(j == 0),
                                                                          stop=(j == n)), [pt, Vaug], [pO])
                    pOv = pO[:, 0:390].rearrange("p (a b) -> p a b", a=6)
                    dve(lambda e: e.reciprocal(out=hst[:, 2, 0:6], in_=pOv[:, :, 64]), [pO], [hst])
                    dve(lambda e: e.tensor_tensor(out=ha[:], in0=pOv[:, :, 0:64],
                                                  in1=hst[:, 2, 0:6, None].broadcast_to([128, 6, 64]), op=ALU.mult),
                        [pO, hst], [ha])

                    mk.stream = L1
                    cur["pool"] = "A"
                    pT4 = proj_tok(O_CU, O_CU + 512)
                    act(lambda e: e.activation(out=ga[:], in_=pT4[:], func=AF.Square), [pT4], [ga])
                    dve(lambda e: e.tensor_scalar(out=ga[:], in0=ga[:], scalar1=0.044715, scalar2=1.0, op0=ALU.mult,
                                                  op1=ALU.add), [ga], [ga])
                    dve(lambda e: e.tensor_tensor(out=ga[:], in0=ga[:], in1=pT4[:], op=ALU.mult), [ga, pT4], [ga])
                    act(lambda e: e.activation(out=gb_[:], in_=ga[:], func=AF.Exp, scale=-2.0 * math.sqrt(2.0 / math.pi)),
                        [ga], [gb_])
                    act(lambda e: e.activation(out=gb_[:], in_=gb_[:], func=AF.Ln, bias=c_one), [gb_, sc], [gb_])
                    act(lambda e: e.activation(out=gb_[:], in_=gb_[:], func=AF.Exp, scale=-1.0), [gb_], [gb_])
                    dve(lambda e: e.tensor_tensor(out=guv[:], in0=gb_[:], in1=pT4[:], op=ALU.mult), [gb_, pT4], [guv])
                    gv = guv[:, 256:512].rearrange("p (a b) -> p a b", a=4)
                    jv = junk[:, 0:256].rearrange("p (a b) -> p a b", a=4)
                    act(lambda e: e.activation(out=jv, in_=gv, func=AF.Square), [guv], [junk])
                    dve(lambda e: e.reduce_sum(out=g_t[:, 6, :], in_=jv, axis=AX.X), [junk], [g_t])
                    rsqrt(g_t[:, 7, :], g_t[:, 6, :], 1.0 / 64, c_eps, [g_t], [g_t])
                    dve(lambda e: e.tensor_tensor(out=vn[:].rearrange("p (a b) -> p a b", a=4), in0=gv,
                                                  in1=g_t[:, 7, :, None].broadcast_to([128, 4, 64]), op=ALU.mult),
                        [guv, g_t], [vn])
                    pM = ps1()
                    for g in range(4):
                        pe(lambda e, g=g: e.matmul(pM[:, g * 64:(g + 1) * 64], lhsT=wsT_b[:, g, :],
                                                   rhs=vn[:, g * 64:(g + 1) * 64], start=True, stop=True), [wsT_b, vn], [pM])
                    dve(lambda e: e.tensor_tensor(out=hc[:], in0=pM[:, 0:256], in1=cvg, op=ALU.mult), [pM, rvec], [hc])
                    hcv = hc[:].rearrange("p (a b) -> p a b", a=4)
                    dve(lambda e: e.tensor_tensor(out=hcv, in0=hcv, in1=bsb[:, :, None].broadcast_to([128, 4, 64]),
                                                  op=ALU.add), [hc, pvec], [hc])
                    dve(lambda e: e.tensor_tensor(out=hc[:], in0=hc[:], in1=guv[:, 0:256], op=ALU.mult), [hc, guv], [hc])

                    mk.stream = None
                    mk.merge([L1, L2])
                    if n + 1 < NT:
                        phaseA(n + 1)
                    jm = junk[:, 0:384].rearrange("p (a b) -> p a b", a=4)
                    act(lambda e: e.activation(out=jm, in_=hm[:], func=AF.Square), [hm], [junk])
                    dve(lambda e: e.reduce_sum(out=hst[:, 0, 0:4], in_=jm, axis=AX.X), [junk], [hst])
                    ja = junk[:, 384:768].rearrange("p (a b) -> p a b", a=6)
                    act(lambda e: e.activation(out=ja, in_=ha[:], func=AF.Square), [ha], [junk])
                    dve(lambda e: e.reduce_sum(out=hst[:, 0, 4:10], in_=ja, axis=AX.X), [junk], [hst])
                    jc = junk[:, 768:1024].rearrange("p (a b) -> p a b", a=4)
                    act(lambda e: e.activation(out=jc, in_=hcv, func=AF.Square), [hc], [junk])
                    dve(lambda e: e.reduce_sum(out=hst[:, 0, 10:14], in_=jc, axis=AX.X), [junk], [hst])
                    rsqrt(hst[:, 1, 0:4], hst[:, 0, 0:4], 1.0 / 96, c_eps, [hst], [hst])
                    rsqrt(hst[:, 1, 4:14], hst[:, 0, 4:14], 1.0 / 64, c_eps, [hst], [hst])
                    dve(lambda e: e.tensor_tensor(out=ycat[:, 0:384].rearrange("p (a b) -> p a b", a=4), in0=hm[:],
                                                  in1=hst[:, 1, 0:4, None].broadcast_to([128, 4, 96]), op=ALU.mult),
                        [hm, hst], [ycat])
                    dve(lambda e: e.tensor_tensor(out=ycat[:, 384:768].rearrange("p (a b) -> p a b", a=6), in0=ha[:],
                                                  in1=hst[:, 1, 4:10, None].broadcast_to([128, 6, 64]), op=ALU.mult),
                        [ha, hst], [ycat])
                    dve(lambda e: e.tensor_tensor(out=ycat[:, 768:1024].rearrange("p (a b) -> p a b", a=4), in0=hcv,
                                                  in1=hst[:, 1, 10:14, None].broadcast_to([128, 4, 64]), op=ALU.mult),
                        [hc, hst], [ycat])
                    if dbg and sq == 0 and l == 0:
                        dve(lambda e: e.tensor_copy(out=junk[:], in_=ycat[:]), [ycat], [junk])
                        mk.dma("sp", dbg_d["y"][tsl, :], junk[:], reads=[junk], writes=[Buf("dbgy")], stream="dbg")
                    py = ps1()
                    pyb = py[:].bitcast(BF16)
                    for k in range(8):
                        pe(lambda e, k=k: e.transpose(out=pyb[:, k * 128:(k + 1) * 128], in_=ycat[:, k * 128:(k + 1) * 128],
                                                      identity=identb[:]), [ycat, identb], [py])
                    act(lambda e: e.copy(out=yT[:].rearrange("p a b -> p (a b)"), in_=pyb[:, 0:1024]), [py], [yT])
                    po = [ps1(), ps1()]
                    for hf in range(2):
                        for k in range(8):
                            pe(lambda e, hf=hf, k=k: e.matmul(po[hf][:], lhsT=yT[:, k, :],
                                                              rhs=wout_b[:, k, hf * 512:(hf + 1) * 512],
                                                              start=(k == 0), stop=(k == 7)), [yT, wout_b], [po[hf]])
                    for hf in range(2):
                        dve(lambda e, hf=hf: e.tensor_tensor(out=xt[:, hf * 512:(hf + 1) * 512],
                                                             in0=xt[:, hf * 512:(hf + 1) * 512], in1=po[hf][:], op=ALU.add),
                            [xt, po[hf]], [xt])
                    if dbg and sq == 0 and l == 0:
                        mk.dma("sp", dbg_d["x1"][tsl, :], xt[:], reads=[xt], writes=[Buf("dbgx1")], stream="dbg")
                    if do_moe or l < n_layers - 1:
                        mk.dma("sp", xs_d[tsl, :], xt[:], reads=[xt], writes=[xsb[n]], stream="xs_st")
                    else:
                        mk.dma("sp", out_d[sq, tsl, :], xt[:], reads=[xt], writes=[Buf("outd")], stream="out")

                if not do_moe:
                    continue
                mk.barrier()
                junk = mjunk
                for n in range(NT):
                    mk.dma("sp", xres[n][:], xs_d[n * 128:(n + 1) * 128, :], reads=[xsb[n]], writes=[xres[n]])
                mk.dma("sp", wr[:, :, 0:4], w_g_d[l].rearrange("(k p) n -> p k n", p=128), writes=[wr])
                mk.dma("sp", wr[:, :, 4:36], w_e_d[l].rearrange("(k p) n -> p k n", p=128), writes=[wr])
                dve(lambda e: e.tensor_copy(out=wrh[:], in_=wr[:]), [wr], [wrh])
                dve(lambda e: e.tensor_tensor(out=wrl[:], in0=wr[:], in1=wrh[:], op=ALU.subtract), [wr, wrh], [wrl])
                for n in range(NT):
                    xt = xres[n]
                    act(lambda e: e.activation(out=junk[:], in_=xt[:], func=AF.Square, accum_out=stt[:, 0:1]),
                        [xt], [junk, stt])
                    rsqrt(stt[:, 1:2], stt[:, 0:1], 1.0 / D, c_eps, [stt], [stt])
                    dve(lambda e: e.scalar_tensor_tensor(out=xnf[:], in0=xt[:], scalar=stt[:, 1:2], in1=gains[:, 1, :],
                                                         op0=ALU.mult, op1=ALU.mult), [xt, stt, gains], [xnf])
                    act(lambda e: e.copy(out=xh[:], in_=xnf[:]), [xnf], [xh])
                    dve(lambda e: e.tensor_tensor(out=xl[:], in0=xnf[:], in1=xh[:], op=ALU.subtract), [xnf, xh], [xl])
                    for src, dst3 in ((xh, xnT[:, :, n * 128:(n + 1) * 128]), (xl, xlT[:])):
                        pt_ = ps1()
                        ptb = pt_[:].bitcast(BF16)
                        for k in range(8):
                            pe(lambda e, k=k, src=src, ptb=ptb: e.transpose(out=ptb[:, k * 128:(k + 1) * 128],
                                                                            in_=src[:, k * 128:(k + 1) * 128],
                                                                            identity=identb[:]), [src, identb], [pt_])
                        if src is xh:
                            act(lambda e, dst3=dst3, ptb=ptb: e.copy(out=dst3, in_=v3(ptb[:, 0:1024], 8)), [pt_], [xnT])
                        else:
                            dve(lambda e, dst3=dst3, ptb=ptb: e.tensor_copy(out=dst3, in_=v3(ptb[:, 0:1024], 8)),
                                [pt_], [xlT])
                    pr = ps1()
                    terms = []
                    for k in range(8):
                        terms += [(xnT[:, k, n * 128:(n + 1) * 128], wrh[:, k, :], xnT),
                                  (xlT[:, k, :], wrh[:, k, :], xlT),
                                  (xnT[:, k, n * 128:(n + 1) * 128], wrl[:, k, :], xnT)]
                    for i, (lt, rh, lb) in enumerate(terms):
                        pe(lambda e, lt=lt, rh=rh, i=i: e.matmul(pr[:, 0:36], lhsT=lt, rhs=rh, start=(i == 0),
                                                                 stop=(i == len(terms) - 1)), [lb, wrh, wrl], [pr])
                    act(lambda e, n=n: e.copy(out=rl[:, n, :], in_=pr[:, 0:36]), [pr], [rl])
                R = lambda i, w: rtmp[:, i, :, 0:w]
                gl = rl[:, :, 0:4]
                el = rl[:, :, 4:36]
                glb, gmx, goh, gex, gsm, ggt = R(0, 4), R(1, 1), R(2, 4), R(3, 4), R(4, 1), R(5, 1)
                RB = [rtmp, t32t]
                dve(lambda e: e.tensor_tensor(out=glb, in0=gl, in1=bgb[:, None, :].broadcast_to([128, NT, 4]), op=ALU.add),
                    [rl, rvec], RB)
                dve(lambda e: e.tensor_reduce(out=gmx, in_=glb, axis=AX.X, op=ALU.max), RB, RB)
                dve(lambda e: e.tensor_tensor(out=goh, in0=glb, in1=gmx.broadcast_to([128, NT, 4]), op=ALU.is_ge), RB, RB)
                dve(lambda e: e.tensor_reduce(out=gmx, in_=gl, axis=AX.X, op=ALU.max), [rl], RB)
                dve(lambda e: e.tensor_tensor(out=gex, in0=gl, in1=gmx.broadcast_to([128, NT, 4]), op=ALU.subtract),
                    [rl, rtmp], RB)
                act(lambda e: e.activation(out=gex, in_=gex, func=AF.Exp), RB, RB)
                dve(lambda e: e.tensor_reduce(out=gsm, in_=gex, axis=AX.X, op=ALU.add), RB, RB)
                dve(lambda e: e.tensor_tensor(out=gex, in0=gex, in1=goh, op=ALU.mult), RB, RB)
                dve(lambda e: e.tensor_reduce(out=ggt, in_=gex, axis=AX.X, op=ALU.add), RB, RB)
                dve(lambda e: e.reciprocal(out=gsm, in_=gsm), RB, RB)
                dve(lambda e: e.tensor_tensor(out=ggt, in0=ggt, in1=gsm, op=ALU.mult), RB, RB)
                t32, elb8, el8 = t32t[:], R(7, 8), R(8, 8)
                t32v = t32.rearrange("p n (g e) -> p n g e", g=4)
                gohb = goh[:, :, :, None].broadcast_to([128, NT, 4, 8])
                dve(lambda e: e.tensor_tensor(out=t32v, in0=el.rearrange("p n (g e) -> p n g e", g=4), in1=gohb,
                                              op=ALU.mult), [rl, rtmp], RB)
                dve(lambda e: e.tensor_reduce(out=el8, in_=t32.rearrange("p n (g e) -> p n e g", g=4), axis=AX.X,
                                              op=ALU.add), RB, RB)
                dve(lambda e: e.tensor_tensor(out=t32v, in0=beb[:, None, :].broadcast_to([128, NT, 32]).rearrange(
                    "p n (g e) -> p n g e", g=4), in1=gohb, op=ALU.mult), [rvec, rtmp], RB)
                dve(lambda e: e.tensor_reduce(out=elb8, in_=t32.rearrange("p n (g e) -> p n e g", g=4), axis=AX.X,
                                              op=ALU.add), RB, RB)
                dve(lambda e: e.tensor_tensor(out=elb8, in0=elb8, in1=el8, op=ALU.add), RB, RB)
                emx, eex, oh1, oh2, p1, p2 = R(9, 1), R(10, 8), R(11, 8), R(6, 8), R(1, 1), R(4, 1)
                dve(lambda e: e.tensor_reduce(out=emx, in_=el8, axis=AX.X, op=ALU.max), RB, RB)
                dve(lambda e: e.tensor_tensor(out=eex, in0=el8, in1=emx.broadcast_to([128, NT, 8]), op=ALU.subtract), RB, RB)
                act(lambda e: e.activation(out=eex, in_=eex, func=AF.Exp), RB, RB)
                dve(lambda e: e.tensor_reduce(out=emx, in_=elb8, axis=AX.X, op=ALU.max), RB, RB)
                dve(lambda e: e.tensor_tensor(out=oh1, in0=elb8, in1=emx.broadcast_to([128, NT, 8]), op=ALU.is_ge), RB, RB)
                dve(lambda e: e.scalar_tensor_tensor(out=elb8, in0=oh1, scalar=-1e30, in1=elb8, op0=ALU.mult, op1=ALU.add),
                    RB, RB)
                dve(lambda e: e.tensor_reduce(out=emx, in_=elb8, axis=AX.X, op=ALU.max), RB, RB)
                dve(lambda e: e.tensor_tensor(out=oh2, in0=elb8, in1=emx.broadcast_to([128, NT, 8]), op=ALU.is_ge), RB, RB)
                dve(lambda e: e.tensor_tensor(out=oh1, in0=oh1, in1=eex, op=ALU.mult), RB, RB)
                dve(lambda e: e.tensor_tensor(out=oh2, in0=oh2, in1=eex, op=ALU.mult), RB, RB)
                dve(lambda e: e.tensor_tensor(out=oh1, in0=oh1, in1=oh2, op=ALU.add), RB, RB)
                dve(lambda e: e.tensor_reduce(out=p1, in_=oh1, axis=AX.X, op=ALU.add), RB, RB)
                dve(lambda e: e.reciprocal(out=p1, in_=p1), RB, RB)
                dve(lambda e: e.tensor_tensor(out=p1, in0=p1, in1=ggt, op=ALU.mult), RB, RB)
                dve(lambda e: e.tensor_tensor(out=oh1, in0=oh1, in1=p1.broadcast_to([128, NT, 8]), op=ALU.mult), RB, RB)
                dve(lambda e: e.tensor_tensor(out=gate[:].rearrange("p n (g e) -> p n g e", g=4),
                                              in0=oh1[:, :, None, :].broadcast_to([128, NT, 4, 8]), in1=gohb, op=ALU.mult),
                    RB, [gate])
                loaded = set()

                def ensure_w(ex):
                    if ex in loaded:
                        return
                    loaded.add(ex)
                    wbl = web[ex % 2]
                    mk.dma("pool", wbl[0][:].rearrange("p (k n) -> p k n", k=8),
                           w1_d[l, ex].rearrange("(k p) n -> p k n", p=128), writes=[wbl[0]])
                    mk.dma("pool", wbl[1][:].rearrange("p (k n) -> p k n", k=8),
                           w3_d[l, ex].rearrange("(k p) n -> p k n", p=128), writes=[wbl[1]])
                    mk.dma("pool", wbl[2][:].rearrange("p (k n) -> p k n", k=2),
                           w2_d[l, ex].rearrange("(k p) n -> p k n", p=128), writes=[wbl[2]])

                def moe_h(ex, g, c):
                    wbl = web[ex % 2]
                    hg = hh[g % 2]
                    p1_ = ps1()
                    p3_ = ps1()
                    for j, pp in ((0, p1_), (1, p3_)):
                        for k in range(8):
                            pe(lambda e, j=j, pp=pp, k=k: e.matmul(
                                pp[:], lhsT=wbl[j][:, k * 256 + c * 128:k * 256 + (c + 1) * 128],
                                rhs=xnT[:, k, g * 512:(g + 1) * 512], start=(k == 0), stop=(k == 7)),
                               [wbl[j], xnT], [pp])
                    s1t = s1[c]
                    act(lambda e: e.activation(out=s1t[:], in_=p1_[:], func=AF.Silu), [p1_], [s1t])
                    dve(lambda e: e.tensor_tensor(out=hg[:, c, :], in0=s1t[:], in1=p3_[:], op=ALU.mult),
                        [s1t, p3_], [hg])

                def moe_y(ex, g):
                    wbl = web[ex % 2]
                    hg = hh[g % 2]
                    for t in range(4):
                        n = g * 4 + t
                        py_ = ps2()
                        for hf in range(2):
                            for c in range(2):
                                pe(lambda e, hf=hf, c=c: e.matmul(
                                    py_[:, hf * 512:(hf + 1) * 512], lhsT=hg[:, c, t * 128:(t + 1) * 128],
                                    rhs=wbl[2][:, c * 1024 + hf * 512:c * 1024 + (hf + 1) * 512], start=(c == 0),
                                    stop=(c == 1)), [hg, wbl[2]], [py_])
                        xt = xres[n]
                        dve(lambda e: e.scalar_tensor_tensor(
                            out=xt[:], in0=py_[:], scalar=gate[:, n, ex:ex + 1], in1=xt[:], op0=ALU.mult, op1=ALU.add),
                            [py_, gate, xt], [xt])

                items = [(ex, g) for ex in range(n_exp) for g in range(4)]
                if items:
                    ensure_w(0)
                    moe_h(0, 0, 0)
                    moe_h(0, 0, 1)
                for i, (ex, g) in enumerate(items):
                    nxt = items[i + 1] if i + 1 < len(items) else None
                    if nxt:
                        ensure_w(nxt[0])
                        moe_h(nxt[0], nxt[1], 0)
                    moe_y(ex, g)
                    if nxt:
                        moe_h(nxt[0], nxt[1], 1)
                for n in range(NT):
                    if l < n_layers - 1:
                        mk.dma("sp", xs_d[n * 128:(n + 1) * 128, :], xres[n][:], reads=[xres[n]], writes=[xsb[n]],
                               stream="xs_st")
                    else:
                        mk.dma("sp", out_d[sq, n * 128:(n + 1) * 128, :], xres[n][:], reads=[xres[n]],
                               writes=[Buf("outd")], stream="out")

        mk.barrier()
        mk.emit()
    return nc, mk


def _host_inputs(inputs):
    f = lambda k: np.ascontiguousarray(np.asarray(inputs[k], dtype=np.float32))
    x = f("x")
    pos = np.ascontiguousarray(np.asarray(inputs["positions"]).astype(np.int32))
    pvec = np.zeros((L, 128, 64), np.float32)
    pvec[:, :, 0:8] = f("attn_norm").reshape(L, 8, 128).transpose(0, 2, 1)
    pvec[:, :, 8:16] = f("mix_out_norm").reshape(L, 8, 128).transpose(0, 2, 1)
    pvec[:, :, 16:24] = f("ffn_norm").reshape(L, 8, 128).transpose(0, 2, 1)
    pvec[:, :, 24:26] = f("a_q_norm").reshape(L, 2, 128).transpose(0, 2, 1)
    pvec[:, :, 26:27] = f("a_kv_norm").reshape(L, 1, 128).transpose(0, 2, 1)
    pvec[:, :, 27:31] = f("c_b_s").transpose(0, 2, 1)
    convw = np.zeros((L, 96, 40), np.float32)
    convw[:, :, 0:32] = f("m_conv_w").reshape(L, 4, 8, 96).transpose(0, 3, 2, 1).reshape(L, 96, 32)
    convw[:, :, 32:40] = f("m_conv_b").reshape(L, 8, 96).transpose(0, 2, 1)
    rrow = np.zeros((L, 496), np.float32)
    rrow[:, 0:8] = f("m_gate_bias")
    rrow[:, 8:104] = f("a_q_head_norm")
    rrow[:, 104:200] = f("a_k_head_norm")
    rrow[:, 200:456] = f("c_v_norm")
    rrow[:, 456:460] = f("b_group")
    rrow[:, 460:492] = f("b_expert")
    rvec = np.ascontiguousarray(np.broadcast_to(rrow[:, None, :], (L, 128, 496)))
    grow = np.concatenate([f("attn_norm"), f("ffn_norm")], axis=1)
    gvec = np.ascontiguousarray(np.broadcast_to(grow[:, None, :], (L, 128, 2048)))
    consts = np.zeros((128, 528), np.float32)
    p = np.arange(128)
    consts[:, 0:128] = np.eye(128, dtype=np.float32)
    consts[:, 128:256] = (p[:, None] <= p[None, :]).astype(np.float32)
    consts[:, 256:384] = (p[:, None] >= p[None, :]).astype(np.float32)
    consts[:, 384:512] = 1.0
    consts[:, 512:528] = (1.0 / (10000.0 ** (np.arange(0, 32, 2, dtype=np.float32) / 32.0))).astype(np.float32)[None, :]
    shared = {
        "w_in": f("w_in"), "w_out": f("w_out"), "a_w_uq": f("a_w_uq"), "a_w_ukv": f("a_w_ukv"), "c_w_s": f("c_w_s"),
        "w_group": f("w_group"), "w_expert": f("w_expert"), "w1": f("w1"), "w3": f("w3"), "w2": f("w2"),
        "pvec": pvec, "convw": convw, "rvec": rvec, "gvec": gvec, "consts": consts,
    }
    return x, pos, shared


def kernel(**inputs):
    x, pos, shared = _host_inputs(inputs)
    B = x.shape[0]
    per = B // NCORES
    nc, _ = build(n_seq=per, n_layers=L)
    in_maps = []
    for c in range(NCORES):
        m = dict(shared)
        m["x"] = np.ascontiguousarray(x[c * per:(c + 1) * per])
        pc = pos[c * per:(c + 1) * per].reshape(per, NT, 128).transpose(0, 2, 1)
        m["posT"] = np.ascontiguousarray(pc)
        in_maps.append(m)
    res = run_bass_kernel_spmd(nc, in_maps, core_ids=list(range(NCORES)))
    return np.concatenate([r["out"] for r in res.results], axis=0).astype(np.float32)
```
